# Optimizing a Trainium2 kernel written in Bass

```python
import jax
import jax.numpy as jnp
from jax import lax
import numpy as np

D_MODEL = 1024
BATCH = 2
SEQ = 8192
DEPTH = 2

GRID_W = 64
CTX_LEN = 256
N_MIXERS = 2
EXPAND = 2
D_INNER = EXPAND * D_MODEL
MLSTM_HEADS = 4
MLSTM_HEAD_DIM = D_INNER // MLSTM_HEADS
QKV_BLOCK = 4
CONV_W = 3
CHUNK = 64
POOL_WINDOWS = (2, 4, 8, 16)
POOL_GROUPS = len(POOL_WINDOWS)
POOL_GROUP_DIM = D_INNER // POOL_GROUPS
N_LAYERS_A = (DEPTH + 1) // 2
N_LAYERS_B = DEPTH // 2
EPS = 1e-6

kernel_name = "hybrid_mlstm_pool_diffusion_block"


def rmsnorm(x, g):
    xf = x.astype(jnp.float32)
    y = xf * lax.rsqrt(jnp.mean(xf * xf, axis=-1, keepdims=True) + EPS)
    return (y * g.astype(jnp.float32)).astype(x.dtype)


def ada(cond, w, b):
    mod = jax.nn.silu(cond) @ w + b
    return jnp.split(mod, 3, axis=-1)


def modulate(h, shift, scale):
    return h * (1.0 + scale) + shift


def short_conv(x, w, b):
    pad = CONV_W // 2
    t = x.shape[1]
    xp = jnp.pad(x, ((0, 0), (pad, CONV_W - 1 - pad), (0, 0)))
    y = xp[:, 0:t] * w[0]
    for j in range(1, CONV_W):
        y = y + xp[:, j:j + t] * w[j]
    return y + b


def headwise(x, w):
    bsz, t, e = x.shape
    xb = x.reshape(bsz, t, e // QKV_BLOCK, QKV_BLOCK)
    return jnp.einsum("btgi,gio->btgo", xb, w).reshape(bsz, t, e)


def to_heads(x):
    bsz, t, _ = x.shape
    return x.reshape(bsz, t, MLSTM_HEADS, MLSTM_HEAD_DIM).transpose(0, 2, 1, 3).astype(jnp.float32)


def to_chunks(a):
    bsz, h, t = a.shape[:3]
    a = a.reshape(bsz, h, t // CHUNK, CHUNK, *a.shape[3:])
    return jnp.moveaxis(a, 2, 0)


def mlstm_scan(q, k, v, log_i, log_f, state):
    tril = jnp.tril(jnp.ones((CHUNK, CHUNK), dtype=bool))

    def step(carry, chunk):
        c_mem, n_vec, m_prev = carry
        qc, kc, vc, ic, fc = chunk
        b = jnp.cumsum(fc, axis=-1)
        d = jnp.where(tril, b[..., :, None] - b[..., None, :] + ic[..., None, :], -jnp.inf)
        m_inter = b + m_prev[..., None]
        m_t = jnp.maximum(m_inter, jnp.max(d, axis=-1))
        s = jnp.einsum("bhld,bhsd->bhls", qc, kc) * jnp.exp(d - m_t[..., None])
        w_inter = jnp.exp(m_inter - m_t)
        num = jnp.einsum("bhls,bhse->bhle", s, vc) + w_inter[..., None] * jnp.einsum("bhld,bhde->bhle", qc, c_mem)
        den = jnp.sum(s, axis=-1) + w_inter * jnp.einsum("bhld,bhd->bhl", qc, n_vec)
        h = num / jnp.maximum(jnp.abs(den), jnp.exp(-m_t))[..., None]
        b_last = b[..., -1]
        g = b_last[..., None] - b + ic
        m_new = jnp.maximum(b_last + m_prev, jnp.max(g, axis=-1))
        decay = jnp.exp(b_last + m_prev - m_new)
        wk = kc * jnp.exp(g - m_new[..., None])[..., None]
        c_new = decay[..., None, None] * c_mem + jnp.einsum("bhsd,bhse->bhde", wk, vc)
        n_new = decay[..., None] * n_vec + jnp.sum(wk, axis=2)
        return (c_new, n_new, m_new), h

    xs = (to_chunks(q), to_chunks(k), to_chunks(v), to_chunks(log_i), to_chunks(log_f))
    state, h = lax.scan(step, state, xs)
    h = jnp.moveaxis(h, 0, 2)
    bsz, nh, nc, l, dh = h.shape
    return h.reshape(bsz, nh, nc * l, dh), state


def mlstm_features(h, w_in, conv_w, conv_b, w_q, w_k, w_v):
    xm, z, og = jnp.split(h @ w_in, 3, axis=-1)
    xc = jax.nn.silu(short_conv(xm, conv_w, conv_b))
    q = headwise(xc, w_q)
    k = headwise(xc, w_k) * (MLSTM_HEAD_DIM ** -0.5)
    v = headwise(xm, w_v)
    return q, k, v, xc, z, og


def gate_logs(qkv, w_g, b_g):
    pre = (qkv @ w_g + b_g).astype(jnp.float32)
    log_i = pre[..., :MLSTM_HEADS]
    log_f = jax.nn.log_sigmoid(pre[..., MLSTM_HEADS:])
    return log_i.transpose(0, 2, 1), log_f.transpose(0, 2, 1)


def bidir_mlstm(q, k, v, w_gf, b_gf, w_gb, b_gb, init_f, init_b):
    qkv = jnp.concatenate([q, k, v], axis=-1)
    li_f, lf_f = gate_logs(qkv, w_gf, b_gf)
    li_b, lf_b = gate_logs(qkv, w_gb, b_gb)
    qh, kh, vh = to_heads(q), to_heads(k), to_heads(v)
    h_f, st_f = mlstm_scan(qh, kh, vh, li_f, lf_f, init_f)
    flip = lambda a: jnp.flip(a, axis=2)
    h_b, st_b = mlstm_scan(flip(qh), flip(kh), flip(vh), jnp.flip(li_b, -1), jnp.flip(lf_b, -1), init_b)
    return h_f + flip(h_b), st_f, st_b


def mlstm_output(hsum, xc, z, og, head_norm, skip, w_out):
    hs = hsum.transpose(0, 2, 1, 3)
    mu = jnp.mean(hs, axis=-1, keepdims=True)
    var = jnp.mean(jnp.square(hs - mu), axis=-1, keepdims=True)
    hn = ((hs - mu) * lax.rsqrt(var + EPS)).reshape(hs.shape[0], hs.shape[1], D_INNER).astype(xc.dtype) * head_norm
    y = jax.nn.sigmoid(og) * hn + skip * xc
    return (y * jax.nn.silu(z)) @ w_out


def mlstm_mixer(h_lat, h_ctx, ctx_out, w_in, conv_w, conv_b, w_q, w_k, w_v,
                w_gf, b_gf, w_gb, b_gb, head_norm, skip, w_out):
    bsz = h_lat.shape[0]
    zero = (jnp.zeros((bsz, MLSTM_HEADS, MLSTM_HEAD_DIM, MLSTM_HEAD_DIM), jnp.float32),
            jnp.zeros((bsz, MLSTM_HEADS, MLSTM_HEAD_DIM), jnp.float32),
            jnp.zeros((bsz, MLSTM_HEADS), jnp.float32))
    qc, kc, vc, xcc, zc, ogc = mlstm_features(h_ctx, w_in, conv_w, conv_b, w_q, w_k, w_v)
    h_c, st_f, st_b = bidir_mlstm(qc, kc, vc, w_gf, b_gf, w_gb, b_gb, zero, zero)
    ql, kl, vl, xcl, zl, ogl = mlstm_features(h_lat, w_in, conv_w, conv_b, w_q, w_k, w_v)
    h_l, _, _ = bidir_mlstm(ql, kl, vl, w_gf, b_gf, w_gb, b_gb, st_f, st_b)
    y_lat = mlstm_output(h_l, xcl, zl, ogl, head_norm, skip, w_out)
    y_ctx = mlstm_output(h_c, xcc, zc, ogc, head_norm, skip, w_out) if ctx_out else None
    return y_lat, y_ctx


def box_mean(x, w, axis):
    n = x.shape[axis]
    pad = [(0, 0)] * x.ndim
    pad[axis] = (1, 0)
    cs = jnp.pad(jnp.cumsum(x, axis=axis), pad)
    idx = np.arange(n)
    lo = np.clip(idx - w // 2, 0, n)
    hi = np.clip(idx + w - w // 2, 0, n)
    s = jnp.take(cs, hi, axis=axis) - jnp.take(cs, lo, axis=axis)
    shape = [1] * x.ndim
    shape[axis] = n
    cnt = (hi - lo).astype(np.float32).reshape(shape)
    return s / cnt


def pool_mixer(h, grid, w_in, w_pool, pool_scale, w_out):
    u, z = jnp.split(h @ w_in, 2, axis=-1)
    bsz, t, _ = u.shape
    ug = u.reshape(bsz, t, POOL_GROUPS, POOL_GROUP_DIM).astype(jnp.float32)
    diffs = []
    for g, w in enumerate(POOL_WINDOWS):
        xg = ug[:, :, g, :]
        if grid:
            rows = t // GRID_W
            x2 = xg.reshape(bsz, rows, GRID_W, POOL_GROUP_DIM)
            m = box_mean(box_mean(x2, w, 2), w, 1).reshape(bsz, t, POOL_GROUP_DIM)
        else:
            m = box_mean(xg, w, 1)
        diffs.append(m - xg)
    d = jnp.stack(diffs, axis=2).astype(h.dtype)
    y = jnp.einsum("btgi,gio->btgo", d, w_pool).reshape(bsz, t, D_INNER) * pool_scale
    return (y * jax.nn.silu(z)) @ w_out


def setup_inputs(seed: int = 0) -> dict:
    key = jax.random.key(seed)
    ks = jax.random.split(key, 32)
    nrm = lambda k, shape, s: jax.random.normal(k, shape, jnp.float32) * s
    d, e, h = D_MODEL, D_INNER, MLSTM_HEADS
    na, nb = N_LAYERS_A, N_LAYERS_B
    b_gate = lambda k: jnp.concatenate([
        nrm(k, (na, h), 0.1),
        jnp.broadcast_to(jnp.linspace(3.0, 6.0, h, dtype=jnp.float32), (na, h)) + nrm(jax.random.fold_in(k, 1), (na, h), 0.1)], axis=-1)
    return {
        "x": nrm(ks[0], (BATCH, SEQ, d), 1.0),
        "c": nrm(ks[1], (BATCH, d), 1.0),
        "ctx": nrm(ks[2], (BATCH, CTX_LEN, d), 1.0),
        "c_ctx": nrm(ks[3], (d,), 1.0),
        "w_ada": nrm(ks[4], (DEPTH, d, 3 * d), d ** -0.5),
        "b_ada": nrm(ks[5], (DEPTH, 3 * d), 0.02),
        "a_norm_pre": 1.0 + nrm(ks[6], (na, d), 0.02),
        "a_norm_post": 1.0 + nrm(ks[7], (na, d), 0.02),
        "a_w_in": nrm(ks[8], (na, d, 3 * e), d ** -0.5),
        "a_conv_w": nrm(ks[9], (na, CONV_W, e), CONV_W ** -0.5),
        "a_conv_b": nrm(ks[10], (na, e), 0.02),
        "a_w_q": nrm(ks[11], (na, e // QKV_BLOCK, QKV_BLOCK, QKV_BLOCK), QKV_BLOCK ** -0.5),
        "a_w_k": nrm(ks[12], (na, e // QKV_BLOCK, QKV_BLOCK, QKV_BLOCK), QKV_BLOCK ** -0.5),
        "a_w_v": nrm(ks[13], (na, e // QKV_BLOCK, QKV_BLOCK, QKV_BLOCK), QKV_BLOCK ** -0.5),
        "a_w_gate_f": nrm(ks[14], (na, 3 * e, 2 * h), 0.5 * (3 * e) ** -0.5),
        "a_b_gate_f": b_gate(ks[15]),
        "a_w_gate_b": nrm(ks[16], (na, 3 * e, 2 * h), 0.5 * (3 * e) ** -0.5),
        "a_b_gate_b": b_gate(ks[17]),
        "a_head_norm": 1.0 + nrm(ks[18], (na, e), 0.02),
        "a_skip": 1.0 + nrm(ks[19], (na, e), 0.02),
        "a_w_out": nrm(ks[20], (na, e, d), e ** -0.5),
        "b_norm_pre": 1.0 + nrm(ks[21], (nb, d), 0.02),
        "b_norm_post": 1.0 + nrm(ks[22], (nb, d), 0.02),
        "b_w_in": nrm(ks[23], (nb, d, 2 * e), d ** -0.5),
        "b_w_pool": nrm(ks[24], (nb, POOL_GROUPS, POOL_GROUP_DIM, POOL_GROUP_DIM), POOL_GROUP_DIM ** -0.5),
        "b_pool_scale": 1.0 + nrm(ks[25], (nb, e), 0.1),
        "b_w_out": nrm(ks[26], (nb, e, d), e ** -0.5),
    }


def reference(x, c, ctx, c_ctx, w_ada, b_ada,
              a_norm_pre, a_norm_post, a_w_in, a_conv_w, a_conv_b, a_w_q, a_w_k, a_w_v,
              a_w_gate_f, a_b_gate_f, a_w_gate_b, a_b_gate_b, a_head_norm, a_skip, a_w_out,
              b_norm_pre, b_norm_post, b_w_in, b_w_pool, b_pool_scale, b_w_out):
    for i in range(DEPTH):
        kind = i % N_MIXERS
        j = i // N_MIXERS
        ctx_out = any(l % N_MIXERS == 0 for l in range(i + 1, DEPTH))
        ctx_in = (kind == 0) or ctx_out
        shift, scale, gate = ada(c, w_ada[i], b_ada[i])
        pre_g = a_norm_pre[j] if kind == 0 else b_norm_pre[j]
        post_g = a_norm_post[j] if kind == 0 else b_norm_post[j]
        h_lat = modulate(rmsnorm(x, pre_g), shift[:, None, :], scale[:, None, :])
        if ctx_in:
            c_shift, c_scale, c_gate = ada(c_ctx, w_ada[i], b_ada[i])
            h_ctx = modulate(rmsnorm(ctx, pre_g), c_shift, c_scale)
        if kind == 0:
            y_lat, y_ctx = mlstm_mixer(h_lat, h_ctx, ctx_out, a_w_in[j], a_conv_w[j], a_conv_b[j],
                                       a_w_q[j], a_w_k[j], a_w_v[j], a_w_gate_f[j], a_b_gate_f[j],
                                       a_w_gate_b[j], a_b_gate_b[j], a_head_norm[j], a_skip[j], a_w_out[j])
        else:
            y_lat = pool_mixer(h_lat, True, b_w_in[j], b_w_pool[j], b_pool_scale[j], b_w_out[j])
            y_ctx = pool_mixer(h_ctx, False, b_w_in[j], b_w_pool[j], b_pool_scale[j], b_w_out[j]) if ctx_out else None
        x = x + gate[:, None, :] * rmsnorm(y_lat, post_g)
        if ctx_out:
            ctx = ctx + c_gate * rmsnorm(y_ctx, post_g)
    return x
```

```python
import contextlib
import numpy as np
import ml_dtypes
import concourse.bass as bass
import concourse.mybir as mybir
from concourse.bass_utils import run_bass_kernel_spmd

F32 = mybir.dt.float32
BF16 = mybir.dt.bfloat16
ALU = mybir.AluOpType
AF = mybir.ActivationFunctionType
AX = mybir.AxisListType

SAME_SYNC = True
EPS = 1e-6
NHEAD = 4
DH = 512
TCTX = 256
TLAT = 8192
TT = TCTX + TLAT
KSCALE = float(DH ** -0.5)


class SemSlot:
    __slots__ = ("cnt", "handle", "idx")

    def __init__(self, idx):
        self.cnt = 0
        self.handle = None
        self.idx = idx


class Tok:
    __slots__ = ("name", "lw", "rd", "slot")

    def __init__(self, name):
        self.name = name
        self.lw = {}
        self.rd = {}
        self.slot = None


class Prog:
    ENG = ("pe", "act", "dve", "pool", "sp")

    def __init__(self, nc):
        self.nc = nc
        self.stack = contextlib.ExitStack()
        self.eng_ops = {e: [] for e in self.ENG}
        self.nsem = 0
        self.banks = None
        self.bank_i = 0
        self.scopes = []
        self.fence = {}
        self.free_slots = []
        self.all_slots = []

    def sb(self, name, shape, dt=F32):
        self.uid = getattr(self, "uid", 0) + 1
        st = self.scopes[-1][0] if self.scopes else self.stack
        return st.enter_context(self.nc.sbuf_tensor("sb%d_%s" % (self.uid, name), list(shape), dt))

    def push_scope(self):
        self.scopes.append((contextlib.ExitStack(), []))

    def pop_scope(self):
        st, toks = self.scopes.pop()
        for t in toks:
            for dct in (t.lw, t.rd):
                for k, v in dct.items():
                    if self.fence.get(k, -1) < v:
                        self.fence[k] = v
            if t.slot is not None:
                self.free_slots.append(t.slot)
                t.slot = None
        st.close()

    def ps(self, name, shape, dt=F32):
        return self.stack.enter_context(self.nc.psum_tensor("ps_" + name, list(shape), dt))

    def sem(self, name):
        self.nsem += 1
        return self.stack.enter_context(self.nc.semaphore("%s_%d" % (name, self.nsem)))

    def tile(self, name, shape, dt=F32):
        tk = Tok(name)
        tk.lw = dict(self.fence)
        if self.scopes:
            self.scopes[-1][1].append(tk)
        return self.sb(name, shape, dt), tk

    def init_banks(self, n=8):
        self.banks = [(self.ps("bank%d" % i, [128, 512], F32), Tok("bank%d" % i)) for i in range(n)]

    def bank(self):
        b = self.banks[self.bank_i % len(self.banks)]
        self.bank_i += 1
        return b

    def op(self, eng, fn, r=(), w=(), dma=None, inc=16):
        assert fn is not None or not w, "wait-only op cannot produce"
        lst = self.eng_ops[eng]
        i = len(lst)
        if dma is not None:
            if dma.slot is None:
                if self.free_slots:
                    dma.slot = self.free_slots.pop()
                else:
                    dma.slot = SemSlot(len(self.all_slots))
                    self.all_slots.append(dma.slot)
            dma.slot.cnt += inc
            ev = (("d", dma.slot), dma.slot.cnt)
        else:
            ev = (("e", eng), i)
        deps = {}

        def add(d):
            for k, v in d.items():
                if deps.get(k, -1) < v:
                    deps[k] = v

        for b in r:
            add(b.lw)
        for b in w:
            add(b.lw)
            add(b.rd)
        for b in r:
            if b in w:
                continue
            if b.rd.get(ev[0], -1) < ev[1]:
                b.rd[ev[0]] = ev[1]
        for b in w:
            if b.rd:
                b.lw = {}
                b.rd = {}
            b.lw[ev[0]] = ev[1]
        rec = dict(eng=eng, fn=fn, deps=deps, ev=ev, sig=False, dma=dma, waits=[], inc=inc, dslot=(dma.slot if dma is not None else None))
        lst.append(rec)
        return rec

    def dma(self, out, in_, r=(), w=(), tok=None, q="sp"):
        self.op(q, lambda e: e.dma_start(out=out, in_=in_), r=r, w=w, dma=tok)

    def finalize(self):
        nc = self.nc
        for eng in self.ENG:
            waited = {}
            for rec in self.eng_ops[eng]:
                for k, v in rec["deps"].items():
                    if k[0] == "e" and k[1] == eng:
                        if eng in ("pe", "sp") or not SAME_SYNC:
                            continue
                    if waited.get(k, -1) >= v:
                        continue
                    waited[k] = v
                    rec["waits"].append((k, v))
                    if k[0] == "e":
                        self.eng_ops[k[1]][v]["sig"] = True
        self.esem = {}
        self.rank = {}
        for eng in self.ENG:
            if eng == "sp":
                continue
            self.esem[eng] = self.sem("s_" + eng)
            c = 0
            rk = []
            for rec in self.eng_ops[eng]:
                if rec["sig"] and rec["dma"] is None:
                    c += 1
                rk.append(c)
            self.rank[eng] = rk
        for sl in self.all_slots:
            sl.handle = self.sem("dq%d" % sl.idx)
        with nc.Block() as block:
            def emit(name, e):
                for rec in self.eng_ops[name]:
                    for k, v in rec["waits"]:
                        if k[0] == "e":
                            e.wait_ge(self.esem[k[1]], self.rank[k[1]][v])
                        else:
                            e.wait_ge(k[1].handle, v)
                    if rec["fn"] is None:
                        continue
                    ins = rec["fn"](e)
                    if rec["dma"] is not None:
                        ins.then_inc(rec["dslot"].handle, rec["inc"])
                    elif rec["sig"]:
                        ins.then_inc(self.esem[name], 1)

            @block.tensor
            def _(e):
                emit("pe", e)

            @block.scalar
            def _(e):
                emit("act", e)

            @block.vector
            def _(e):
                emit("dve", e)

            @block.gpsimd
            def _(e):
                emit("pool", e)

            @block.sync
            def _(e):
                emit("sp", e)
        self.stack.close()

    def stats(self):
        return {e: len(v) for e, v in self.eng_ops.items()}


class Ring:
    def __init__(self, P, name, n, shape, dt=F32):
        self.items = [P.tile("%s%d" % (name, i), shape, dt) for i in range(n)]
        self.i = 0

    def next(self):
        it = self.items[self.i % len(self.items)]
        self.i += 1
        return it


def emit_consts(P, D):
    K = {}
    K["idf"], K["T_idf"] = P.tile("idf", [128, 128])
    K["idb"], K["T_idb"] = P.tile("idb", [128, 128], BF16)
    K["ones"], K["T_ones"] = P.tile("ones", [128, 128])
    K["nh"], K["T_nh"] = P.tile("nh", [128, 1])
    P.dma(K["idf"][:], D["ident"], w=[K["T_idf"]], tok=K["T_idf"])
    P.op("dve", lambda e: e.tensor_copy(out=K["idb"][:], in_=K["idf"][:]), r=[K["T_idf"]], w=[K["T_idb"]])
    P.op("pool", lambda e: e.memset(K["ones"][:], 1.0), w=[K["T_ones"]])
    P.op("pool", lambda e: e.memset(K["nh"][:], -0.5), w=[K["T_nh"]])
    return K


def emit_ada(P, K, cvec_d, ncv, w_ada_d, b_ada_d, col_lo, ncols, mods, wslots, screp):
    cv, T_cv = P.tile("ada_c", [128, 8, ncv])
    sg, T_sg = P.tile("ada_sg", [128, 8, ncv])
    P.dma(cv[:], cvec_d, w=[T_cv], tok=T_cv)
    P.op("act", lambda e: e.activation(out=sg[:], in_=cv[:], func=AF.Sigmoid), r=[T_cv], w=[T_sg])
    P.op("dve", lambda e: e.tensor_tensor(out=sg[:], in0=sg[:], in1=cv[:], op=ALU.mult), r=[T_sg, T_cv], w=[T_sg])
    rep, T_rep = screp
    repv = rep.rearrange("p (v d m) -> p v d m", v=ncv, d=8)
    for v in range(ncv):
        for dc in range(8):
            P.op("dve", lambda e, v=v, dc=dc: e.tensor_scalar(out=repv[:, v, dc, :], in0=K["ones"][:], scalar1=sg[:, dc, v:v + 1],
                                                             scalar2=None, op0=ALU.mult), r=[K["T_ones"], T_sg], w=[T_rep])
    bb = Ring(P, "ada_b", 2, [128, 256])
    wview = w_ada_d.rearrange("(dc p) c -> p dc c", p=128)
    for cb in range(ncols // 256):
        c0 = col_lo + cb * 256
        wt, T_wt = wslots[cb % len(wslots)]
        P.dma(wt, wview[:, :, c0:c0 + 256], w=[T_wt], tok=T_wt)
        bt, T_bt = bb.next()
        P.dma(bt[:], b_ada_d[c0:c0 + 256].partition_broadcast(128), w=[T_bt], tok=T_bt)
        for v in range(ncv):
            bk, T_bk = P.bank()
            for dc in range(8):
                P.op("pe", lambda e, v=v, dc=dc, bk=bk, wt=wt: e.matmul(bk[:, 0:256], lhsT=repv[:, v, dc, :], rhs=wt[:, dc, :],
                                                                       start=(dc == 0), stop=(dc == 7)), r=[T_rep, T_wt], w=[T_bk])
            mt, T_mt = mods[v]
            P.op("dve", lambda e, bk=bk, mt=mt, bt=bt, cb=cb: e.tensor_tensor(out=mt[:, cb * 256:(cb + 1) * 256], in0=bk[:, 0:256], in1=bt[:],
                                                                             op=ALU.add), r=[T_bk, T_bt], w=[T_mt])


def emit_norm_A(P, K, xt, T_xt, gs, T_gs, sh, T_sh, rings):
    st, T_st = rings["st"].next()
    junk, T_junk = rings["junk"].next()
    tmp, T_tmp = rings["tmp"].next()
    hb, T_hb = rings["hb"].next()
    P.op("act", lambda e: e.activation(out=junk[:], in_=xt[:], func=AF.Square, accum_out=st[:, 0:1]), r=[T_xt], w=[T_junk, T_st])
    P.op("dve", lambda e: e.tensor_scalar(out=st[:, 1:2], in0=st[:, 0:1], scalar1=1.0 / 1024, scalar2=EPS, op0=ALU.mult, op1=ALU.add),
         r=[T_st], w=[T_st])
    P.op("pool", lambda e: e.tensor_tensor(out=st[:, 2:3], in0=st[:, 1:2], in1=K["nh"][:], op=ALU.pow), r=[T_st, K["T_nh"]], w=[T_st])
    P.op("dve", lambda e: e.scalar_tensor_tensor(out=tmp[:], in0=xt[:], scalar=st[:, 2:3], in1=gs, op0=ALU.mult, op1=ALU.mult),
         r=[T_xt, T_st, T_gs], w=[T_tmp])
    P.op("pool", lambda e: e.tensor_tensor(out=hb[:], in0=tmp[:], in1=sh, op=ALU.add), r=[T_tmp, T_sh], w=[T_hb])
    return hb, T_hb


def emit_norm_B(P, K, hb, T_hb, hT_dst, T_hT):
    bk, T_bk = P.bank()
    bkb = bk[:].bitcast(BF16).rearrange("p (a b) -> p a b", a=8)
    for dc in range(8):
        P.op("pe", lambda e, dc=dc: e.transpose(out=bkb[:, dc, :], in_=hb[:, dc * 128:(dc + 1) * 128], identity=K["idb"][:]),
             r=[T_hb, K["T_idb"]], w=[T_bk])
    P.op("act", lambda e: e.copy(out=hT_dst, in_=bkb), r=[T_bk], w=[T_hT])


def emit_norm_T(P, K, xt, T_xt, gs, T_gs, sh, T_sh, rings, hT_dst, T_hT):
    hb, T_hb = emit_norm_A(P, K, xt, T_xt, gs, T_gs, sh, T_sh, rings)
    emit_norm_B(P, K, hb, T_hb, hT_dst, T_hT)


def emit_p1(P, K, D, TK=None):
    blocks = [(0, 2, 1)] + [(TCTX + 512 * i, 4, 0) for i in range(16)]
    rings = dict(st=Ring(P, "n_st", 4, [128, 4]), junk=Ring(P, "n_junk", 1, [128, 1024], BF16),
                 tmp=Ring(P, "n_tmp", 2, [128, 1024]), hb=Ring(P, "n_hb", 3, [128, 1024], BF16))
    xr = Ring(P, "xt", 3, [128, 1024])
    hTr = Ring(P, "hT", 2, [128, 8, 512], BF16)
    xmTr = Ring(P, "xmT", 2, [128, 4, 514])
    lastc = Ring(P, "lastc", 3, [128, 4, 1])
    cv, T_cv = P.tile("cv", [128, 4, 512])
    sgt, T_sgt = P.tile("sgt", [128, 4, 512])
    xcT, T_xcT = P.tile("xcT", [128, 4, 512], BF16)
    xmTb, T_xmTb = P.tile("xmTb", [128, 4, 512], BF16)
    SZr = Ring(P, "SZ", 2, [128, 4, 512])
    tzr = Ring(P, "tz", 2, [128, 512])
    tor = Ring(P, "to", 2, [128, 512])
    Ar = Ring(P, "Ast", 2, [128, 512])
    st4 = Ring(P, "st4", 3, [128, 4, 512], BF16)
    gstr = Ring(P, "gst", 2, [16, 512])
    ktr = Ring(P, "kts", 2, [128, 512], BF16)
    vtr = Ring(P, "vts", 2, [128, 512], BF16)
    bcr = Ring(P, "bcs", 2, [128, 512])

    mods = [P.tile("mod%d" % v, [128, 2048]) for v in range(2)]
    wslots = [(cv[:].rearrange("p a b -> p (a b)").rearrange("p (d c) -> p d c", d=8), T_cv),
              (sgt[:].rearrange("p a b -> p (a b)").rearrange("p (d c) -> p d c", d=8), T_sgt)]
    sz0, T_sz0 = SZr.items[0]
    screp = (sz0[:].rearrange("p a b -> p (a b)"), T_sz0)
    emit_ada(P, K, D["cvec"], 2, D["w_ada"], D["b_ada"], 0, 2048, mods, wslots, screp)
    gp, T_gp = rings["tmp"].items[0]
    P.dma(gp[:], D["g_pre"].partition_broadcast(128), w=[T_gp], tok=T_gp)
    for v in range(2):
        mt, T_mt = mods[v]
        P.op("dve", lambda e, mt=mt: e.scalar_tensor_tensor(out=mt[:, 1024:2048], in0=mt[:, 1024:2048], scalar=1.0, in1=gp[:],
                                                           op0=ALU.add, op1=ALU.mult), r=[T_mt, T_gp], w=[T_mt])

    wb = [P.tile("wb%d" % i, [128, 8, 512], BF16) for i in range(3)]
    wview = D["w_in"].rearrange("(dc p) c -> p dc c", p=128)
    for i in range(3):
        for hf in range(2):
            wt, T_wt = wslots[(2 * i + hf) % 2]
            c0 = i * 512 + hf * 256
            P.dma(wt, wview[:, :, c0:c0 + 256], w=[T_wt], tok=T_wt)
            eng = "act" if hf == 0 else "pool"
            if eng == "act":
                P.op("act", lambda e, i=i, hf=hf, wt=wt: e.copy(out=wb[i][0][:, :, hf * 256:(hf + 1) * 256], in_=wt), r=[T_wt], w=[wb[i][1]])
            else:
                P.op("pool", lambda e, i=i, hf=hf, wt=wt: e.tensor_copy(out=wb[i][0][:, :, hf * 256:(hf + 1) * 256], in_=wt), r=[T_wt], w=[wb[i][1]])
    cw, T_cw = P.tile("cw", [128, 4, 3])
    cb, T_cb = P.tile("cb", [128, 4])
    skp, T_skp = P.tile("skp", [128, 4])
    hnb, T_hnb = P.tile("hnb", [128, 512])
    P.dma(cw[:], D["convw"], w=[T_cw], tok=T_cw)
    P.dma(cb[:], D["convb"], w=[T_cb], tok=T_cb)
    P.dma(skp[:], D["skip"], w=[T_skp], tok=T_skp)
    P.dma(hnb[:], D["hn"].partition_broadcast(128), w=[T_hnb], tok=T_hnb)
    BD, T_BD = P.tile("BD", [128, 3, 4, 128])
    BDT, T_BDT = P.tile("BDT", [128, 3, 4, 128])
    wg, T_wg = P.tile("wg", [128, 3, 4, 16])
    P.dma(BD[:], D["BD"], w=[T_BD], tok=T_BD)
    P.dma(BDT[:], D["BDT"], w=[T_BDT], tok=T_BDT)
    P.dma(wg[:], D["wg"], w=[T_wg], tok=T_wg)
    BDb, T_BDb = P.tile("BDb", [128, 3, 4, 128], BF16)
    KS, T_KS = P.tile("KS", [128, 4, 256], BF16)
    Wgc, T_Wgc = P.tile("Wgc", [128, 4, 16], BF16)
    Wgv, T_Wgv = P.tile("Wgv", [128, 4, 16], BF16)
    P.op("dve", lambda e: e.tensor_copy(out=BDb[:], in_=BD[:]), r=[T_BD], w=[T_BDb])
    P.op("act", lambda e: e.mul(out=KS[:, :, 0:128], in_=BD[:, 1, :, :], mul=KSCALE), r=[T_BD], w=[T_KS])
    for fc in range(4):
        P.op("dve", lambda e, fc=fc: e.tensor_scalar(out=KS[:, fc, 128:256], in0=K["idf"][:], scalar1=skp[:, fc:fc + 1], scalar2=None,
                                                     op0=ALU.mult), r=[K["T_idf"], T_skp], w=[T_KS])
    P.op("act", lambda e: e.mul(out=wg[:, 1, :, :], in_=wg[:, 1, :, :], mul=KSCALE), r=[T_wg], w=[T_wg])
    bk, T_bk = P.bank()
    bk2, T_bk2 = P.bank()
    for fc in range(4):
        P.op("pe", lambda e, fc=fc: e.matmul(bk[:, fc * 16:(fc + 1) * 16], lhsT=BDT[:, 0, fc, :], rhs=wg[:, 0, fc, :], start=True, stop=False),
             r=[T_BDT, T_wg], w=[T_bk])
        P.op("pe", lambda e, fc=fc: e.matmul(bk[:, fc * 16:(fc + 1) * 16], lhsT=BDT[:, 1, fc, :], rhs=wg[:, 1, fc, :], start=False, stop=True),
             r=[T_BDT, T_wg], w=[T_bk])
        P.op("pe", lambda e, fc=fc: e.matmul(bk2[:, fc * 16:(fc + 1) * 16], lhsT=BDT[:, 2, fc, :], rhs=wg[:, 2, fc, :], start=True, stop=True),
             r=[T_BDT, T_wg], w=[T_bk2])
    P.op("dve", lambda e: e.tensor_copy(out=Wgc[:].rearrange("p a b -> p (a b)"), in_=bk[:, 0:64]), r=[T_bk], w=[T_Wgc])
    P.op("dve", lambda e: e.tensor_copy(out=Wgv[:].rearrange("p a b -> p (a b)"), in_=bk2[:, 0:64]), r=[T_bk2], w=[T_Wgv])

    out_toks = []
    if TK is None:
        TK = {k: Tok("o_" + k) for k in ("qT", "kT", "ktok", "vtok", "A", "Bc", "gpT")}
    out_toks = list(TK.values())
    xin = D["xin"]
    qTv = D["qT"].rearrange("(fc p) t -> p fc t", p=128)
    kTv = D["kT"].rearrange("(fc p) t -> p fc t", p=128)

    T_cvf = [Tok("cvf%d" % fc) for fc in range(4)]

    hTs = {}

    def S1a(j):
        t0, nt, mi = blocks[j]
        ntok = nt * 128
        hT, T_hT = hTr.next()
        hTs[j] = (hT, T_hT)
        mt, T_mt = mods[mi]
        xts = {}

        def ldx(t):
            xt, T_xt = xr.next()
            P.dma(xt[:], xin[t0 + t * 128:t0 + (t + 1) * 128, :], w=[T_xt], tok=T_xt)
            xts[t] = (xt, T_xt)

        ldx(0)
        if nt > 1:
            ldx(1)
        pend = None
        for t in range(nt):
            if t + 2 < nt:
                ldx(t + 2)
            xt, T_xt = xts.pop(t)
            cur = emit_norm_A(P, K, xt, T_xt, mt[:, 1024:2048], T_mt, mt[:, 0:1024], T_mt, rings)
            if pend is not None:
                emit_norm_B(P, K, pend[0], pend[1], hT[:, :, (t - 1) * 128:t * 128], T_hT)
                yield
            pend = cur
        emit_norm_B(P, K, pend[0], pend[1], hT[:, :, (nt - 1) * 128:nt * 128], T_hT)
        yield

    def S1b(j):
        t0, nt, mi = blocks[j]
        ntok = nt * 128
        hT, T_hT = hTs.pop(j)
        xmT, T_xmT = xmTr.items[j % 2]
        for fc in range(4):
            bk, T_bk = P.bank()
            for dc in range(8):
                P.op("pe", lambda e, fc=fc, dc=dc, bk=bk: e.matmul(bk[:, 0:ntok], lhsT=wb[0][0][:, dc, fc * 128:(fc + 1) * 128],
                                                                   rhs=hT[:, dc, 0:ntok], start=(dc == 0), stop=(dc == 7)),
                     r=[wb[0][1], T_hT], w=[T_bk])
            P.op("act", lambda e, fc=fc, bk=bk: e.copy(out=xmT[:, fc, 1:1 + ntok], in_=bk[:, 0:ntok]), r=[T_bk], w=[T_xmT])
            if fc % 2 == 1:
                yield
        lc, T_lc = lastc.items[j % 3]
        P.op("pool", lambda e: e.tensor_copy(out=lc[:], in_=xmT[:, :, ntok:ntok + 1]), r=[T_xmT], w=[T_lc])
        if mi == 0:
            SZ, T_SZ = SZr.items[j % 2]
            tl = t0 - TCTX
            for t in range(nt):
                bz, T_bz = P.bank()
                bo, T_bo = P.bank()
                for dc in range(8):
                    P.op("pe", lambda e, t=t, dc=dc, bz=bz: e.matmul(bz[:], lhsT=hT[:, dc, t * 128:(t + 1) * 128], rhs=wb[1][0][:, dc, :],
                                                                     start=(dc == 0), stop=(dc == 7)), r=[wb[1][1], T_hT], w=[T_bz])
                for dc in range(8):
                    P.op("pe", lambda e, t=t, dc=dc, bo=bo: e.matmul(bo[:], lhsT=hT[:, dc, t * 128:(t + 1) * 128], rhs=wb[2][0][:, dc, :],
                                                                     start=(dc == 0), stop=(dc == 7)), r=[wb[2][1], T_hT], w=[T_bo])
                tz, T_tz = tzr.next()
                to, T_to = tor.next()
                At, T_At = Ar.next()
                P.op("act", lambda e, bz=bz, tz=tz: e.activation(out=tz[:], in_=bz[:], func=AF.Sigmoid), r=[T_bz], w=[T_tz])
                P.op("dve", lambda e, bz=bz, tz=tz, t=t: e.tensor_tensor(out=SZ[:, t, :], in0=bz[:], in1=tz[:], op=ALU.mult), r=[T_bz, T_tz], w=[T_SZ])
                P.op("act", lambda e, bo=bo, to=to: e.activation(out=to[:], in_=bo[:], func=AF.Sigmoid), r=[T_bo], w=[T_to])
                P.op("pool", lambda e, to=to: e.tensor_tensor(out=to[:], in0=to[:], in1=hnb[:], op=ALU.mult), r=[T_to, T_hnb], w=[T_to])
                P.op("pool", lambda e, to=to, At=At, t=t: e.tensor_tensor(out=At[:], in0=to[:], in1=SZ[:, t, :], op=ALU.mult), r=[T_to, T_SZ], w=[T_At])
                P.dma(D["A"][tl + t * 128:tl + (t + 1) * 128, :], At[:], r=[T_At], w=[TK["A"]], tok=T_At, q="pool")
                yield

    def S2(j, prev, nxt):
        t0, nt, mi = blocks[j]
        ntok = nt * 128
        xmT, T_xmT = xmTr.items[j % 2]
        if prev is None:
            P.op("pool", lambda e: e.memset(xmT[:, :, 0:1], 0.0), w=[T_xmT])
        else:
            lc, T_lc = lastc.items[prev % 3]
            P.op("pool", lambda e: e.tensor_copy(out=xmT[:, :, 0:1], in_=lc[:]), r=[T_lc], w=[T_xmT])
        if nxt is None:
            P.op("pool", lambda e: e.memset(xmT[:, :, ntok + 1:ntok + 2], 0.0), w=[T_xmT])
        else:
            xn, T_xn = xmTr.items[nxt % 2]
            P.op("pool", lambda e: e.tensor_copy(out=xmT[:, :, ntok + 1:ntok + 2], in_=xn[:, :, 1:2]), r=[T_xn], w=[T_xmT])
        for fc in range(4):
            P.op("dve", lambda e, fc=fc: e.tensor_scalar(out=cv[:, fc, 0:ntok], in0=xmT[:, fc, 1:1 + ntok], scalar1=cw[:, fc, 1:2],
                                                         scalar2=cb[:, fc:fc + 1], op0=ALU.mult, op1=ALU.add), r=[T_xmT, T_cw, T_cb],
                 w=[T_cvf[fc]] + ([T_cv] if j == 0 else []))
        for fc in range(4):
            P.op("dve", lambda e, fc=fc: e.scalar_tensor_tensor(out=cv[:, fc, 0:ntok], in0=xmT[:, fc, 0:ntok], scalar=cw[:, fc, 0:1],
                                                                in1=cv[:, fc, 0:ntok], op0=ALU.mult, op1=ALU.add), r=[T_xmT, T_cw], w=[T_cvf[fc]])
        for fc in range(4):
            P.op("dve", lambda e, fc=fc: e.scalar_tensor_tensor(out=cv[:, fc, 0:ntok], in0=xmT[:, fc, 2:2 + ntok], scalar=cw[:, fc, 2:3],
                                                                in1=cv[:, fc, 0:ntok], op0=ALU.mult, op1=ALU.add), r=[T_xmT, T_cw], w=[T_cvf[fc]])
        yield
        P.op("act", lambda e: e.activation(out=sgt[:, :, 0:ntok], in_=cv[:, :, 0:ntok], func=AF.Sigmoid), r=T_cvf, w=[T_sgt])
        P.op("pool", lambda e: e.tensor_tensor(out=xcT[:, 0:2, 0:ntok], in0=cv[:, 0:2, 0:ntok], in1=sgt[:, 0:2, 0:ntok], op=ALU.mult),
             r=T_cvf + [T_sgt], w=[T_xcT])
        P.op("dve", lambda e: e.tensor_tensor(out=xcT[:, 2:4, 0:ntok], in0=cv[:, 2:4, 0:ntok], in1=sgt[:, 2:4, 0:ntok], op=ALU.mult),
             r=T_cvf + [T_sgt], w=[T_xcT])
        P.op("act", lambda e: e.copy(out=xmTb[:, :, 0:ntok], in_=xmT[:, :, 1:1 + ntok]), r=[T_xmT], w=[T_xmTb])
        yield
        for m, dst, okey in ((0, qTv, "qT"), (1, kTv, "kT")):
            stg, T_stg = st4.next()
            for fc in range(4):
                bk, T_bk = P.bank()
                P.op("pe", lambda e, fc=fc, bk=bk, m=m: e.matmul(bk[:, 0:ntok], lhsT=BDb[:, m, fc, :], rhs=xcT[:, fc, 0:ntok], start=True, stop=True),
                     r=[T_BDb, T_xcT], w=[T_bk])
                sc = 1.0 if m == 0 else KSCALE
                if fc % 2 == 0:
                    P.op("act", lambda e, fc=fc, bk=bk, stg=stg, sc=sc: e.mul(out=stg[:, fc, 0:ntok], in_=bk[:, 0:ntok], mul=sc), r=[T_bk], w=[T_stg])
                else:
                    P.op("dve", lambda e, fc=fc, bk=bk, stg=stg, sc=sc: e.tensor_scalar(out=stg[:, fc, 0:ntok], in0=bk[:, 0:ntok], scalar1=sc, scalar2=None, op0=ALU.mult),
                         r=[T_bk], w=[T_stg])
            P.dma(dst[:, :, t0:t0 + ntok], stg[:, :, 0:ntok], r=[T_stg], w=[TK[okey]], tok=T_stg)
            yield
        bk, T_bk = P.bank()
        for fc in range(4):
            P.op("pe", lambda e, fc=fc, bk=bk: e.matmul(bk[0:16, 0:ntok], lhsT=Wgc[:, fc, :], rhs=xcT[:, fc, 0:ntok], start=(fc == 0), stop=False),
                 r=[T_Wgc, T_xcT], w=[T_bk])
        for fc in range(4):
            P.op("pe", lambda e, fc=fc, bk=bk: e.matmul(bk[0:16, 0:ntok], lhsT=Wgv[:, fc, :], rhs=xmTb[:, fc, 0:ntok], start=False, stop=(fc == 3)),
                 r=[T_Wgv, T_xmTb], w=[T_bk])
        gst, T_gst = gstr.next()
        P.op("act", lambda e, bk=bk, gst=gst: e.copy(out=gst[:, 0:ntok], in_=bk[0:16, 0:ntok]), r=[T_bk], w=[T_gst])
        P.dma(D["gpT"][:, t0:t0 + ntok], gst[:, 0:ntok], r=[T_gst], w=[TK["gpT"]], tok=T_gst)
        SZ, T_SZ = SZr.items[j % 2]
        for t in range(nt):
            b1, T_b1 = P.bank()
            b2, T_b2 = P.bank()
            b3, T_b3 = P.bank()
            for fc in range(4):
                bb, T_bb = (b1, T_b1) if fc < 2 else (b2, T_b2)
                P.op("pe", lambda e, fc=fc, bb=bb, t=t: e.matmul(bb[:, (fc % 2) * 256:(fc % 2 + 1) * 256], lhsT=xcT[:, fc, t * 128:(t + 1) * 128],
                                                                 rhs=KS[:, fc, :], start=True, stop=True), r=[T_xcT, T_KS], w=[T_bb])
            for fc in range(4):
                P.op("pe", lambda e, fc=fc, t=t, b3=b3: e.matmul(b3[:, fc * 128:(fc + 1) * 128], lhsT=xmTb[:, fc, t * 128:(t + 1) * 128],
                                                                 rhs=BDb[:, 2, fc, :], start=True, stop=True), r=[T_xmTb, T_BDb], w=[T_b3])
            kts, T_kts = ktr.next()
            vts, T_vts = vtr.next()
            for hf, (bb, T_bb) in enumerate(((b1, T_b1), (b2, T_b2))):
                bv = bb[:].rearrange("p (a b) -> p a b", a=2)
                P.op("act", lambda e, bv=bv, kts=kts, hf=hf: e.copy(out=kts[:, hf * 256:(hf + 1) * 256].rearrange("p (a b) -> p a b", a=2),
                                                                   in_=bv[:, :, 0:128]), r=[T_bb], w=[T_kts])
            P.op("dve", lambda e, b3=b3, vts=vts: e.tensor_copy(out=vts[:], in_=b3[:]), r=[T_b3], w=[T_vts])
            P.dma(D["ktok"][t0 + t * 128:t0 + (t + 1) * 128, :], kts[:], r=[T_kts], w=[TK["ktok"]], tok=T_kts)
            P.dma(D["vtok"][t0 + t * 128:t0 + (t + 1) * 128, :], vts[:], r=[T_vts], w=[TK["vtok"]], tok=T_vts)
            if mi == 0:
                bcs, T_bcs = bcr.next()
                tl = t0 - TCTX
                for hf, (bb, T_bb) in enumerate(((b1, T_b1), (b2, T_b2))):
                    bv = bb[:].rearrange("p (a b) -> p a b", a=2)
                    P.op("dve", lambda e, bv=bv, bcs=bcs, hf=hf, t=t: e.tensor_tensor(
                        out=bcs[:, hf * 256:(hf + 1) * 256].rearrange("p (a b) -> p a b", a=2), in0=bv[:, :, 128:256],
                        in1=SZ[:, t, hf * 256:(hf + 1) * 256].rearrange("p (a b) -> p a b", a=2), op=ALU.mult), r=[T_bb, T_SZ], w=[T_bcs])
                P.dma(D["Bc"][tl + t * 128:tl + (t + 1) * 128, :], bcs[:], r=[T_bcs], w=[TK["Bc"]], tok=T_bcs)
            yield

    def run(*gens):
        gens = list(gens)
        while gens:
            for g in list(gens):
                try:
                    next(g)
                except StopIteration:
                    gens.remove(g)

    nb = len(blocks)
    run(S1a(0)); run(S1b(0))
    run(S2(0, None, None))
    run(S1a(1)); run(S1b(1))
    run(S1a(2)); run(S1b(2))
    for k in range(2, nb):
        gens = [S2(k - 1, (k - 2) if k - 2 >= 1 else None, k)]
        if k + 1 < nb:
            gens.insert(0, S1a(k + 1))
        run(*gens)
        if k + 1 < nb:
            run(S1b(k + 1))
    run(S2(nb - 1, nb - 2, None))
    return out_toks


def host_p1_inputs(inp, b, h):
    f32 = np.float32
    d = {}
    d["xin"] = np.ascontiguousarray(np.concatenate([inp["ctx"][b], inp["x"][b]], axis=0))
    cv = np.stack([inp["c"][b], inp["c_ctx"]], axis=-1)
    d["cvec"] = np.ascontiguousarray(cv.reshape(8, 128, 2).transpose(1, 0, 2))
    d["w_ada"] = np.ascontiguousarray(inp["w_ada"][0])
    d["b_ada"] = np.ascontiguousarray(inp["b_ada"][0])
    d["g_pre"] = np.ascontiguousarray(inp["a_norm_pre"][0])
    w = inp["a_w_in"][0]
    hs = slice(h * DH, (h + 1) * DH)
    d["w_in"] = np.ascontiguousarray(np.concatenate([w[:, 0:2048][:, hs], w[:, 2048:4096][:, hs], w[:, 4096:6144][:, hs]], axis=1))
    d["convw"] = np.ascontiguousarray(inp["a_conv_w"][0][:, hs].T.reshape(4, 128, 3).transpose(1, 0, 2))
    d["convb"] = np.ascontiguousarray(inp["a_conv_b"][0][hs].reshape(4, 128).T)
    d["skip"] = np.ascontiguousarray(inp["a_skip"][0][hs].reshape(4, 128).T)
    d["hn"] = np.ascontiguousarray(inp["a_head_norm"][0][hs])
    BD = np.zeros((128, 3, 4, 128), f32)
    BDT = np.zeros((128, 3, 4, 128), f32)
    for m, key in enumerate(("a_w_q", "a_w_k", "a_w_v")):
        wq = inp[key][0][h * 128:(h + 1) * 128]
        for fc in range(4):
            for g in range(32):
                blk = wq[fc * 32 + g]
                BD[g * 4:(g + 1) * 4, m, fc, g * 4:(g + 1) * 4] = blk
                BDT[g * 4:(g + 1) * 4, m, fc, g * 4:(g + 1) * 4] = blk.T
    d["BD"] = BD
    d["BDT"] = BDT
    wgf = inp["a_w_gate_f"][0]
    wgb = inp["a_w_gate_b"][0]
    wgc = np.concatenate([wgf, wgb], axis=1)
    wg = np.zeros((128, 3, 4, 16), f32)
    for m in range(3):
        rows = wgc[m * 2048 + h * DH:m * 2048 + (h + 1) * DH]
        wg[:, m, :, :] = rows.reshape(4, 128, 16).transpose(1, 0, 2)
    d["wg"] = wg
    d["ident"] = np.eye(128, dtype=f32)
    return d


def build_p1():
    nc = bass.Bass("TRN2", target_bir_lowering=False)
    P = Prog(nc)
    P.init_banks(8)
    D = {}

    def din(name, shape, dt=F32):
        D[name] = nc.dram_tensor(name, list(shape), dt, kind="ExternalInput").ap()

    def dout(name, shape, dt=F32):
        D[name] = nc.dram_tensor(name, list(shape), dt, kind="ExternalOutput").ap()

    din("xin", [TT, 1024]); din("cvec", [128, 8, 2]); din("w_ada", [1024, 3072]); din("b_ada", [3072]); din("g_pre", [1024])
    din("w_in", [1024, 1536]); din("convw", [128, 4, 3]); din("convb", [128, 4]); din("skip", [128, 4]); din("hn", [512])
    din("BD", [128, 3, 4, 128]); din("BDT", [128, 3, 4, 128]); din("wg", [128, 3, 4, 16]); din("ident", [128, 128])
    dout("qT", [512, TT], BF16); dout("kT", [512, TT], BF16); dout("ktok", [TT, 512], BF16); dout("vtok", [TT, 512], BF16)
    dout("A", [TLAT, 512]); dout("Bc", [TLAT, 512]); dout("gpT", [16, TT])
    K = emit_consts(P, D)
    toks = emit_p1(P, K, D)
    P.op("sp", None, r=toks)
    P.finalize()
    return nc, P


NCH = TT // 128
ORDER_F = list(range(NCH))
ORDER_B = [1, 0] + list(range(NCH - 1, 1, -1))


def emit_gates(P, K, D, d, T_GP=None):
    nm = "g%d_" % d
    n = NCH
    X = {}
    pre = []
    for ri in range(2):
        r = 2 * d + ri
        if T_GP is None:
            pt, T_pt = P.tile(nm + "p%d" % ri, [n, 4, 128])
            P.dma(pt[:], D["gsel"][r].rearrange("s (p l) -> p s l", l=128), w=[T_pt], tok=T_pt)
        else:
            pt, T_pt = P.tile(nm + "p%d" % ri, [128, 4, 128])
            GPv = D["GP"].rearrange("r (p l) -> (r p) l", l=128)
            for s_ in range(4):
                P.op("pool", lambda e, pt=pt, s_=s_, r=r: e.indirect_dma_start(out=pt[:, s_, :], out_offset=None, in_=GPv,
                                                                             in_offset=bass.IndirectOffsetOnAxis(ap=K["gidx"][:, r * 4 + s_:r * 4 + s_ + 1], axis=0)),
                     r=[T_GP, K["T_gidx"]], w=[T_pt], dma=T_pt)
        xt, T_x = P.tile(nm + "x%d" % ri, [n, 128])
        P.op("dve", lambda e, pt=pt, xt=xt: e.tensor_tensor(out=xt[:], in0=pt[0:n, 0, :], in1=pt[0:n, 1, :], op=ALU.add), r=[T_pt], w=[T_x])
        P.op("dve", lambda e, pt=pt, xt=xt: e.tensor_tensor(out=xt[:], in0=xt[:], in1=pt[0:n, 2, :], op=ALU.add), r=[T_pt, T_x], w=[T_x])
        P.op("dve", lambda e, pt=pt, xt=xt: e.tensor_tensor(out=xt[:], in0=xt[:], in1=pt[0:n, 3, :], op=ALU.add), r=[T_pt, T_x], w=[T_x])
        P.op("dve", lambda e, xt=xt, r=r: e.tensor_scalar(out=xt[:], in0=xt[:], scalar1=K["gbias"][0:n, r:r + 1], scalar2=None, op0=ALU.add),
             r=[T_x, K["T_gbias"]], w=[T_x])
        if d == 1:
            bk, T_bk = P.bank()
            P.op("pe", lambda e, bk=bk, xt=xt: e.matmul(bk[0:n, 0:128], lhsT=K["perm"][:], rhs=xt[:], start=True, stop=True),
                 r=[K["T_perm"], T_x], w=[T_bk])
            P.op("dve", lambda e, bk=bk, xt=xt: e.tensor_copy(out=xt[:], in_=bk[0:n, 0:128]), r=[T_bk], w=[T_x])
        pre.append((xt, T_x))
    (li, T_li), (lf, T_lf) = pre
    P.op("act", lambda e: e.activation(out=lf[:], in_=lf[:], func=AF.Exp, scale=-1.0), r=[T_lf], w=[T_lf])
    P.op("act", lambda e: e.activation(out=lf[:], in_=lf[:], func=AF.Ln, bias=1.0), r=[T_lf], w=[T_lf])
    G, T_G = P.tile(nm + "G", [n, 128])
    P.op("dve", lambda e: e.tensor_tensor_scan(out=G[:], data0=K["ones"][0:n, :], data1=lf[:], initial=0.0, op0=ALU.mult, op1=ALU.add),
         r=[K["T_ones"], T_lf], w=[T_G])
    sm, T_sm = P.tile(nm + "sm", [n, 8])
    P.op("dve", lambda e: e.tensor_copy(out=sm[:, 0:1], in_=G[:, 127:128]), r=[T_G], w=[T_sm])
    if d == 1:
        P.op("dve", lambda e: e.tensor_scalar(out=G[:], in0=G[:], scalar1=-1.0, scalar2=sm[:, 0:1], op0=ALU.mult, op1=ALU.add), r=[T_G, T_sm], w=[T_G])
        P.op("dve", lambda e: e.tensor_tensor(out=G[:], in0=G[:], in1=lf[:], op=ALU.add), r=[T_G, T_lf], w=[T_G])
    bk, T_bk = P.bank()
    P.op("pe", lambda e: e.matmul(bk[0:n, 0:1], lhsT=K["triu"][:], rhs=sm[:, 0:1], start=True, stop=True), r=[K["T_triu"], T_sm], w=[T_bk])
    P.op("dve", lambda e: e.tensor_copy(out=sm[:, 1:2], in_=bk[0:n, 0:1]), r=[T_bk], w=[T_sm])
    P.op("dve", lambda e: e.tensor_scalar(out=G[:], in0=G[:], scalar1=sm[:, 1:2], scalar2=None, op0=ALU.add), r=[T_G, T_sm], w=[T_G])
    a, T_a = P.tile(nm + "a", [n, 128])
    P.op("dve", lambda e: e.tensor_tensor(out=a[:], in0=li[:], in1=G[:], op=ALU.add), r=[T_li, T_G], w=[T_a])
    P.op("dve", lambda e: e.tensor_reduce(out=sm[:, 2:3], in_=a[:], axis=AX.X, op=ALU.max), r=[T_a], w=[T_sm])
    bk2, T_bk2 = P.bank()
    P.op("pe", lambda e: e.matmul(bk2[0:1, 0:n], lhsT=sm[:, 2:3], rhs=K["idf"][0:n, 0:n], start=True, stop=True), r=[T_sm, K["T_idf"]], w=[T_bk2])
    row, T_row = P.tile(nm + "row", [1, 4, n])
    P.op("dve", lambda e: e.tensor_copy(out=row[:, 0, :], in_=bk2[0:1, 0:n]), r=[T_bk2], w=[T_row])
    P.op("dve", lambda e: e.tensor_tensor_scan(out=row[:, 1, :], data0=row[:, 0, :], data1=row[:, 0, :], initial=0.0, op0=ALU.max, op1=ALU.max),
         r=[T_row], w=[T_row])
    P.op("dve", lambda e: e.memset(row[:, 2, 0:1], 0.0), w=[T_row])
    P.op("dve", lambda e: e.tensor_copy(out=row[:, 2, 1:n], in_=row[:, 1, 0:n - 1]), r=[T_row], w=[T_row])
    P.op("dve", lambda e: e.tensor_tensor(out=row[:, 3, :], in0=row[:, 2, :], in1=row[:, 1, :], op=ALU.subtract), r=[T_row], w=[T_row])
    P.op("act", lambda e: e.activation(out=row[:, 3, :], in_=row[:, 3, :], func=AF.Exp), r=[T_row], w=[T_row])
    bk3, T_bk3 = P.bank()
    P.op("pe", lambda e: e.matmul(bk3[0:n, 0:1], lhsT=row[:, 2, :], rhs=K["ones"][0:1, 0:1], start=True, stop=True), r=[T_row, K["T_ones"]], w=[T_bk3])
    P.op("dve", lambda e: e.tensor_scalar(out=sm[:, 3:4], in0=bk3[0:n, 0:1], scalar1=-1.0, scalar2=None, op0=ALU.mult), r=[T_bk3], w=[T_sm])
    bk4, T_bk4 = P.bank()
    P.op("pe", lambda e: e.matmul(bk4[:, 0:n], lhsT=K["ones"][0:1, :], rhs=row[:, 3, :], start=True, stop=True), r=[T_row, K["T_ones"]], w=[T_bk4])
    decb, T_decb = P.tile(nm + "decb", [128, n])
    P.op("dve", lambda e: e.tensor_copy(out=decb[:], in_=bk4[:, 0:n]), r=[T_bk4], w=[T_decb])
    P.op("act", lambda e: e.activation(out=a[:], in_=a[:], func=AF.Exp, bias=sm[:, 3:4]), r=[T_a, T_sm], w=[T_a])
    P.op("act", lambda e: e.activation(out=G[:], in_=G[:], func=AF.Exp, bias=sm[:, 3:4]), r=[T_G, T_sm], w=[T_G])
    outs = {}
    for key, (src, T_src) in (("e1T", (a, T_a)), ("thrT", (G, T_G))):
        bk5, T_bk5 = P.bank()
        P.op("pe", lambda e, bk5=bk5, src=src: e.transpose(out=bk5[:, 0:n], in_=src[:], identity=K["idf"][0:n, 0:n]), r=[T_src, K["T_idf"]], w=[T_bk5])
        dst, T_dst = P.tile(nm + key, [128, n])
        P.op("dve", lambda e, bk5=bk5, dst=dst: e.tensor_copy(out=dst[:], in_=bk5[:, 0:n]), r=[T_bk5], w=[T_dst])
        outs[key] = (dst, T_dst)
    outs["decb"] = (decb, T_decb)
    return outs


def emit_scan(P, K, D, G, TK=None, fuse_out=False):
    qTv = D["qT"].rearrange("(fc p) t -> p fc t", p=128)
    kTv = D["kT"].rearrange("(fc p) t -> p fc t", p=128)
    NR = 4
    KTr = Ring(P, "KT", NR, [128, 4, 128], BF16)
    QTr = Ring(P, "QT", NR, [128, 4, 128], BF16)
    Ktr = Ring(P, "Kt", NR, [128, 512], BF16)
    Vtr = Ring(P, "Vt", NR, [128, 512], BF16)
    Ver = Ring(P, "Ve", 3, [128, 513], BF16)
    Smr = Ring(P, "Sm", 2, [128, 128], BF16)
    dnr = Ring(P, "dn", 2, [128, 4])
    hor = Ring(P, "ho", 3, [128, 512])
    Cb = [[P.tile("Cb%d_%d" % (d, i), [128, 4, 513], BF16)[0] for i in range(2)] for d in range(2)]
    TC = [[[Tok("TC%d_%d_%d" % (d, i, dc)) for dc in range(5)] for i in range(2)] for d in range(2)]
    for d in range(2):
        c0 = Cb[d][0]
        P.op("pool", lambda e, c0=c0: e.memset(c0[:], 0.0), w=TC[d][0])
    orders = (ORDER_F, ORDER_B)
    rk = (lambda k: [TK[k]]) if TK is not None else (lambda k: [])
    out_toks = [[None] * 64 for _ in range(2)]
    items = [(s, d) for s in range(NCH) for d in range(2)]
    L = {}
    if fuse_out:
        NO = 11
        ho2r = Ring(P, "ho2", NO, [128, 512])
        hotr = Ring(P, "hoth", NO, [128, 512])
        Ainr = Ring(P, "Ain", NO, [128, 512])
        Binr = Ring(P, "Bin", NO, [128, 512])
        ojunk, T_ojunk = P.tile("ojunk", [128, 512], BF16)
        ostr = Ring(P, "ost", NO, [128, 8])
        ybr = Ring(P, "yb", 3, [128, 512], BF16)
        T_yc = [Tok("yc%d" % k) for k in range(8)]
        T_Yg = Tok("Yg")
        T_cc = Tok("ccY")
        kcount = [0] * 8
        pend = []

    def ost1(X):
        hf, T_hf, hb, T_hb, st, T_st = X["ho"], X["T_ho"], X["hoth"], X["T_hoth"], X["st"], X["T_st"]
        P.op("dve", lambda e: e.tensor_tensor(out=hf[:], in0=hf[:], in1=hb[:], op=ALU.add), r=[T_hf, T_hb], w=[T_hf])
        P.op("act", lambda e: e.activation(out=ojunk[:], in_=hf[:], func=AF.Copy, accum_out=st[:, 0:1]), r=[T_hf], w=[T_ojunk, T_st])
        P.op("act", lambda e: e.activation(out=ojunk[:], in_=hf[:], func=AF.Square, accum_out=st[:, 1:2]), r=[T_hf], w=[T_ojunk, T_st])

    def ost2(X):
        st, T_st = X["st"], X["T_st"]
        P.op("dve", lambda e: e.tensor_scalar(out=st[:, 2:3], in0=st[:, 0:1], scalar1=1.0 / 512, scalar2=None, op0=ALU.mult), r=[T_st], w=[T_st])
        P.op("dve", lambda e: e.tensor_tensor(out=st[:, 3:4], in0=st[:, 2:3], in1=st[:, 2:3], op=ALU.mult), r=[T_st], w=[T_st])
        P.op("dve", lambda e: e.scalar_tensor_tensor(out=st[:, 4:5], in0=st[:, 1:2], scalar=1.0 / 512, in1=st[:, 3:4], op0=ALU.mult, op1=ALU.subtract),
             r=[T_st], w=[T_st])
        P.op("dve", lambda e: e.tensor_scalar(out=st[:, 4:5], in0=st[:, 4:5], scalar1=EPS, scalar2=None, op0=ALU.add), r=[T_st], w=[T_st])
        P.op("pool", lambda e: e.tensor_tensor(out=st[:, 5:6], in0=st[:, 4:5], in1=K["nh"][:], op=ALU.pow), r=[T_st, K["T_nh"]], w=[T_st])

    def ost3(X):
        hf, T_hf, st, T_st, At, T_At, Bt, T_Bt, cl = X["ho"], X["T_ho"], X["st"], X["T_st"], X["At"], X["T_At"], X["Bt"], X["T_Bt"], X["cl"]
        P.op("dve", lambda e: e.tensor_scalar(out=hf[:], in0=hf[:], scalar1=st[:, 2:3], scalar2=st[:, 5:6], op0=ALU.subtract, op1=ALU.mult),
             r=[T_hf, T_st], w=[T_hf])
        P.op("dve", lambda e: e.tensor_tensor(out=hf[:], in0=hf[:], in1=At[:], op=ALU.mult), r=[T_hf, T_At], w=[T_hf])
        yb, T_yb = ybr.next()
        P.op("dve", lambda e: e.tensor_tensor(out=yb[:], in0=hf[:], in1=Bt[:], op=ALU.add), r=[T_hf, T_Bt], w=[T_yb])
        k = cl // 8
        P.dma(D["ycin%d" % k][(cl % 8) * 128:(cl % 8 + 1) * 128, :], yb[:], r=[T_yb], w=[T_yc[k]], tok=T_yb)
        kcount[k] += 1
        if kcount[k] == 8:
            P.op("pool", lambda e: e.collective_compute("AllGather", ALU.bypass, replica_groups=RG4, ins=[D["ycin%d" % k]], outs=[D["Yg"][k]]),
                 r=[T_yc[k]], w=[T_Yg], dma=T_cc, inc=1)

    def advance(flush=False):
        for X in list(pend):
            X["age"] += 1
            if X["age"] == 7:
                ost1(X)
            elif X["age"] == 8:
                ost2(X)
            elif X["age"] == 9:
                ost3(X)
                pend.remove(X)

    def loads(i):
        s, d = items[i]
        c = orders[d][s]
        t0 = c * 128
        lat = c >= 2
        KT, T_KT = KTr.next(); QT, T_QT = QTr.next(); Kt, T_Kt = Ktr.next(); Vt, T_Vt = Vtr.next()
        P.dma(Kt[:], D["ktok"][t0:t0 + 128, :], r=rk("ktok"), w=[T_Kt], tok=T_Kt)
        P.dma(Vt[:], D["vtok"][t0:t0 + 128, :], r=rk("vtok"), w=[T_Vt], tok=T_Vt)
        if lat:
            P.dma(KT[:], kTv[:, :, t0:t0 + 128], r=rk("kT"), w=[T_KT], tok=T_KT)
            P.dma(QT[:], qTv[:, :, t0:t0 + 128], r=rk("qT"), w=[T_QT], tok=T_QT)
        L[i] = dict(KT=KT, T_KT=T_KT, QT=QT, T_QT=T_QT, Kt=Kt, T_Kt=T_Kt, Vt=Vt, T_Vt=T_Vt)

    def prescale(i):
        s, d = items[i]
        e1T, T_e1T = G[d]["e1T"]
        Ve, T_Ve = Ver.next()
        Vt, T_Vt = L[i]["Vt"], L[i]["T_Vt"]
        P.op("dve", lambda e, Ve=Ve, Vt=Vt, s=s, e1T=e1T: e.tensor_scalar(out=Ve[:, 0:512], in0=Vt[:], scalar1=e1T[:, s:s + 1], scalar2=None,
                                                                          op0=ALU.mult), r=[T_Vt, T_e1T], w=[T_Ve])
        P.op("act", lambda e, Ve=Ve, s=s, e1T=e1T: e.copy(out=Ve[:, 512:513], in_=e1T[:, s:s + 1]), r=[T_e1T], w=[T_Ve])
        L[i]["Ve"] = Ve
        L[i]["T_Ve"] = T_Ve

    def compute(i):
        s, d = items[i]
        c = orders[d][s]
        lat = c >= 2
        cl = c - 2
        thrT, T_thr = G[d]["thrT"]
        decb, T_decb = G[d]["decb"]
        Cc, TCc = Cb[d][s % 2], TC[d][s % 2]
        Cn, TCn = Cb[d][(s + 1) % 2], TC[d][(s + 1) % 2]
        X = L.pop(i)
        KT, T_KT, QT, T_QT, Kt, T_Kt, Ve, T_Ve = X["KT"], X["T_KT"], X["QT"], X["T_QT"], X["Kt"], X["T_Kt"], X["Ve"], X["T_Ve"]
        (bs, T_bs), (bn, T_bn), bu0, bu1 = [P.banks[4 * d + j] for j in range(4)]
        bus = (bu0, bu1)

        def state(dc):
            bu, T_bu = bus[dc % 2]
            P.op("pe", lambda e, dc=dc, bu=bu, Cc=Cc: e.matmul(bu[:], lhsT=K["idb"][:], rhs=Cc[:, dc, 0:512], start=True, stop=False), r=[K["T_idb"], TCc[dc]], w=[T_bu])
            P.op("pe", lambda e, dc=dc, bu=bu, Kt=Kt, Ve=Ve: e.matmul(bu[:], lhsT=Kt[:, dc * 128:(dc + 1) * 128], rhs=Ve[:, 0:512], start=False, stop=True),
                 r=[T_Kt, T_Ve], w=[T_bu])
            if dc % 2 == 0:
                P.op("act", lambda e, dc=dc, bu=bu, Cn=Cn, s=s, decb=decb: e.activation(out=Cn[:, dc, 0:512], in_=bu[:], func=AF.Copy, scale=decb[:, s:s + 1]),
                     r=[T_bu, T_decb], w=[TCn[dc]])
            else:
                P.op("dve", lambda e, dc=dc, bu=bu, Cn=Cn, s=s, decb=decb: e.tensor_scalar(out=Cn[:, dc, 0:512], in0=bu[:], scalar1=decb[:, s:s + 1], scalar2=None, op0=ALU.mult),
                     r=[T_bu, T_decb], w=[TCn[dc]])

        if lat:
            for dc in range(4):
                P.op("pe", lambda e, dc=dc, bs=bs, KT=KT, QT=QT: e.matmul(bs[:, 0:128], lhsT=KT[:, dc, :], rhs=QT[:, dc, :], start=(dc == 0), stop=(dc == 3)),
                     r=[T_KT, T_QT], w=[T_bs])
            Sm, T_Sm = Smr.next()
            P.op("dve", lambda e, bs=bs, Sm=Sm, d=d: e.tensor_tensor(out=Sm[:], in0=bs[:, 0:128], in1=K["mask"][:, d, :], op=ALU.mult),
                 r=[T_bs, K["T_mask"]], w=[T_Sm])
        state(0)
        state(1)
        if lat:
            for dc in range(4):
                P.op("pe", lambda e, dc=dc, bn=bn, QT=QT, Cc=Cc: e.matmul(bn[:], lhsT=QT[:, dc, :], rhs=Cc[:, dc, 0:512], start=(dc == 0), stop=False),
                     r=[T_QT, TCc[dc]], w=[T_bn])
            P.op("pe", lambda e, bn=bn, Sm=Sm, Ve=Ve: e.matmul(bn[:], lhsT=Sm[:], rhs=Ve[:, 0:512], start=False, stop=True), r=[T_Sm, T_Ve], w=[T_bn])
            for dc in range(4):
                P.op("pe", lambda e, dc=dc, bs=bs, QT=QT, Cc=Cc: e.matmul(bs[:, 256:257], lhsT=QT[:, dc, :], rhs=Cc[:, dc, 512:513], start=(dc == 0), stop=False),
                     r=[T_QT, TCc[4]], w=[T_bs])
            P.op("pe", lambda e, bs=bs, Sm=Sm, Ve=Ve: e.matmul(bs[:, 256:257], lhsT=Sm[:], rhs=Ve[:, 512:513], start=False, stop=True), r=[T_Sm, T_Ve], w=[T_bs])
        for dc in range(4):
            P.op("pe", lambda e, dc=dc, bs=bs, Cc=Cc: e.matmul(bs[:, 260 + dc:261 + dc], lhsT=K["idb"][:], rhs=Cc[:, dc, 512:513], start=True, stop=False),
                 r=[K["T_idb"], TCc[4]], w=[T_bs])
            P.op("pe", lambda e, dc=dc, bs=bs, Kt=Kt, Ve=Ve: e.matmul(bs[:, 260 + dc:261 + dc], lhsT=Kt[:, dc * 128:(dc + 1) * 128], rhs=Ve[:, 512:513], start=False, stop=True),
                 r=[T_Kt, T_Ve], w=[T_bs])
        state(2)
        state(3)
        P.op("dve", lambda e, bs=bs, Cn=Cn, s=s, decb=decb: e.tensor_scalar(out=Cn[:, :, 512], in0=bs[:, 260:264], scalar1=decb[:, s:s + 1], scalar2=None, op0=ALU.mult),
             r=[T_bs, T_decb], w=[TCn[4]])
        if lat:
            dn, T_dn = dnr.next()
            P.op("dve", lambda e, bs=bs, dn=dn, s=s, thrT=thrT: e.tensor_scalar(out=dn[:, 0:1], in0=bs[:, 256:257], scalar1=-1.0, scalar2=thrT[:, s:s + 1], op0=ALU.mult, op1=ALU.max),
                 r=[T_bs, T_thr], w=[T_dn])
            P.op("dve", lambda e, bs=bs, dn=dn: e.tensor_tensor(out=dn[:, 1:2], in0=bs[:, 256:257], in1=dn[:, 0:1], op=ALU.max), r=[T_bs, T_dn], w=[T_dn])
            P.op("dve", lambda e, dn=dn: e.reciprocal(out=dn[:, 3:4], in_=dn[:, 1:2]), r=[T_dn], w=[T_dn])
            second = fuse_out and ((d == 0 and cl >= 32) or (d == 1 and cl <= 31))
            if not second:
                ho, T_ho = hor.next()
                P.op("act", lambda e, bn=bn, ho=ho, dn=dn: e.activation(out=ho[:], in_=bn[:], func=AF.Copy, scale=dn[:, 3:4]), r=[T_bn, T_dn], w=[T_ho])
                T_o = Tok("h_o%d_%d" % (d, cl))
                out_toks[d][cl] = T_o
                P.dma(D["hdir%d" % d][cl * 128:(cl + 1) * 128, :], ho[:], r=[T_ho], w=[T_o], tok=T_ho)
            else:
                ho, T_ho = ho2r.next()
                P.op("act", lambda e, bn=bn, ho=ho, dn=dn: e.activation(out=ho[:], in_=bn[:], func=AF.Copy, scale=dn[:, 3:4]), r=[T_bn, T_dn], w=[T_ho])
                hoth, T_hoth = hotr.next(); At, T_At = Ainr.next(); Bt, T_Bt = Binr.next(); st, T_st = ostr.next()
                rows = slice(cl * 128, (cl + 1) * 128)
                P.dma(hoth[:], D["hdir%d" % (1 - d)][rows, :], r=[out_toks[1 - d][cl]], w=[T_hoth], tok=T_hoth)
                P.dma(At[:], D["A"][rows, :], r=[TK["A"]], w=[T_At], tok=T_At)
                P.dma(Bt[:], D["Bc"][rows, :], r=[TK["Bc"]], w=[T_Bt], tok=T_Bt)
                pend.append(dict(age=0, cl=cl, ho=ho, T_ho=T_ho, hoth=hoth, T_hoth=T_hoth, At=At, T_At=T_At, Bt=Bt, T_Bt=T_Bt, st=st, T_st=T_st))
        if fuse_out:
            advance()

    n = len(items)
    for idx in range(-2, n):
        if 0 <= idx + 2 < n:
            loads(idx + 2)
        if 0 <= idx + 1 < n:
            prescale(idx + 1)
        if idx >= 0:
            compute(idx)
    if fuse_out:
        while pend:
            advance()
        return T_Yg
    return out_toks


def emit_outstage(P, K, D, h_toks):
    wo, T_wo = P.tile("wo", [128, 4, 1024], BF16)
    wst = Ring(P, "wost", 2, [128, 1024])
    wov = D["w_out"].rearrange("(fc p) c -> p fc c", p=128)
    for fc in range(4):
        wt, T_wt = wst.next()
        P.dma(wt[:], wov[:, fc, :], w=[T_wt], tok=T_wt)
        P.op("pool", lambda e, fc=fc, wt=wt: e.tensor_copy(out=wo[:, fc, :], in_=wt[:]), r=[T_wt], w=[T_wo])
    hfr = Ring(P, "hf", 2, [128, 512])
    hbr = Ring(P, "hb", 2, [128, 512])
    Ar = Ring(P, "Ain", 2, [128, 512])
    Br = Ring(P, "Bin", 2, [128, 512])
    junk, T_junk = P.tile("ojunk", [128, 512], BF16)
    str_ = Ring(P, "ost", 2, [128, 8])
    ybr = Ring(P, "yb", 2, [128, 512], BF16)
    yTr = Ring(P, "yT", 2, [128, 4, 128], BF16)
    psr = Ring(P, "pst", 2, [128, 1024])
    outs = []
    for cl in range(64):
        hf, T_hf = hfr.next(); hb, T_hb = hbr.next(); At, T_At = Ar.next(); Bt, T_Bt = Br.next()
        rows = slice(cl * 128, (cl + 1) * 128)
        P.dma(hf[:], D["hdir0"][rows, :], r=[h_toks[0][cl]], w=[T_hf], tok=T_hf)
        P.dma(hb[:], D["hdir1"][rows, :], r=[h_toks[1][cl]], w=[T_hb], tok=T_hb)
        P.dma(At[:], D["A"][rows, :], w=[T_At], tok=T_At)
        P.dma(Bt[:], D["Bc"][rows, :], w=[T_Bt], tok=T_Bt)
        st, T_st = str_.next()
        P.op("pool", lambda e, hf=hf, hb=hb: e.tensor_tensor(out=hf[:], in0=hf[:], in1=hb[:], op=ALU.add), r=[T_hf, T_hb], w=[T_hf])
        P.op("act", lambda e, hf=hf, st=st: e.activation(out=junk[:], in_=hf[:], func=AF.Copy, accum_out=st[:, 0:1]), r=[T_hf], w=[T_junk, T_st])
        P.op("act", lambda e, hf=hf, st=st: e.activation(out=junk[:], in_=hf[:], func=AF.Square, accum_out=st[:, 1:2]), r=[T_hf], w=[T_junk, T_st])
        P.op("dve", lambda e, st=st: e.tensor_scalar(out=st[:, 2:3], in0=st[:, 0:1], scalar1=1.0 / 512, scalar2=None, op0=ALU.mult), r=[T_st], w=[T_st])
        P.op("dve", lambda e, st=st: e.tensor_tensor(out=st[:, 3:4], in0=st[:, 2:3], in1=st[:, 2:3], op=ALU.mult), r=[T_st], w=[T_st])
        P.op("dve", lambda e, st=st: e.scalar_tensor_tensor(out=st[:, 4:5], in0=st[:, 1:2], scalar=1.0 / 512, in1=st[:, 3:4], op0=ALU.mult, op1=ALU.subtract),
             r=[T_st], w=[T_st])
        P.op("dve", lambda e, st=st: e.tensor_scalar(out=st[:, 4:5], in0=st[:, 4:5], scalar1=EPS, scalar2=None, op0=ALU.add), r=[T_st], w=[T_st])
        P.op("pool", lambda e, st=st: e.tensor_tensor(out=st[:, 5:6], in0=st[:, 4:5], in1=K["nh"][:], op=ALU.pow), r=[T_st, K["T_nh"]], w=[T_st])
        P.op("dve", lambda e, hf=hf, st=st: e.tensor_scalar(out=hf[:], in0=hf[:], scalar1=st[:, 2:3], scalar2=st[:, 5:6], op0=ALU.subtract, op1=ALU.mult),
             r=[T_hf, T_st], w=[T_hf])
        P.op("pool", lambda e, hf=hf, At=At: e.tensor_tensor(out=hf[:], in0=hf[:], in1=At[:], op=ALU.mult), r=[T_hf, T_At], w=[T_hf])
        yb, T_yb = ybr.next()
        P.op("dve", lambda e, hf=hf, Bt=Bt, yb=yb: e.tensor_tensor(out=yb[:], in0=hf[:], in1=Bt[:], op=ALU.add), r=[T_hf, T_Bt], w=[T_yb])
        bk, T_bk = P.bank()
        bkb = bk[:].bitcast(BF16).rearrange("p (a b) -> p a b", a=8)
        for fc in range(4):
            P.op("pe", lambda e, fc=fc, bkb=bkb, yb=yb: e.transpose(out=bkb[:, fc, :], in_=yb[:, fc * 128:(fc + 1) * 128], identity=K["idb"][:]),
                 r=[T_yb, K["T_idb"]], w=[T_bk])
        yT, T_yT = yTr.next()
        P.op("act", lambda e, bkb=bkb, yT=yT: e.copy(out=yT[:], in_=bkb[:, 0:4, :]), r=[T_bk], w=[T_yT])
        pst, T_pst = psr.next()
        for hfi in range(2):
            bo, T_bo = P.bank()
            for fc in range(4):
                P.op("pe", lambda e, fc=fc, bo=bo, yT=yT, hfi=hfi: e.matmul(bo[:], lhsT=yT[:, fc, :], rhs=wo[:, fc, hfi * 512:(hfi + 1) * 512], start=(fc == 0), stop=(fc == 3)),
                     r=[T_yT, T_wo], w=[T_bo])
            if hfi == 0:
                P.op("act", lambda e, bo=bo, pst=pst: e.copy(out=pst[:, 0:512], in_=bo[:]), r=[T_bo], w=[T_pst])
            else:
                P.op("dve", lambda e, bo=bo, pst=pst: e.tensor_copy(out=pst[:, 512:1024], in_=bo[:]), r=[T_bo], w=[T_pst])
        T_o = Tok("part_o")
        outs.append(T_o)
        P.dma(D["part"][rows, :], pst[:], r=[T_pst], w=[T_o], tok=T_pst)
    return outs


def p2_consts(P, K, D):
    for name, shape in (("gbias", [128, 4]), ("perm", [NCH, NCH]), ("triu", [NCH, NCH]), ("mask", [128, 2, 128])):
        K[name], K["T_" + name] = P.tile("k_" + name, shape)
        P.dma(K[name][:], D[name], w=[K["T_" + name]], tok=K["T_" + name])


def host_p2_consts(inp, h):
    f32 = np.float32
    d = {}
    bf = inp["a_b_gate_f"][0]
    bb = inp["a_b_gate_b"][0]
    d["gbias"] = np.tile(np.array([bf[h], bf[4 + h], bb[h], bb[4 + h]], f32)[None, :], (128, 1))
    perm = np.zeros((NCH, NCH), f32)
    for s, c in enumerate(ORDER_B):
        perm[c, s] = 1.0
    d["perm"] = perm
    d["triu"] = np.triu(np.ones((NCH, NCH), f32), 1)
    m = np.zeros((128, 2, 128), f32)
    m[:, 0, :] = np.triu(np.ones((128, 128), f32))
    m[:, 1, :] = np.tril(np.ones((128, 128), f32))
    d["mask"] = m
    d["ident"] = np.eye(128, dtype=f32)
    return d


def build_p2():
    nc = bass.Bass("TRN2", target_bir_lowering=False)
    P = Prog(nc)
    P.init_banks(8)
    D = {}

    def din(name, shape, dt=F32):
        D[name] = nc.dram_tensor(name, list(shape), dt, kind="ExternalInput").ap()

    def dout(name, shape, dt=F32):
        D[name] = nc.dram_tensor(name, list(shape), dt, kind="ExternalOutput").ap()

    din("qT", [512, TT], BF16); din("kT", [512, TT], BF16); din("ktok", [TT, 512], BF16); din("vtok", [TT, 512], BF16)
    din("A", [TLAT, 512]); din("Bc", [TLAT, 512]); din("gsel", [4, 4, TT]); din("gbias", [128, 4]); din("w_out", [512, 1024])
    din("perm", [NCH, NCH]); din("triu", [NCH, NCH]); din("mask", [128, 2, 128]); din("ident", [128, 128])
    D["hdir0"] = nc.dram_tensor("hdir0", [TLAT, 512], F32, kind="Internal").ap()
    D["hdir1"] = nc.dram_tensor("hdir1", [TLAT, 512], F32, kind="Internal").ap()
    dout("part", [TLAT, 1024])
    K = emit_consts(P, D)
    p2_consts(P, K, D)
    G = [emit_gates(P, K, D, d) for d in range(2)]
    h_toks = emit_scan(P, K, D, G)
    outs = emit_outstage(P, K, D, h_toks)
    P.op("sp", None, r=outs)
    P.finalize()
    return nc, P


POOLW = (2, 4, 8, 16)
DELTAS = ((-1, 0), (-1, 0, 1), (-2, -1, 0, 1, 2), (-4, -3, -2, -1, 0, 1, 2, 3, 4))
BOFF = (0, 2, 5, 10)
NWT = 24


def host_bn(q):
    BN = np.zeros((16, 19, 128, 128), np.float32)
    ar = np.arange(128)
    for j in range(16):
        gt = q * 16 + j
        blk = 0
        for g, w in enumerate(POOLW):
            lo_off, hi_off = -(w // 2), w - 1 - w // 2
            for dl in DELTAS[g]:
                it = gt + dl
                M = BN[j, blk]
                if 0 <= it < 64:
                    for tl in range(128):
                        r = 2 * gt + tl // 64
                        c = tl % 64
                        r_lo = max(r + lo_off, 0); r_hi = min(r + hi_off, 127)
                        c_lo = max(c + lo_off, 0); c_hi = min(c + hi_off, 63)
                        coef = 1.0 / ((r_hi - r_lo + 1) * (c_hi - c_lo + 1))
                        for rr in (2 * it, 2 * it + 1):
                            if r_lo <= rr <= r_hi:
                                tp0 = (rr - 2 * it) * 64
                                M[tp0 + c_lo:tp0 + c_hi + 1, tl] = coef
                    if dl == 0:
                        M[ar, ar] -= 1.0
                blk += 1
    return BN.astype(ml_dtypes.bfloat16)


def emit_p3_ada(P, K, D):
    g0m = [P.tile("g0m", [128, 1024])]
    m1 = [P.tile("m1", [128, 3072])]
    P.push_scope()
    wsl = Ring(P, "adaw", 2, [128, 8, 256])
    wslots = [(t[:], tk) for t, tk in wsl.items]
    scr, T_scr = P.tile("screp", [128, 1024])
    gv, T_gv = P.tile("gvb", [128, 1024])
    emit_ada(P, K, D["cvec"], 1, D["w_ada0"], D["b_ada0"], 2048, 1024, g0m, wslots, (scr[:], T_scr))
    emit_ada(P, K, D["cvec"], 1, D["w_ada1"], D["b_ada1"], 0, 3072, m1, wslots, (scr[:], T_scr))
    gg0, T_gg0 = g0m[0]
    mm1, T_m1 = m1[0]
    for key, dst, T_dst, sl, addone in (("g_post0", gg0, T_gg0, slice(0, 1024), False), ("g_pre1", mm1, T_m1, slice(1024, 2048), True),
                                        ("g_post1", mm1, T_m1, slice(2048, 3072), False)):
        P.dma(gv[:], D[key].partition_broadcast(128), w=[T_gv], tok=T_gv)
        if addone:
            P.op("dve", lambda e, dst=dst, sl=sl: e.scalar_tensor_tensor(out=dst[:, sl], in0=dst[:, sl], scalar=1.0, in1=gv[:], op0=ALU.add, op1=ALU.mult),
                 r=[T_dst, T_gv], w=[T_dst])
        else:
            P.op("dve", lambda e, dst=dst, sl=sl: e.tensor_tensor(out=dst[:, sl], in0=dst[:, sl], in1=gv[:], op=ALU.mult), r=[T_dst, T_gv], w=[T_dst])
    P.pop_scope()
    return g0m, m1


def emit_p3(P, K, D, fused=False, T_Yg=None, ada=None):
    if ada is None:
        ada = emit_p3_ada(P, K, D)
    g0m, m1 = ada
    rings = dict(st=Ring(P, "n_st", 6, [128, 4]), junk=Ring(P, "n_junk", 1, [128, 1024], BF16),
                 tmp=Ring(P, "n_tmp", 2, [128, 1024]), hb=Ring(P, "n_hb", 3, [128, 1024], BF16))
    P.push_scope()
    wu, T_wu = P.tile("wu", [128, 8, 2048], BF16)
    wz, T_wz = P.tile("wz", [128, 8, 2048], BF16)
    if fused:
        wo0, T_wo0 = P.tile("wo0", [128, 16, 1024], BF16)
    P.push_scope()
    wsl = Ring(P, "adaw2", 2, [128, 8, 256])
    wslots = [(t[:], tk) for t, tk in wsl.items]
    gg0, T_gg0 = g0m[0]
    mm1, T_m1 = m1[0]
    sh1 = mm1[:, 0:1024]; gs1 = mm1[:, 1024:2048]; gg1 = mm1[:, 2048:3072]
    wview = D["w_in1"].rearrange("(dc p) c -> p dc c", p=128)
    i = 0
    for dst, T_dst, cbase in ((wu, T_wu, 0), (wz, T_wz, 2048)):
        for cb in range(8):
            wt, T_wt = wslots[i % 2]
            P.dma(wt, wview[:, :, cbase + cb * 256:cbase + (cb + 1) * 256], w=[T_wt], tok=T_wt)
            if i % 2 == 0:
                P.op("act", lambda e, dst=dst, cb=cb, wt=wt: e.copy(out=dst[:, :, cb * 256:(cb + 1) * 256], in_=wt), r=[T_wt], w=[T_dst])
            else:
                P.op("pool", lambda e, dst=dst, cb=cb, wt=wt: e.tensor_copy(out=dst[:, :, cb * 256:(cb + 1) * 256], in_=wt), r=[T_wt], w=[T_dst])
            i += 1
    if fused:
        wov0 = D["w_out0"].rearrange("(fc p) c -> p fc c", p=128)
        for fc2 in range(8):
            wt, T_wt = wslots[fc2 % 2]
            wtv = wt.rearrange("p a b -> p (a b)").rearrange("p (f c) -> p f c", f=2)
            P.dma(wtv, wov0[:, fc2 * 2:(fc2 + 1) * 2, :], w=[T_wt], tok=T_wt)
            if fc2 % 2 == 0:
                P.op("act", lambda e, fc2=fc2, wtv=wtv: e.copy(out=wo0[:, fc2 * 2:(fc2 + 1) * 2, :], in_=wtv), r=[T_wt], w=[T_wo0])
            else:
                P.op("pool", lambda e, fc2=fc2, wtv=wtv: e.tensor_copy(out=wo0[:, fc2 * 2:(fc2 + 1) * 2, :], in_=wtv), r=[T_wt], w=[T_wo0])
    P.pop_scope()
    P.push_scope()
    xr = Ring(P, "xw", 2, [128, 1024])
    if fused:
        ytr = Ring(P, "ytl", 3, [128, 4, 512], BF16)
        yTr = Ring(P, "yTw", 2, [128, 16, 128], BF16)
        ysr = Ring(P, "ysum", 2, [128, 1024])
        Ygv = D["Yg"].rearrange("k r c -> (k r) c")
    else:
        pr = Ring(P, "pw", 12, [128, 1024])
    x1r = Ring(P, "x1", 2, [128, 1024])
    hTr = Ring(P, "h1T", 2, [128, 8, 512], BF16)
    ustr = Ring(P, "ust", 2, [128, 2048], BF16)
    zsr = Ring(P, "zst", 3, [128, 512], BF16)
    sgr = Ring(P, "zsg", 2, [128, 512])
    T_us = [Tok("us%d" % i) for i in range(NWT)]
    T_x1s = [Tok("x1s%d" % i) for i in range(16)]
    T_sz = [[Tok("sz%d_%d" % (tb, f)) for f in range(16)] for tb in range(4)]
    ST = {}
    ST2 = {}
    hTcur = {}

    def GA(wt_i):
        yt, T_yt = ytr.next()
        for r_ in range(4):
            P.op("pool", lambda e, yt=yt, r_=r_, wt_i=wt_i: e.indirect_dma_start(out=yt[:, r_, :], out_offset=None, in_=Ygv,
                                                                               in_offset=bass.IndirectOffsetOnAxis(ap=K["yidx"][:, wt_i * 4 + r_:wt_i * 4 + r_ + 1], axis=0)),
                 r=[T_Yg, K["T_yidx"]], w=[T_yt], dma=T_yt)
        ST[wt_i] = dict(yt=yt, T_yt=T_yt)

    def SA(wt_i):
        rows = slice(wt_i * 128, (wt_i + 1) * 128)
        X = ST.setdefault(wt_i, {})
        xt, T_xt = xr.next()
        P.dma(xt[:], D["xw"][rows, :], w=[T_xt], tok=T_xt)
        X["xt"], X["T_xt"] = xt, T_xt
        if fused:
            yt, T_yt = X["yt"], X["T_yt"]
            yTw, T_yTw = yTr.next()
            for hb_ in range(2):
                bk, T_bk = P.bank()
                bkb = bk[:].bitcast(BF16).rearrange("p (a b) -> p a b", a=8)
                for i8 in range(8):
                    fch = hb_ * 8 + i8
                    P.op("pe", lambda e, bkb=bkb, i8=i8, fch=fch, yt=yt: e.transpose(out=bkb[:, i8, :], in_=yt[:, fch // 4, (fch % 4) * 128:(fch % 4 + 1) * 128],
                                                                                    identity=K["idb"][:]), r=[T_yt, K["T_idb"]], w=[T_bk])
                if hb_ == 0:
                    P.op("act", lambda e, bkb=bkb, yTw=yTw: e.copy(out=yTw[:, 0:8, :], in_=bkb), r=[T_bk], w=[T_yTw])
                else:
                    P.op("dve", lambda e, bkb=bkb, yTw=yTw: e.tensor_copy(out=yTw[:, 8:16, :], in_=bkb), r=[T_bk], w=[T_yTw])
            p0, T0 = ysr.next()
            for hf in range(2):
                bk, T_bk = P.bank()
                for fch in range(16):
                    P.op("pe", lambda e, bk=bk, fch=fch, hf=hf, yTw=yTw: e.matmul(bk[:], lhsT=yTw[:, fch, :], rhs=wo0[:, fch, hf * 512:(hf + 1) * 512],
                                                                                 start=(fch == 0), stop=(fch == 15)), r=[T_yTw, T_wo0], w=[T_bk])
                if hf == 0:
                    P.op("act", lambda e, bk=bk, p0=p0: e.copy(out=p0[:, 0:512], in_=bk[:]), r=[T_bk], w=[T0])
                else:
                    P.op("dve", lambda e, bk=bk, p0=p0: e.tensor_copy(out=p0[:, 512:1024], in_=bk[:]), r=[T_bk], w=[T0])
        else:
            ps_ = []
            for s_ in range(4):
                pt, T_pt = pr.next()
                P.dma(pt[:], D["parts"][s_, rows, :], w=[T_pt], tok=T_pt)
                ps_.append((pt, T_pt))
            (p0, T0), (p1, T1), (p2, T2), (p3, T3) = ps_
            P.op("dve", lambda e, p0=p0, p1=p1: e.tensor_tensor(out=p0[:], in0=p0[:], in1=p1[:], op=ALU.add), r=[T0, T1], w=[T0])
            P.op("pool", lambda e, p2=p2, p3=p3: e.tensor_tensor(out=p2[:], in0=p2[:], in1=p3[:], op=ALU.add), r=[T2, T3], w=[T2])
            P.op("dve", lambda e, p0=p0, p2=p2: e.tensor_tensor(out=p0[:], in0=p0[:], in1=p2[:], op=ALU.add), r=[T0, T2], w=[T0])
        X["p0"], X["T0"] = p0, T0

    def SB(wt_i):
        wb, t = divmod(wt_i, 4)
        own = 1 <= wb <= 4
        rows = slice(wt_i * 128, (wt_i + 1) * 128)
        X = ST.pop(wt_i)
        xt, T_xt, p0, T0 = X["xt"], X["T_xt"], X["p0"], X["T0"]
        if t == 0:
            hTcur[wb] = hTr.next()
        hT, T_hT = hTcur[wb]
        st, T_st = rings["st"].next()
        junk, T_junk = rings["junk"].next()
        P.op("act", lambda e, p0=p0, st=st, junk=junk: e.activation(out=junk[:], in_=p0[:], func=AF.Square, accum_out=st[:, 0:1]), r=[T0], w=[T_junk, T_st])
        P.op("dve", lambda e, st=st: e.tensor_scalar(out=st[:, 1:2], in0=st[:, 0:1], scalar1=1.0 / 1024, scalar2=EPS, op0=ALU.mult, op1=ALU.add), r=[T_st], w=[T_st])
        P.op("pool", lambda e, st=st: e.tensor_tensor(out=st[:, 2:3], in0=st[:, 1:2], in1=K["nh"][:], op=ALU.pow), r=[T_st, K["T_nh"]], w=[T_st])
        x1, T_x1 = x1r.next()
        P.op("dve", lambda e, p0=p0, st=st: e.scalar_tensor_tensor(out=p0[:], in0=p0[:], scalar=st[:, 2:3], in1=gg0[:], op0=ALU.mult, op1=ALU.mult),
             r=[T0, T_st, T_gg0], w=[T0])
        P.op("dve", lambda e, p0=p0, xt=xt, x1=x1: e.tensor_tensor(out=x1[:], in0=p0[:], in1=xt[:], op=ALU.add), r=[T0, T_xt], w=[T_x1])
        if own:
            oi = wt_i - 4
            P.dma(D["x1s"][oi * 128:(oi + 1) * 128, :], x1[:], r=[T_x1], w=[T_x1s[oi]], tok=T_x1)
        hb, T_hb = emit_norm_A(P, K, x1, T_x1, gs1, T_m1, sh1, T_m1, rings)
        ST2[wt_i] = (hb, T_hb, hT, T_hT)

    def SB2(wt_i):
        wb, t = divmod(wt_i, 4)
        rows = slice(wt_i * 128, (wt_i + 1) * 128)
        hb, T_hb, hT, T_hT = ST2.pop(wt_i)
        emit_norm_B(P, K, hb, T_hb, hT[:, :, t * 128:(t + 1) * 128], T_hT)
        ust, T_ust = ustr.next()
        for g in range(4):
            bk, T_bk = P.bank()
            for dc in range(8):
                P.op("pe", lambda e, g=g, dc=dc, bk=bk, t=t, hT=hT: e.matmul(bk[:], lhsT=hT[:, dc, t * 128:(t + 1) * 128], rhs=wu[:, dc, g * 512:(g + 1) * 512],
                                                                            start=(dc == 0), stop=(dc == 7)), r=[T_hT, T_wu], w=[T_bk])
            if g % 2 == 0:
                P.op("act", lambda e, g=g, bk=bk, ust=ust: e.copy(out=ust[:, g * 512:(g + 1) * 512], in_=bk[:]), r=[T_bk], w=[T_ust])
            else:
                P.op("dve", lambda e, g=g, bk=bk, ust=ust: e.tensor_copy(out=ust[:, g * 512:(g + 1) * 512], in_=bk[:]), r=[T_bk], w=[T_ust])
        P.dma(D["u_s"][rows, :], ust[:], r=[T_ust], w=[T_us[wt_i]], tok=T_ust)

    def Z(wb):
        hT, T_hT = hTcur[wb]
        tb = wb - 1
        for fz in range(16):
            bk, T_bk = P.bank()
            for dc in range(8):
                P.op("pe", lambda e, fz=fz, dc=dc, bk=bk, hT=hT: e.matmul(bk[:], lhsT=wz[:, dc, fz * 128:(fz + 1) * 128], rhs=hT[:, dc, :], start=(dc == 0), stop=(dc == 7)),
                     r=[T_wz, T_hT], w=[T_bk])
            sg, T_sg = sgr.next()
            zs, T_zs = zsr.next()
            P.op("act", lambda e, bk=bk, sg=sg: e.activation(out=sg[:], in_=bk[:], func=AF.Sigmoid), r=[T_bk], w=[T_sg])
            P.op("dve", lambda e, bk=bk, sg=sg, zs=zs: e.tensor_tensor(out=zs[:], in0=bk[:], in1=sg[:], op=ALU.mult), r=[T_bk, T_sg], w=[T_zs])
            P.dma(D["szT"][fz * 128:(fz + 1) * 128, tb * 512:(tb + 1) * 512], zs[:], r=[T_zs], w=[T_sz[tb][fz]], tok=T_zs)

    for i in range(-3, NWT):
        if fused and 0 <= i + 3 < NWT:
            GA(i + 3)
        if 0 <= i + 2 < NWT:
            SA(i + 2)
        if 0 <= i + 1 < NWT:
            SB(i + 1)
        if i >= 0:
            SB2(i)
            if i % 4 == 3 and 1 <= i // 4 <= 4:
                Z(i // 4)
    P.pop_scope()
    P.pop_scope()
    vT, T_vT = P.tile("vT", [128, 16, 2048], BF16)
    wo, T_wo = P.tile("wo1", [128, 16, 1024], BF16)
    P.push_scope()
    wov = D["w_out1"].rearrange("(fc p) c -> p fc c", p=128)
    wost = Ring(P, "wo1st", 2, [128, 1024])
    for fc in range(16):
        wt, T_wt = wost.next()
        P.dma(wt[:], wov[:, fc, :], w=[T_wt], tok=T_wt)
        P.op("pool", lambda e, fc=fc, wt=wt: e.tensor_copy(out=wo[:, fc, :], in_=wt[:]), r=[T_wt], w=[T_wo])
    psc, T_psc = P.tile("psc", [128, 16])
    P.dma(psc[:], D["pscale"], w=[T_psc], tok=T_psc)
    ugr = Ring(P, "ug", 1, [128, NWT, 512], BF16)
    dTg, T_dTg = P.tile("dTg", [128, 4, 2048], BF16)
    wpst = Ring(P, "wpst", 1, [128, 4, 512])
    wpr = Ring(P, "wp", 2, [128, 4, 512], BF16)
    bnr = Ring(P, "bn", 3, [128, 9, 128], BF16)
    szr = Ring(P, "szt", 3, [128, 512], BF16)
    usv = D["u_s"].rearrange("(w p) f -> p w f", p=128)
    for g in range(4):
        nd = len(DELTAS[g])
        ug, T_ug = ugr.next()
        P.dma(ug[:], usv[:, :, g * 512:(g + 1) * 512], r=T_us, w=[T_ug], tok=T_ug)
        wps, T_wps = wpst.next()
        wp, T_wp = wpr.next()
        P.dma(wps[:], D["w_pool"][g].rearrange("(fic p) fo -> p fic fo", p=128), w=[T_wps], tok=T_wps)
        P.op("pool", lambda e, wps=wps, wp=wp: e.tensor_copy(out=wp[:], in_=wps[:]), r=[T_wps], w=[T_wp])
        bns = {}

        def ldbn(j):
            bn, T_bn = bnr.next()
            P.dma(bn[:, 0:nd, :], D["BN"][j, BOFF[g]:BOFF[g] + nd].rearrange("k a b -> a k b"), w=[T_bn], tok=T_bn)
            bns[j] = (bn, T_bn)

        ldbn(0)
        for j in range(16):
            if j + 1 < 16:
                ldbn(j + 1)
            bn, T_bn = bns.pop(j)
            bk, T_bk = P.bank()
            for fc in range(4):
                for di, dl in enumerate(DELTAS[g]):
                    P.op("pe", lambda e, fc=fc, di=di, dl=dl, bk=bk, bn=bn, ug=ug, j=j: e.matmul(bk[:, fc * 128:(fc + 1) * 128], lhsT=ug[:, 4 + j + dl, fc * 128:(fc + 1) * 128],
                                                                                              rhs=bn[:, di, :], start=(di == 0), stop=(di == nd - 1)),
                         r=[T_ug, T_bn], w=[T_bk])
            if j % 2 == 0:
                P.op("act", lambda e, bk=bk, j=j: e.copy(out=dTg[:, :, j * 128:(j + 1) * 128], in_=bk[:].rearrange("p (a b) -> p a b", a=4)), r=[T_bk], w=[T_dTg])
            else:
                P.op("dve", lambda e, bk=bk, j=j: e.tensor_copy(out=dTg[:, :, j * 128:(j + 1) * 128], in_=bk[:].rearrange("p (a b) -> p a b", a=4)), r=[T_bk], w=[T_dTg])
        szs = {}

        def ldsz(idx):
            tb, fo = divmod(idx, 4)
            fch = g * 4 + fo
            szt, T_szt = szr.next()
            P.dma(szt[:], D["szT"][fch * 128:(fch + 1) * 128, tb * 512:(tb + 1) * 512], r=[T_sz[tb][fch]], w=[T_szt], tok=T_szt)
            szs[idx] = (szt, T_szt)

        ldsz(0)
        for tb in range(4):
            for fo in range(4):
                fch = g * 4 + fo
                if tb * 4 + fo + 1 < 16:
                    ldsz(tb * 4 + fo + 1)
                szt, T_szt = szs.pop(tb * 4 + fo)
                bk, T_bk = P.bank()
                for fic in range(4):
                    P.op("pe", lambda e, fic=fic, fo=fo, tb=tb, bk=bk, wp=wp: e.matmul(bk[:], lhsT=wp[:, fic, fo * 128:(fo + 1) * 128], rhs=dTg[:, fic, tb * 512:(tb + 1) * 512],
                                                                                      start=(fic == 0), stop=(fic == 3)), r=[T_wp, T_dTg], w=[T_bk])
                P.op("dve", lambda e, bk=bk, fch=fch, tb=tb, szt=szt: e.scalar_tensor_tensor(out=vT[:, fch, tb * 512:(tb + 1) * 512], in0=bk[:], scalar=psc[:, fch:fch + 1], in1=szt[:],
                                                                                            op0=ALU.mult, op1=ALU.mult), r=[T_bk, T_psc, T_szt], w=[T_vT])
    P.pop_scope()
    P.push_scope()
    y2r = Ring(P, "y2", 2, [128, 1024])
    x1lr = Ring(P, "x1l", 2, [128, 1024])
    outs = []
    for j in range(16):
        y2, T_y2 = y2r.next()
        for hf in range(2):
            bk, T_bk = P.bank()
            for fch in range(16):
                P.op("pe", lambda e, fch=fch, hf=hf, bk=bk, j=j: e.matmul(bk[:], lhsT=vT[:, fch, j * 128:(j + 1) * 128], rhs=wo[:, fch, hf * 512:(hf + 1) * 512],
                                                                        start=(fch == 0), stop=(fch == 15)), r=[T_vT, T_wo], w=[T_bk])
            if hf == 0:
                P.op("act", lambda e, bk=bk, y2=y2: e.copy(out=y2[:, 0:512], in_=bk[:]), r=[T_bk], w=[T_y2])
            else:
                P.op("dve", lambda e, bk=bk, y2=y2: e.tensor_copy(out=y2[:, 512:1024], in_=bk[:]), r=[T_bk], w=[T_y2])
        x1l, T_x1l = x1lr.next()
        P.dma(x1l[:], D["x1s"][j * 128:(j + 1) * 128, :], r=[T_x1s[j]], w=[T_x1l], tok=T_x1l)
        st, T_st = rings["st"].next()
        junk, T_junk = rings["junk"].next()
        P.op("act", lambda e, y2=y2, st=st, junk=junk: e.activation(out=junk[:], in_=y2[:], func=AF.Square, accum_out=st[:, 0:1]), r=[T_y2], w=[T_junk, T_st])
        P.op("dve", lambda e, st=st: e.tensor_scalar(out=st[:, 1:2], in0=st[:, 0:1], scalar1=1.0 / 1024, scalar2=EPS, op0=ALU.mult, op1=ALU.add), r=[T_st], w=[T_st])
        P.op("pool", lambda e, st=st: e.tensor_tensor(out=st[:, 2:3], in0=st[:, 1:2], in1=K["nh"][:], op=ALU.pow), r=[T_st, K["T_nh"]], w=[T_st])
        P.op("dve", lambda e, y2=y2, st=st: e.scalar_tensor_tensor(out=y2[:], in0=y2[:], scalar=st[:, 2:3], in1=gg1, op0=ALU.mult, op1=ALU.mult), r=[T_y2, T_st, T_m1], w=[T_y2])
        P.op("pool", lambda e, y2=y2, x1l=x1l: e.tensor_tensor(out=y2[:], in0=y2[:], in1=x1l[:], op=ALU.add), r=[T_y2, T_x1l], w=[T_y2])
        T_o = Tok("out_o")
        outs.append(T_o)
        P.dma(D["out"][j * 128:(j + 1) * 128, :], y2[:], r=[T_y2], w=[T_o], tok=T_y2)
    P.pop_scope()
    return outs


def host_p3_inputs(inp, b, q, parts_b):
    f32 = np.float32
    d = {}
    lo = q * 2048 - 512
    xw = np.zeros((NWT * 128, 1024), f32)
    pw = np.zeros((4, NWT * 128, 1024), f32)
    a = max(lo, 0); e_ = min(lo + NWT * 128, TLAT)
    xw[a - lo:e_ - lo] = inp["x"][b][a:e_]
    pw[:, a - lo:e_ - lo] = parts_b[:, a:e_]
    d["xw"] = xw
    d["parts"] = pw
    d["cvec"] = np.ascontiguousarray(inp["c"][b].reshape(8, 128, 1).transpose(1, 0, 2))
    d["w_ada0"] = np.ascontiguousarray(inp["w_ada"][0]); d["b_ada0"] = np.ascontiguousarray(inp["b_ada"][0])
    d["w_ada1"] = np.ascontiguousarray(inp["w_ada"][1]); d["b_ada1"] = np.ascontiguousarray(inp["b_ada"][1])
    d["g_post0"] = np.ascontiguousarray(inp["a_norm_post"][0]); d["g_pre1"] = np.ascontiguousarray(inp["b_norm_pre"][0])
    d["g_post1"] = np.ascontiguousarray(inp["b_norm_post"][0])
    d["w_in1"] = np.ascontiguousarray(inp["b_w_in"][0]); d["w_pool"] = np.ascontiguousarray(inp["b_w_pool"][0])
    d["pscale"] = np.ascontiguousarray(inp["b_pool_scale"][0].reshape(16, 128).T)
    d["w_out1"] = np.ascontiguousarray(inp["b_w_out"][0])
    d["BN"] = host_bn(q)
    d["ident"] = np.eye(128, dtype=f32)
    return d


def build_p3(debug=False):
    nc = bass.Bass("TRN2", target_bir_lowering=False)
    P = Prog(nc)
    P.init_banks(8)
    D = {}

    def din(name, shape, dt=F32):
        D[name] = nc.dram_tensor(name, list(shape), dt, kind="ExternalInput").ap()

    din("xw", [NWT * 128, 1024]); din("parts", [4, NWT * 128, 1024]); din("cvec", [128, 8, 1])
    din("w_ada0", [1024, 3072]); din("b_ada0", [3072]); din("w_ada1", [1024, 3072]); din("b_ada1", [3072])
    din("g_post0", [1024]); din("g_pre1", [1024]); din("g_post1", [1024])
    din("w_in1", [1024, 4096]); din("w_pool", [4, 512, 512]); din("pscale", [128, 16]); din("w_out1", [2048, 1024])
    din("BN", [16, 19, 128, 128], BF16); din("ident", [128, 128])
    kd = "ExternalOutput" if debug else "Internal"
    D["x1s"] = nc.dram_tensor("x1s", [2048, 1024], F32, kind=kd).ap()
    D["u_s"] = nc.dram_tensor("u_s", [NWT * 128, 2048], BF16, kind=kd).ap()
    D["szT"] = nc.dram_tensor("szT", [2048, 2048], BF16, kind=kd).ap()
    D["out"] = nc.dram_tensor("out", [2048, 1024], F32, kind="ExternalOutput").ap()
    K = emit_consts(P, D)
    outs = emit_p3(P, K, D)
    P.op("sp", None, r=outs)
    P.finalize()
    return nc, P


I32 = mybir.dt.int32
RG4 = [[0, 1, 2, 3], [4, 5, 6, 7]]


def emit_outstage_f(P, K, D, h_toks, TK):
    NR = 6
    hfr = Ring(P, "hf", NR, [128, 512])
    hbr = Ring(P, "hb", NR, [128, 512])
    Ar = Ring(P, "Ain", NR, [128, 512])
    Br = Ring(P, "Bin", NR, [128, 512])
    junk, T_junk = P.tile("ojunk", [128, 512], BF16)
    str_ = Ring(P, "ost", 5, [128, 8])
    ybr = Ring(P, "yb", 3, [128, 512], BF16)
    T_yc = [Tok("yc%d" % k) for k in range(8)]
    T_Yg = Tok("Yg")
    T_cc = Tok("ccY")
    L = {}

    def loads(cl):
        hf, T_hf = hfr.next(); hb, T_hb = hbr.next(); At, T_At = Ar.next(); Bt, T_Bt = Br.next()
        rows = slice(cl * 128, (cl + 1) * 128)
        P.dma(hf[:], D["hdir0"][rows, :], r=[h_toks[0][cl]], w=[T_hf], tok=T_hf)
        P.dma(hb[:], D["hdir1"][rows, :], r=[h_toks[1][cl]], w=[T_hb], tok=T_hb)
        P.dma(At[:], D["A"][rows, :], r=[TK["A"]], w=[T_At], tok=T_At)
        P.dma(Bt[:], D["Bc"][rows, :], r=[TK["Bc"]], w=[T_Bt], tok=T_Bt)
        L[cl] = (hf, T_hf, hb, T_hb, At, T_At, Bt, T_Bt)

    def st1(cl):
        hf, T_hf, hb, T_hb, At, T_At, Bt, T_Bt = L[cl]
        st, T_st = str_.next()
        L[cl] = L[cl] + (st, T_st)
        P.op("dve", lambda e, hf=hf, hb=hb: e.tensor_tensor(out=hf[:], in0=hf[:], in1=hb[:], op=ALU.add), r=[T_hf, T_hb], w=[T_hf])
        P.op("act", lambda e, hf=hf, st=st: e.activation(out=junk[:], in_=hf[:], func=AF.Copy, accum_out=st[:, 0:1]), r=[T_hf], w=[T_junk, T_st])
        P.op("act", lambda e, hf=hf, st=st: e.activation(out=junk[:], in_=hf[:], func=AF.Square, accum_out=st[:, 1:2]), r=[T_hf], w=[T_junk, T_st])

    def st2(cl):
        st, T_st = L[cl][8], L[cl][9]
        P.op("dve", lambda e, st=st: e.tensor_scalar(out=st[:, 2:3], in0=st[:, 0:1], scalar1=1.0 / 512, scalar2=None, op0=ALU.mult), r=[T_st], w=[T_st])
        P.op("dve", lambda e, st=st: e.tensor_tensor(out=st[:, 3:4], in0=st[:, 2:3], in1=st[:, 2:3], op=ALU.mult), r=[T_st], w=[T_st])
        P.op("dve", lambda e, st=st: e.scalar_tensor_tensor(out=st[:, 4:5], in0=st[:, 1:2], scalar=1.0 / 512, in1=st[:, 3:4], op0=ALU.mult, op1=ALU.subtract),
             r=[T_st], w=[T_st])
        P.op("dve", lambda e, st=st: e.tensor_scalar(out=st[:, 4:5], in0=st[:, 4:5], scalar1=EPS, scalar2=None, op0=ALU.add), r=[T_st], w=[T_st])
        P.op("pool", lambda e, st=st: e.tensor_tensor(out=st[:, 5:6], in0=st[:, 4:5], in1=K["nh"][:], op=ALU.pow), r=[T_st, K["T_nh"]], w=[T_st])

    def st3(cl):
        hf, T_hf, hb, T_hb, At, T_At, Bt, T_Bt, st, T_st = L.pop(cl)
        P.op("dve", lambda e, hf=hf, st=st: e.tensor_scalar(out=hf[:], in0=hf[:], scalar1=st[:, 2:3], scalar2=st[:, 5:6], op0=ALU.subtract, op1=ALU.mult),
             r=[T_hf, T_st], w=[T_hf])
        P.op("dve", lambda e, hf=hf, At=At: e.tensor_tensor(out=hf[:], in0=hf[:], in1=At[:], op=ALU.mult), r=[T_hf, T_At], w=[T_hf])
        yb, T_yb = ybr.next()
        P.op("dve", lambda e, hf=hf, Bt=Bt, yb=yb: e.tensor_tensor(out=yb[:], in0=hf[:], in1=Bt[:], op=ALU.add), r=[T_hf, T_Bt], w=[T_yb])
        k = cl // 8
        P.dma(D["ycin%d" % k][(cl % 8) * 128:(cl % 8 + 1) * 128, :], yb[:], r=[T_yb], w=[T_yc[k]], tok=T_yb)
        if cl % 8 == 7:
            P.op("pool", lambda e, k=k: e.collective_compute("AllGather", ALU.bypass, replica_groups=RG4, ins=[D["ycin%d" % k]], outs=[D["Yg"][k]]),
                 r=[T_yc[k]], w=[T_Yg], dma=T_cc, inc=1)

    for cl in range(-4, 64):
        if 0 <= cl + 4 < 64:
            loads(cl + 4)
        if 0 <= cl + 2 < 64:
            st1(cl + 2)
        if 0 <= cl + 1 < 64:
            st2(cl + 1)
        if cl >= 0:
            st3(cl)
    return T_Yg


def host_fused_inputs(inp, b, h):
    d = host_p1_inputs(inp, b, h)
    d["cvec2"] = d.pop("cvec")
    d["w_ada0"] = d.pop("w_ada")
    d["b_ada0"] = d.pop("b_ada")
    d.update(host_p2_consts(inp, h))
    q = h
    lo = q * 2048 - 512
    xw = np.zeros((NWT * 128, 1024), np.float32)
    a = max(lo, 0); e_ = min(lo + NWT * 128, TLAT)
    xw[a - lo:e_ - lo] = inp["x"][b][a:e_]
    d["xw"] = xw
    d["cvec1"] = np.ascontiguousarray(inp["c"][b].reshape(8, 128, 1).transpose(1, 0, 2))
    d["w_ada1"] = np.ascontiguousarray(inp["w_ada"][1]); d["b_ada1"] = np.ascontiguousarray(inp["b_ada"][1])
    d["g_post0"] = np.ascontiguousarray(inp["a_norm_post"][0]); d["g_pre1"] = np.ascontiguousarray(inp["b_norm_pre"][0])
    d["g_post1"] = np.ascontiguousarray(inp["b_norm_post"][0])
    d["w_in1"] = np.ascontiguousarray(inp["b_w_in"][0]); d["w_pool"] = np.ascontiguousarray(inp["b_w_pool"][0])
    d["pscale"] = np.ascontiguousarray(inp["b_pool_scale"][0].reshape(16, 128).T)
    d["w_out1"] = np.ascontiguousarray(inp["b_w_out"][0])
    d["w_out0"] = np.ascontiguousarray(inp["a_w_out"][0])
    d["BN"] = host_bn(q)
    gidx = np.zeros((128, 16), np.int32)
    cols = (h, 4 + h, 8 + h, 12 + h)
    p = np.arange(NCH)
    for r in range(4):
        for s in range(4):
            gidx[:NCH, r * 4 + s] = (s * 16 + cols[r]) * NCH + p
    d["gidx"] = gidx
    yidx = np.zeros((128, NWT * 4), np.int32)
    pp = np.arange(128)
    for w in range(NWT):
        t = np.clip(lo + 128 * w + pp, 0, TLAT - 1)
        for r in range(4):
            yidx[:, w * 4 + r] = (t // 1024) * 4096 + r * 1024 + (t % 1024)
    d["yidx"] = yidx
    return d


def build_fused():
    nc = bass.Bass("TRN2", target_bir_lowering=False)
    P = Prog(nc)
    P.init_banks(8)
    D = {}

    def din(name, shape, dt=F32):
        D[name] = nc.dram_tensor(name, list(shape), dt, kind="ExternalInput").ap()

    def dint(name, shape, dt=F32):
        D[name] = nc.dram_tensor(name, list(shape), dt, kind="Internal").ap()

    din("xin", [TT, 1024]); din("cvec2", [128, 8, 2]); din("w_ada0", [1024, 3072]); din("b_ada0", [3072]); din("g_pre", [1024])
    din("w_in", [1024, 1536]); din("convw", [128, 4, 3]); din("convb", [128, 4]); din("skip", [128, 4]); din("hn", [512])
    din("BD", [128, 3, 4, 128]); din("BDT", [128, 3, 4, 128]); din("wg", [128, 3, 4, 16]); din("ident", [128, 128])
    din("gbias", [128, 4]); din("perm", [NCH, NCH]); din("triu", [NCH, NCH]); din("mask", [128, 2, 128])
    din("gidx", [128, 16], I32); din("yidx", [128, NWT * 4], I32)
    din("xw", [NWT * 128, 1024]); din("cvec1", [128, 8, 1]); din("w_ada1", [1024, 3072]); din("b_ada1", [3072])
    din("g_post0", [1024]); din("g_pre1", [1024]); din("g_post1", [1024])
    din("w_in1", [1024, 4096]); din("w_pool", [4, 512, 512]); din("pscale", [128, 16]); din("w_out1", [2048, 1024]); din("w_out0", [2048, 1024])
    din("BN", [16, 19, 128, 128], BF16)
    dint("qT", [512, TT], BF16); dint("kT", [512, TT], BF16); dint("ktok", [TT, 512], BF16); dint("vtok", [TT, 512], BF16)
    dint("A", [TLAT, 512]); dint("Bc", [TLAT, 512]); dint("gpT", [16, TT]); dint("GP", [64, TT])
    dint("hdir0", [TLAT, 512]); dint("hdir1", [TLAT, 512])
    for k in range(8):
        dint("ycin%d" % k, [1024, 512], BF16)
    dint("Yg", [8, 4096, 512], BF16)
    dint("x1s", [2048, 1024]); dint("u_s", [NWT * 128, 2048], BF16); dint("szT", [2048, 2048], BF16)
    D["out"] = nc.dram_tensor("out", [2048, 1024], F32, kind="ExternalOutput").ap()

    K = emit_consts(P, D)
    p2_consts(P, K, D)
    for name, shape in (("gidx", [128, 16]), ("yidx", [128, NWT * 4])):
        K[name], K["T_" + name] = P.tile("k_" + name, shape, I32)
        P.dma(K[name][:], D[name], w=[K["T_" + name]], tok=K["T_" + name])
    TK = {k: Tok("o_" + k) for k in ("qT", "kT", "ktok", "vtok", "A", "Bc", "gpT")}
    P.push_scope()
    D1 = dict(D, cvec=D["cvec2"], w_ada=D["w_ada0"], b_ada=D["b_ada0"])
    emit_p1(P, K, D1, TK)
    P.pop_scope()
    T_GP = Tok("GP")
    T_ccg = Tok("ccG")
    P.op("pool", lambda e: e.collective_compute("AllGather", ALU.bypass, replica_groups=RG4, ins=[D["gpT"]], outs=[D["GP"]]),
         r=[TK["gpT"]], w=[T_GP], dma=T_ccg, inc=1)
    D3 = dict(D, cvec=D["cvec1"])
    ada = emit_p3_ada(P, K, D3)
    P.push_scope()
    G = [emit_gates(P, K, D, d, T_GP) for d in range(2)]
    T_Yg = emit_scan(P, K, D, G, TK, fuse_out=True)
    P.pop_scope()
    outs = emit_p3(P, K, D3, fused=True, T_Yg=T_Yg, ada=ada)
    P.op("sp", None, r=outs)
    P.finalize()
    return nc, P


def kernel(**inputs):
    inp = {k: np.ascontiguousarray(np.asarray(v)) for k, v in inputs.items()}
    cores = [(b, h) for b in range(2) for h in range(NHEAD)]
    nc, _ = build_fused()
    maps = [host_fused_inputs(inp, b, h) for b, h in cores]
    res = run_bass_kernel_spmd(nc, maps, core_ids=list(range(8))).results
    out = np.zeros((2, TLAT, 1024), np.float32)
    for ci, (b, q) in enumerate(cores):
        out[b, q * 2048:(q + 1) * 2048] = res[ci]["out"]
    return out
```

```python
import contextlib
import numpy as np
import ml_dtypes
import concourse.bass as bass
import concourse.mybir as mybir
from concourse.bass_utils import run_bass_kernel_spmd

F32 = mybir.dt.float32
BF16 = mybir.dt.bfloat16
ALU = mybir.AluOpType
AF = mybir.ActivationFunctionType
AX = mybir.AxisListType

SAME_SYNC = True
EPS = 1e-6
NHEAD = 4
DH = 512
TCTX = 256
TLAT = 8192
TT = TCTX + TLAT
KSCALE = float(DH ** -0.5)


class SemSlot:
    __slots__ = ("cnt", "handle", "idx")

    def __init__(self, idx):
        self.cnt = 0
        self.handle = None
        self.idx = idx


class Tok:
    __slots__ = ("name", "lw", "rd", "slot")

    def __init__(self, name):
        self.name = name
        self.lw = {}
        self.rd = {}
        self.slot = None


class Prog:
    ENG = ("pe", "act", "dve", "pool", "sp")

    def __init__(self, nc):
        self.nc = nc
        self.stack = contextlib.ExitStack()
        self.eng_ops = {e: [] for e in self.ENG}
        self.nsem = 0
        self.banks = None
        self.bank_i = 0
        self.scopes = []
        self.fence = {}
        self.free_slots = []
        self.all_slots = []

    def sb(self, name, shape, dt=F32):
        self.uid = getattr(self, "uid", 0) + 1
        st = self.scopes[-1][0] if self.scopes else self.stack
        return st.enter_context(self.nc.sbuf_tensor("sb%d_%s" % (self.uid, name), list(shape), dt))

    def push_scope(self):
        self.scopes.append((contextlib.ExitStack(), []))

    def pop_scope(self):
        st, toks = self.scopes.pop()
        for t in toks:
            for dct in (t.lw, t.rd):
                for k, v in dct.items():
                    if self.fence.get(k, -1) < v:
                        self.fence[k] = v
            if t.slot is not None:
                self.free_slots.append(t.slot)
                t.slot = None
        st.close()

    def ps(self, name, shape, dt=F32):
        return self.stack.enter_context(self.nc.psum_tensor("ps_" + name, list(shape), dt))

    def sem(self, name):
        self.nsem += 1
        return self.stack.enter_context(self.nc.semaphore("%s_%d" % (name, self.nsem)))

    def tile(self, name, shape, dt=F32):
        tk = Tok(name)
        tk.lw = dict(self.fence)
        if self.scopes:
            self.scopes[-1][1].append(tk)
        return self.sb(name, shape, dt), tk

    def init_banks(self, n=8):
        self.banks = [(self.ps("bank%d" % i, [128, 512], F32), Tok("bank%d" % i)) for i in range(n)]

    def bank(self):
        b = self.banks[self.bank_i % len(self.banks)]
        self.bank_i += 1
        return b

    def op(self, eng, fn, r=(), w=(), dma=None, inc=16):
        assert fn is not None or not w, "wait-only op cannot produce"
        lst = self.eng_ops[eng]
        i = len(lst)
        if dma is not None:
            if dma.slot is None:
                if self.free_slots:
                    dma.slot = self.free_slots.pop()
                else:
                    dma.slot = SemSlot(len(self.all_slots))
                    self.all_slots.append(dma.slot)
            dma.slot.cnt += inc
            ev = (("d", dma.slot), dma.slot.cnt)
        else:
            ev = (("e", eng), i)
        deps = {}

        def add(d):
            for k, v in d.items():
                if deps.get(k, -1) < v:
                    deps[k] = v

        for b in r:
            add(b.lw)
        for b in w:
            add(b.lw)
            add(b.rd)
        for b in r:
            if b in w:
                continue
            if b.rd.get(ev[0], -1) < ev[1]:
                b.rd[ev[0]] = ev[1]
        for b in w:
            if b.rd:
                b.lw = {}
                b.rd = {}
            b.lw[ev[0]] = ev[1]
        rec = dict(eng=eng, fn=fn, deps=deps, ev=ev, sig=False, dma=dma, waits=[], inc=inc, dslot=(dma.slot if dma is not None else None))
        lst.append(rec)
        return rec

    def dma(self, out, in_, r=(), w=(), tok=None, q="sp"):
        self.op(q, lambda e: e.dma_start(out=out, in_=in_), r=r, w=w, dma=tok)

    def finalize(self):
        nc = self.nc
        for eng in self.ENG:
            waited = {}
            for rec in self.eng_ops[eng]:
                for k, v in rec["deps"].items():
                    if k[0] == "e" and k[1] == eng:
                        if eng in ("pe", "sp") or not SAME_SYNC:
                            continue
                    if waited.get(k, -1) >= v:
                        continue
                    waited[k] = v
                    rec["waits"].append((k, v))
                    if k[0] == "e":
                        self.eng_ops[k[1]][v]["sig"] = True
        self.esem = {}
        self.rank = {}
        for eng in self.ENG:
            if eng == "sp":
                continue
            self.esem[eng] = self.sem("s_" + eng)
            c = 0
            rk = []
            for rec in self.eng_ops[eng]:
                if rec["sig"] and rec["dma"] is None:
                    c += 1
                rk.append(c)
            self.rank[eng] = rk
        for sl in self.all_slots:
            sl.handle = self.sem("dq%d" % sl.idx)
        with nc.Block() as block:
            def emit(name, e):
                for rec in self.eng_ops[name]:
                    for k, v in rec["waits"]:
                        if k[0] == "e":
                            e.wait_ge(self.esem[k[1]], self.rank[k[1]][v])
                        else:
                            e.wait_ge(k[1].handle, v)
                    if rec["fn"] is None:
                        continue
                    ins = rec["fn"](e)
                    if rec["dma"] is not None:
                        ins.then_inc(rec["dslot"].handle, rec["inc"])
                    elif rec["sig"]:
                        ins.then_inc(self.esem[name], 1)

            @block.tensor
            def _(e):
                emit("pe", e)

            @block.scalar
            def _(e):
                emit("act", e)

            @block.vector
            def _(e):
                emit("dve", e)

            @block.gpsimd
            def _(e):
                emit("pool", e)

            @block.sync
            def _(e):
                emit("sp", e)
        self.stack.close()

    def stats(self):
        return {e: len(v) for e, v in self.eng_ops.items()}


class Ring:
    def __init__(self, P, name, n, shape, dt=F32):
        self.items = [P.tile("%s%d" % (name, i), shape, dt) for i in range(n)]
        self.i = 0

    def next(self):
        it = self.items[self.i % len(self.items)]
        self.i += 1
        return it


def emit_consts(P, D):
    K = {}
    K["idf"], K["T_idf"] = P.tile("idf", [128, 128])
    K["idb"], K["T_idb"] = P.tile("idb", [128, 128], BF16)
    K["ones"], K["T_ones"] = P.tile("ones", [128, 128])
    K["nh"], K["T_nh"] = P.tile("nh", [128, 1])
    P.dma(K["idf"][:], D["ident"], w=[K["T_idf"]], tok=K["T_idf"])
    P.op("dve", lambda e: e.tensor_copy(out=K["idb"][:], in_=K["idf"][:]), r=[K["T_idf"]], w=[K["T_idb"]])
    P.op("pool", lambda e: e.memset(K["ones"][:], 1.0), w=[K["T_ones"]])
    P.op("pool", lambda e: e.memset(K["nh"][:], -0.5), w=[K["T_nh"]])
    return K


def emit_ada(P, K, cvec_d, ncv, w_ada_d, b_ada_d, col_lo, ncols, mods, wslots, screp):
    cv, T_cv = P.tile("ada_c", [128, 8, ncv])
    sg, T_sg = P.tile("ada_sg", [128, 8, ncv])
    P.dma(cv[:], cvec_d, w=[T_cv], tok=T_cv)
    P.op("act", lambda e: e.activation(out=sg[:], in_=cv[:], func=AF.Sigmoid), r=[T_cv], w=[T_sg])
    P.op("dve", lambda e: e.tensor_tensor(out=sg[:], in0=sg[:], in1=cv[:], op=ALU.mult), r=[T_sg, T_cv], w=[T_sg])
    rep, T_rep = screp
    repv = rep.rearrange("p (v d m) -> p v d m", v=ncv, d=8)
    for v in range(ncv):
        for dc in range(8):
            P.op("dve", lambda e, v=v, dc=dc: e.tensor_scalar(out=repv[:, v, dc, :], in0=K["ones"][:], scalar1=sg[:, dc, v:v + 1],
                                                             scalar2=None, op0=ALU.mult), r=[K["T_ones"], T_sg], w=[T_rep])
    bb = Ring(P, "ada_b", 2, [128, 256])
    wview = w_ada_d.rearrange("(dc p) c -> p dc c", p=128)
    for cb in range(ncols // 256):
        c0 = col_lo + cb * 256
        wt, T_wt = wslots[cb % len(wslots)]
        P.dma(wt, wview[:, :, c0:c0 + 256], w=[T_wt], tok=T_wt)
        bt, T_bt = bb.next()
        P.dma(bt[:], b_ada_d[c0:c0 + 256].partition_broadcast(128), w=[T_bt], tok=T_bt)
        for v in range(ncv):
            bk, T_bk = P.bank()
            for dc in range(8):
                P.op("pe", lambda e, v=v, dc=dc, bk=bk, wt=wt: e.matmul(bk[:, 0:256], lhsT=repv[:, v, dc, :], rhs=wt[:, dc, :],
                                                                       start=(dc == 0), stop=(dc == 7)), r=[T_rep, T_wt], w=[T_bk])
            mt, T_mt = mods[v]
            P.op("dve", lambda e, bk=bk, mt=mt, bt=bt, cb=cb: e.tensor_tensor(out=mt[:, cb * 256:(cb + 1) * 256], in0=bk[:, 0:256], in1=bt[:],
                                                                             op=ALU.add), r=[T_bk, T_bt], w=[T_mt])


def emit_norm_A(P, K, xt, T_xt, gs, T_gs, sh, T_sh, rings):
    st, T_st = rings["st"].next()
    junk, T_junk = rings["junk"].next()
    tmp, T_tmp = rings["tmp"].next()
    hb, T_hb = rings["hb"].next()
    P.op("act", lambda e: e.activation(out=junk[:], in_=xt[:], func=AF.Square, accum_out=st[:, 0:1]), r=[T_xt], w=[T_junk, T_st])
    P.op("dve", lambda e: e.tensor_scalar(out=st[:, 1:2], in0=st[:, 0:1], scalar1=1.0 / 1024, scalar2=EPS, op0=ALU.mult, op1=ALU.add),
         r=[T_st], w=[T_st])
    P.op("pool", lambda e: e.tensor_tensor(out=st[:, 2:3], in0=st[:, 1:2], in1=K["nh"][:], op=ALU.pow), r=[T_st, K["T_nh"]], w=[T_st])
    P.op("dve", lambda e: e.scalar_tensor_tensor(out=tmp[:], in0=xt[:], scalar=st[:, 2:3], in1=gs, op0=ALU.mult, op1=ALU.mult),
         r=[T_xt, T_st, T_gs], w=[T_tmp])
    P.op("pool", lambda e: e.tensor_tensor(out=hb[:], in0=tmp[:], in1=sh, op=ALU.add), r=[T_tmp, T_sh], w=[T_hb])
    return hb, T_hb


def emit_norm_B(P, K, hb, T_hb, hT_dst, T_hT):
    bk, T_bk = P.bank()
    bkb = bk[:].bitcast(BF16).rearrange("p (a b) -> p a b", a=8)
    for dc in range(8):
        P.op("pe", lambda e, dc=dc: e.transpose(out=bkb[:, dc, :], in_=hb[:, dc * 128:(dc + 1) * 128], identity=K["idb"][:]),
             r=[T_hb, K["T_idb"]], w=[T_bk])
    P.op("act", lambda e: e.copy(out=hT_dst, in_=bkb), r=[T_bk], w=[T_hT])


def emit_norm_T(P, K, xt, T_xt, gs, T_gs, sh, T_sh, rings, hT_dst, T_hT):
    hb, T_hb = emit_norm_A(P, K, xt, T_xt, gs, T_gs, sh, T_sh, rings)
    emit_norm_B(P, K, hb, T_hb, hT_dst, T_hT)


def emit_p1(P, K, D, TK=None):
    blocks = [(0, 2, 1)] + [(TCTX + 512 * i, 4, 0) for i in range(16)]
    rings = dict(st=Ring(P, "n_st", 4, [128, 4]), junk=Ring(P, "n_junk", 1, [128, 1024], BF16),
                 tmp=Ring(P, "n_tmp", 2, [128, 1024]), hb=Ring(P, "n_hb", 3, [128, 1024], BF16))
    xr = Ring(P, "xt", 3, [128, 1024])
    hTr = Ring(P, "hT", 2, [128, 8, 512], BF16)
    xmTr = Ring(P, "xmT", 2, [128, 4, 514])
    lastc = Ring(P, "lastc", 3, [128, 4, 1])
    cv, T_cv = P.tile("cv", [128, 4, 512])
    sgt, T_sgt = P.tile("sgt", [128, 4, 512])
    xcT, T_xcT = P.tile("xcT", [128, 4, 512], BF16)
    xmTb, T_xmTb = P.tile("xmTb", [128, 4, 512], BF16)
    SZr = Ring(P, "SZ", 2, [128, 4, 512])
    tzr = Ring(P, "tz", 2, [128, 512])
    tor = Ring(P, "to", 2, [128, 512])
    Ar = Ring(P, "Ast", 2, [128, 512])
    st4 = Ring(P, "st4", 3, [128, 4, 512], BF16)
    gstr = Ring(P, "gst", 2, [16, 512])
    ktr = Ring(P, "kts", 2, [128, 512], BF16)
    vtr = Ring(P, "vts", 2, [128, 512], BF16)
    bcr = Ring(P, "bcs", 2, [128, 512])

    mods = [P.tile("mod%d" % v, [128, 2048]) for v in range(2)]
    wslots = [(cv[:].rearrange("p a b -> p (a b)").rearrange("p (d c) -> p d c", d=8), T_cv),
              (sgt[:].rearrange("p a b -> p (a b)").rearrange("p (d c) -> p d c", d=8), T_sgt)]
    sz0, T_sz0 = SZr.items[0]
    screp = (sz0[:].rearrange("p a b -> p (a b)"), T_sz0)
    emit_ada(P, K, D["cvec"], 2, D["w_ada"], D["b_ada"], 0, 2048, mods, wslots, screp)
    gp, T_gp = rings["tmp"].items[0]
    P.dma(gp[:], D["g_pre"].partition_broadcast(128), w=[T_gp], tok=T_gp)
    for v in range(2):
        mt, T_mt = mods[v]
        P.op("dve", lambda e, mt=mt: e.scalar_tensor_tensor(out=mt[:, 1024:2048], in0=mt[:, 1024:2048], scalar=1.0, in1=gp[:],
                                                           op0=ALU.add, op1=ALU.mult), r=[T_mt, T_gp], w=[T_mt])

    wb = [P.tile("wb%d" % i, [128, 8, 512], BF16) for i in range(3)]
    wview = D["w_in"].rearrange("(dc p) c -> p dc c", p=128)
    for i in range(3):
        for hf in range(2):
            wt, T_wt = wslots[(2 * i + hf) % 2]
            c0 = i * 512 + hf * 256
            P.dma(wt, wview[:, :, c0:c0 + 256], w=[T_wt], tok=T_wt)
            eng = "act" if hf == 0 else "pool"
            if eng == "act":
                P.op("act", lambda e, i=i, hf=hf, wt=wt: e.copy(out=wb[i][0][:, :, hf * 256:(hf + 1) * 256], in_=wt), r=[T_wt], w=[wb[i][1]])
            else:
                P.op("pool", lambda e, i=i, hf=hf, wt=wt: e.tensor_copy(out=wb[i][0][:, :, hf * 256:(hf + 1) * 256], in_=wt), r=[T_wt], w=[wb[i][1]])
    cw, T_cw = P.tile("cw", [128, 4, 3])
    cb, T_cb = P.tile("cb", [128, 4])
    skp, T_skp = P.tile("skp", [128, 4])
    hnb, T_hnb = P.tile("hnb", [128, 512])
    P.dma(cw[:], D["convw"], w=[T_cw], tok=T_cw)
    P.dma(cb[:], D["convb"], w=[T_cb], tok=T_cb)
    P.dma(skp[:], D["skip"], w=[T_skp], tok=T_skp)
    P.dma(hnb[:], D["hn"].partition_broadcast(128), w=[T_hnb], tok=T_hnb)
    BD, T_BD = P.tile("BD", [128, 3, 4, 128])
    BDT, T_BDT = P.tile("BDT", [128, 3, 4, 128])
    wg, T_wg = P.tile("wg", [128, 3, 4, 16])
    P.dma(BD[:], D["BD"], w=[T_BD], tok=T_BD)
    P.dma(BDT[:], D["BDT"], w=[T_BDT], tok=T_BDT)
    P.dma(wg[:], D["wg"], w=[T_wg], tok=T_wg)
    BDb, T_BDb = P.tile("BDb", [128, 3, 4, 128], BF16)
    KS, T_KS = P.tile("KS", [128, 4, 256], BF16)
    Wgc, T_Wgc = P.tile("Wgc", [128, 4, 16], BF16)
    Wgv, T_Wgv = P.tile("Wgv", [128, 4, 16], BF16)
    P.op("dve", lambda e: e.tensor_copy(out=BDb[:], in_=BD[:]), r=[T_BD], w=[T_BDb])
    P.op("act", lambda e: e.mul(out=KS[:, :, 0:128], in_=BD[:, 1, :, :], mul=KSCALE), r=[T_BD], w=[T_KS])
    for fc in range(4):
        P.op("dve", lambda e, fc=fc: e.tensor_scalar(out=KS[:, fc, 128:256], in0=K["idf"][:], scalar1=skp[:, fc:fc + 1], scalar2=None,
                                                     op0=ALU.mult), r=[K["T_idf"], T_skp], w=[T_KS])
    P.op("act", lambda e: e.mul(out=wg[:, 1, :, :], in_=wg[:, 1, :, :], mul=KSCALE), r=[T_wg], w=[T_wg])
    bk, T_bk = P.bank()
    bk2, T_bk2 = P.bank()
    for fc in range(4):
        P.op("pe", lambda e, fc=fc: e.matmul(bk[:, fc * 16:(fc + 1) * 16], lhsT=BDT[:, 0, fc, :], rhs=wg[:, 0, fc, :], start=True, stop=False),
             r=[T_BDT, T_wg], w=[T_bk])
        P.op("pe", lambda e, fc=fc: e.matmul(bk[:, fc * 16:(fc + 1) * 16], lhsT=BDT[:, 1, fc, :], rhs=wg[:, 1, fc, :], start=False, stop=True),
             r=[T_BDT, T_wg], w=[T_bk])
        P.op("pe", lambda e, fc=fc: e.matmul(bk2[:, fc * 16:(fc + 1) * 16], lhsT=BDT[:, 2, fc, :], rhs=wg[:, 2, fc, :], start=True, stop=True),
             r=[T_BDT, T_wg], w=[T_bk2])
    P.op("dve", lambda e: e.tensor_copy(out=Wgc[:].rearrange("p a b -> p (a b)"), in_=bk[:, 0:64]), r=[T_bk], w=[T_Wgc])
    P.op("dve", lambda e: e.tensor_copy(out=Wgv[:].rearrange("p a b -> p (a b)"), in_=bk2[:, 0:64]), r=[T_bk2], w=[T_Wgv])

    out_toks = []
    if TK is None:
        TK = {k: Tok("o_" + k) for k in ("qT", "kT", "ktok", "vtok", "A", "Bc", "gpT")}
    out_toks = list(TK.values())
    xin = D["xin"]
    qTv = D["qT"].rearrange("(fc p) t -> p fc t", p=128)
    kTv = D["kT"].rearrange("(fc p) t -> p fc t", p=128)

    T_cvf = [Tok("cvf%d" % fc) for fc in range(4)]

    hTs = {}

    def S1a(j):
        t0, nt, mi = blocks[j]
        ntok = nt * 128
        hT, T_hT = hTr.next()
        hTs[j] = (hT, T_hT)
        mt, T_mt = mods[mi]
        xts = {}

        def ldx(t):
            xt, T_xt = xr.next()
            P.dma(xt[:], xin[t0 + t * 128:t0 + (t + 1) * 128, :], w=[T_xt], tok=T_xt)
            xts[t] = (xt, T_xt)

        ldx(0)
        if nt > 1:
            ldx(1)
        pend = None
        for t in range(nt):
            if t + 2 < nt:
                ldx(t + 2)
            xt, T_xt = xts.pop(t)
            cur = emit_norm_A(P, K, xt, T_xt, mt[:, 1024:2048], T_mt, mt[:, 0:1024], T_mt, rings)
            if pend is not None:
                emit_norm_B(P, K, pend[0], pend[1], hT[:, :, (t - 1) * 128:t * 128], T_hT)
                yield
            pend = cur
        emit_norm_B(P, K, pend[0], pend[1], hT[:, :, (nt - 1) * 128:nt * 128], T_hT)
        yield

    def S1b(j):
        t0, nt, mi = blocks[j]
        ntok = nt * 128
        hT, T_hT = hTs.pop(j)
        xmT, T_xmT = xmTr.items[j % 2]
        for fc in range(4):
            bk, T_bk = P.bank()
            for dc in range(8):
                P.op("pe", lambda e, fc=fc, dc=dc, bk=bk: e.matmul(bk[:, 0:ntok], lhsT=wb[0][0][:, dc, fc * 128:(fc + 1) * 128],
                                                                   rhs=hT[:, dc, 0:ntok], start=(dc == 0), stop=(dc == 7)),
                     r=[wb[0][1], T_hT], w=[T_bk])
            P.op("act", lambda e, fc=fc, bk=bk: e.copy(out=xmT[:, fc, 1:1 + ntok], in_=bk[:, 0:ntok]), r=[T_bk], w=[T_xmT])
            if fc % 2 == 1:
                yield
        lc, T_lc = lastc.items[j % 3]
        P.op("pool", lambda e: e.tensor_copy(out=lc[:], in_=xmT[:, :, ntok:ntok + 1]), r=[T_xmT], w=[T_lc])
        if mi == 0:
            SZ, T_SZ = SZr.items[j % 2]
            tl = t0 - TCTX
            for t in range(nt):
                bz, T_bz = P.bank()
                bo, T_bo = P.bank()
                for dc in range(8):
                    P.op("pe", lambda e, t=t, dc=dc, bz=bz: e.matmul(bz[:], lhsT=hT[:, dc, t * 128:(t + 1) * 128], rhs=wb[1][0][:, dc, :],
                                                                     start=(dc == 0), stop=(dc == 7)), r=[wb[1][1], T_hT], w=[T_bz])
                for dc in range(8):
                    P.op("pe", lambda e, t=t, dc=dc, bo=bo: e.matmul(bo[:], lhsT=hT[:, dc, t * 128:(t + 1) * 128], rhs=wb[2][0][:, dc, :],
                                                                     start=(dc == 0), stop=(dc == 7)), r=[wb[2][1], T_hT], w=[T_bo])
                tz, T_tz = tzr.next()
                to, T_to = tor.next()
                At, T_At = Ar.next()
                P.op("act", lambda e, bz=bz, tz=tz: e.activation(out=tz[:], in_=bz[:], func=AF.Sigmoid), r=[T_bz], w=[T_tz])
                P.op("dve", lambda e, bz=bz, tz=tz, t=t: e.tensor_tensor(out=SZ[:, t, :], in0=bz[:], in1=tz[:], op=ALU.mult), r=[T_bz, T_tz], w=[T_SZ])
                P.op("act", lambda e, bo=bo, to=to: e.activation(out=to[:], in_=bo[:], func=AF.Sigmoid), r=[T_bo], w=[T_to])
                P.op("pool", lambda e, to=to: e.tensor_tensor(out=to[:], in0=to[:], in1=hnb[:], op=ALU.mult), r=[T_to, T_hnb], w=[T_to])
                P.op("pool", lambda e, to=to, At=At, t=t: e.tensor_tensor(out=At[:], in0=to[:], in1=SZ[:, t, :], op=ALU.mult), r=[T_to, T_SZ], w=[T_At])
                P.dma(D["A"][tl + t * 128:tl + (t + 1) * 128, :], At[:], r=[T_At], w=[TK["A"]], tok=T_At, q="pool")
                yield

    def S2(j, prev, nxt):
        t0, nt, mi = blocks[j]
        ntok = nt * 128
        xmT, T_xmT = xmTr.items[j % 2]
        if prev is None:
            P.op("pool", lambda e: e.memset(xmT[:, :, 0:1], 0.0), w=[T_xmT])
        else:
            lc, T_lc = lastc.items[prev % 3]
            P.op("pool", lambda e: e.tensor_copy(out=xmT[:, :, 0:1], in_=lc[:]), r=[T_lc], w=[T_xmT])
        if nxt is None:
            P.op("pool", lambda e: e.memset(xmT[:, :, ntok + 1:ntok + 2], 0.0), w=[T_xmT])
        else:
            xn, T_xn = xmTr.items[nxt % 2]
            P.op("pool", lambda e: e.tensor_copy(out=xmT[:, :, ntok + 1:ntok + 2], in_=xn[:, :, 1:2]), r=[T_xn], w=[T_xmT])
        for fc in range(4):
            P.op("dve", lambda e, fc=fc: e.tensor_scalar(out=cv[:, fc, 0:ntok], in0=xmT[:, fc, 1:1 + ntok], scalar1=cw[:, fc, 1:2],
                                                         scalar2=cb[:, fc:fc + 1], op0=ALU.mult, op1=ALU.add), r=[T_xmT, T_cw, T_cb],
                 w=[T_cvf[fc]] + ([T_cv] if j == 0 else []))
        for fc in range(4):
            P.op("dve", lambda e, fc=fc: e.scalar_tensor_tensor(out=cv[:, fc, 0:ntok], in0=xmT[:, fc, 0:ntok], scalar=cw[:, fc, 0:1],
                                                                in1=cv[:, fc, 0:ntok], op0=ALU.mult, op1=ALU.add), r=[T_xmT, T_cw], w=[T_cvf[fc]])
        for fc in range(4):
            P.op("dve", lambda e, fc=fc: e.scalar_tensor_tensor(out=cv[:, fc, 0:ntok], in0=xmT[:, fc, 2:2 + ntok], scalar=cw[:, fc, 2:3],
                                                                in1=cv[:, fc, 0:ntok], op0=ALU.mult, op1=ALU.add), r=[T_xmT, T_cw], w=[T_cvf[fc]])
        yield
        P.op("act", lambda e: e.activation(out=sgt[:, :, 0:ntok], in_=cv[:, :, 0:ntok], func=AF.Sigmoid), r=T_cvf, w=[T_sgt])
        P.op("pool", lambda e: e.tensor_tensor(out=xcT[:, 0:2, 0:ntok], in0=cv[:, 0:2, 0:ntok], in1=sgt[:, 0:2, 0:ntok], op=ALU.mult),
             r=T_cvf + [T_sgt], w=[T_xcT])
        P.op("dve", lambda e: e.tensor_tensor(out=xcT[:, 2:4, 0:ntok], in0=cv[:, 2:4, 0:ntok], in1=sgt[:, 2:4, 0:ntok], op=ALU.mult),
             r=T_cvf + [T_sgt], w=[T_xcT])
        P.op("act", lambda e: e.copy(out=xmTb[:, :, 0:ntok], in_=xmT[:, :, 1:1 + ntok]), r=[T_xmT], w=[T_xmTb])
        yield
        for m, dst, okey in ((0, qTv, "qT"), (1, kTv, "kT")):
            stg, T_stg = st4.next()
            for fc in range(4):
                bk, T_bk = P.bank()
                P.op("pe", lambda e, fc=fc, bk=bk, m=m: e.matmul(bk[:, 0:ntok], lhsT=BDb[:, m, fc, :], rhs=xcT[:, fc, 0:ntok], start=True, stop=True),
                     r=[T_BDb, T_xcT], w=[T_bk])
                sc = 1.0 if m == 0 else KSCALE
                if fc % 2 == 0:
                    P.op("act", lambda e, fc=fc, bk=bk, stg=stg, sc=sc: e.mul(out=stg[:, fc, 0:ntok], in_=bk[:, 0:ntok], mul=sc), r=[T_bk], w=[T_stg])
                else:
                    P.op("dve", lambda e, fc=fc, bk=bk, stg=stg, sc=sc: e.tensor_scalar(out=stg[:, fc, 0:ntok], in0=bk[:, 0:ntok], scalar1=sc, scalar2=None, op0=ALU.mult),
                         r=[T_bk], w=[T_stg])
            P.dma(dst[:, :, t0:t0 + ntok], stg[:, :, 0:ntok], r=[T_stg], w=[TK[okey]], tok=T_stg)
            yield
        bk, T_bk = P.bank()
        for fc in range(4):
            P.op("pe", lambda e, fc=fc, bk=bk: e.matmul(bk[0:16, 0:ntok], lhsT=Wgc[:, fc, :], rhs=xcT[:, fc, 0:ntok], start=(fc == 0), stop=False),
                 r=[T_Wgc, T_xcT], w=[T_bk])
        for fc in range(4):
            P.op("pe", lambda e, fc=fc, bk=bk: e.matmul(bk[0:16, 0:ntok], lhsT=Wgv[:, fc, :], rhs=xmTb[:, fc, 0:ntok], start=False, stop=(fc == 3)),
                 r=[T_Wgv, T_xmTb], w=[T_bk])
        gst, T_gst = gstr.next()
        P.op("act", lambda e, bk=bk, gst=gst: e.copy(out=gst[:, 0:ntok], in_=bk[0:16, 0:ntok]), r=[T_bk], w=[T_gst])
        P.dma(D["gpT"][:, t0:t0 + ntok], gst[:, 0:ntok], r=[T_gst], w=[TK["gpT"]], tok=T_gst)
        SZ, T_SZ = SZr.items[j % 2]
        for t in range(nt):
            b1, T_b1 = P.bank()
            b2, T_b2 = P.bank()
            b3, T_b3 = P.bank()
            for fc in range(4):
                bb, T_bb = (b1, T_b1) if fc < 2 else (b2, T_b2)
                P.op("pe", lambda e, fc=fc, bb=bb, t=t: e.matmul(bb[:, (fc % 2) * 256:(fc % 2 + 1) * 256], lhsT=xcT[:, fc, t * 128:(t + 1) * 128],
                                                                 rhs=KS[:, fc, :], start=True, stop=True), r=[T_xcT, T_KS], w=[T_bb])
            for fc in range(4):
                P.op("pe", lambda e, fc=fc, t=t, b3=b3: e.matmul(b3[:, fc * 128:(fc + 1) * 128], lhsT=xmTb[:, fc, t * 128:(t + 1) * 128],
                                                                 rhs=BDb[:, 2, fc, :], start=True, stop=True), r=[T_xmTb, T_BDb], w=[T_b3])
            kts, T_kts = ktr.next()
            vts, T_vts = vtr.next()
            for hf, (bb, T_bb) in enumerate(((b1, T_b1), (b2, T_b2))):
                bv = bb[:].rearrange("p (a b) -> p a b", a=2)
                P.op("act", lambda e, bv=bv, kts=kts, hf=hf: e.copy(out=kts[:, hf * 256:(hf + 1) * 256].rearrange("p (a b) -> p a b", a=2),
                                                                   in_=bv[:, :, 0:128]), r=[T_bb], w=[T_kts])
            P.op("dve", lambda e, b3=b3, vts=vts: e.tensor_copy(out=vts[:], in_=b3[:]), r=[T_b3], w=[T_vts])
            P.dma(D["ktok"][t0 + t * 128:t0 + (t + 1) * 128, :], kts[:], r=[T_kts], w=[TK["ktok"]], tok=T_kts)
            P.dma(D["vtok"][t0 + t * 128:t0 + (t + 1) * 128, :], vts[:], r=[T_vts], w=[TK["vtok"]], tok=T_vts)
            if mi == 0:
                bcs, T_bcs = bcr.next()
                tl = t0 - TCTX
                for hf, (bb, T_bb) in enumerate(((b1, T_b1), (b2, T_b2))):
                    bv = bb[:].rearrange("p (a b) -> p a b", a=2)
                    P.op("dve", lambda e, bv=bv, bcs=bcs, hf=hf, t=t: e.tensor_tensor(
                        out=bcs[:, hf * 256:(hf + 1) * 256].rearrange("p (a b) -> p a b", a=2), in0=bv[:, :, 128:256],
                        in1=SZ[:, t, hf * 256:(hf + 1) * 256].rearrange("p (a b) -> p a b", a=2), op=ALU.mult), r=[T_bb, T_SZ], w=[T_bcs])
                P.dma(D["Bc"][tl + t * 128:tl + (t + 1) * 128, :], bcs[:], r=[T_bcs], w=[TK["Bc"]], tok=T_bcs)
            yield

    def run(*gens):
        gens = list(gens)
        while gens:
            for g in list(gens):
                try:
                    next(g)
                except StopIteration:
                    gens.remove(g)

    nb = len(blocks)
    run(S1a(0)); run(S1b(0))
    run(S2(0, None, None))
    run(S1a(1)); run(S1b(1))
    run(S1a(2)); run(S1b(2))
    for k in range(2, nb):
        gens = [S2(k - 1, (k - 2) if k - 2 >= 1 else None, k)]
        if k + 1 < nb:
            gens.insert(0, S1a(k + 1))
        run(*gens)
        if k + 1 < nb:
            run(S1b(k + 1))
    run(S2(nb - 1, nb - 2, None))
    return out_toks


def host_p1_inputs(inp, b, h):
    f32 = np.float32
    d = {}
    d["xin"] = np.ascontiguousarray(np.concatenate([inp["ctx"][b], inp["x"][b]], axis=0))
    cv = np.stack([inp["c"][b], inp["c_ctx"]], axis=-1)
    d["cvec"] = np.ascontiguousarray(cv.reshape(8, 128, 2).transpose(1, 0, 2))
    d["w_ada"] = np.ascontiguousarray(inp["w_ada"][0])
    d["b_ada"] = np.ascontiguousarray(inp["b_ada"][0])
    d["g_pre"] = np.ascontiguousarray(inp["a_norm_pre"][0])
    w = inp["a_w_in"][0]
    hs = slice(h * DH, (h + 1) * DH)
    d["w_in"] = np.ascontiguousarray(np.concatenate([w[:, 0:2048][:, hs], w[:, 2048:4096][:, hs], w[:, 4096:6144][:, hs]], axis=1))
    d["convw"] = np.ascontiguousarray(inp["a_conv_w"][0][:, hs].T.reshape(4, 128, 3).transpose(1, 0, 2))
    d["convb"] = np.ascontiguousarray(inp["a_conv_b"][0][hs].reshape(4, 128).T)
    d["skip"] = np.ascontiguousarray(inp["a_skip"][0][hs].reshape(4, 128).T)
    d["hn"] = np.ascontiguousarray(inp["a_head_norm"][0][hs])
    BD = np.zeros((128, 3, 4, 128), f32)
    BDT = np.zeros((128, 3, 4, 128), f32)
    for m, key in enumerate(("a_w_q", "a_w_k", "a_w_v")):
        wq = inp[key][0][h * 128:(h + 1) * 128]
        for fc in range(4):
            for g in range(32):
                blk = wq[fc * 32 + g]
                BD[g * 4:(g + 1) * 4, m, fc, g * 4:(g + 1) * 4] = blk
                BDT[g * 4:(g + 1) * 4, m, fc, g * 4:(g + 1) * 4] = blk.T
    d["BD"] = BD
    d["BDT"] = BDT
    wgf = inp["a_w_gate_f"][0]
    wgb = inp["a_w_gate_b"][0]
    wgc = np.concatenate([wgf, wgb], axis=1)
    wg = np.zeros((128, 3, 4, 16), f32)
    for m in range(3):
        rows = wgc[m * 2048 + h * DH:m * 2048 + (h + 1) * DH]
        wg[:, m, :, :] = rows.reshape(4, 128, 16).transpose(1, 0, 2)
    d["wg"] = wg
    d["ident"] = np.eye(128, dtype=f32)
    return d


def build_p1():
    nc = bass.Bass("TRN2", target_bir_lowering=False)
    P = Prog(nc)
    P.init_banks(8)
    D = {}

    def din(name, shape, dt=F32):
        D[name] = nc.dram_tensor(name, list(shape), dt, kind="ExternalInput").ap()

    def dout(name, shape, dt=F32):
        D[name] = nc.dram_tensor(name, list(shape), dt, kind="ExternalOutput").ap()

    din("xin", [TT, 1024]); din("cvec", [128, 8, 2]); din("w_ada", [1024, 3072]); din("b_ada", [3072]); din("g_pre", [1024])
    din("w_in", [1024, 1536]); din("convw", [128, 4, 3]); din("convb", [128, 4]); din("skip", [128, 4]); din("hn", [512])
    din("BD", [128, 3, 4, 128]); din("BDT", [128, 3, 4, 128]); din("wg", [128, 3, 4, 16]); din("ident", [128, 128])
    dout("qT", [512, TT], BF16); dout("kT", [512, TT], BF16); dout("ktok", [TT, 512], BF16); dout("vtok", [TT, 512], BF16)
    dout("A", [TLAT, 512]); dout("Bc", [TLAT, 512]); dout("gpT", [16, TT])
    K = emit_consts(P, D)
    toks = emit_p1(P, K, D)
    P.op("sp", None, r=toks)
    P.finalize()
    return nc, P


NCH = TT // 128
ORDER_F = list(range(NCH))
ORDER_B = [1, 0] + list(range(NCH - 1, 1, -1))


def emit_gates(P, K, D, d, T_GP=None):
    nm = "g%d_" % d
    n = NCH
    X = {}
    pre = []
    for ri in range(2):
        r = 2 * d + ri
        if T_GP is None:
            pt, T_pt = P.tile(nm + "p%d" % ri, [n, 4, 128])
            P.dma(pt[:], D["gsel"][r].rearrange("s (p l) -> p s l", l=128), w=[T_pt], tok=T_pt)
        else:
            pt, T_pt = P.tile(nm + "p%d" % ri, [128, 4, 128])
            GPv = D["GP"].rearrange("r (p l) -> (r p) l", l=128)
            for s_ in range(4):
                P.op("pool", lambda e, pt=pt, s_=s_, r=r: e.indirect_dma_start(out=pt[:, s_, :], out_offset=None, in_=GPv,
                                                                             in_offset=bass.IndirectOffsetOnAxis(ap=K["gidx"][:, r * 4 + s_:r * 4 + s_ + 1], axis=0)),
                     r=[T_GP, K["T_gidx"]], w=[T_pt], dma=T_pt)
        xt, T_x = P.tile(nm + "x%d" % ri, [n, 128])
        P.op("dve", lambda e, pt=pt, xt=xt: e.tensor_tensor(out=xt[:], in0=pt[0:n, 0, :], in1=pt[0:n, 1, :], op=ALU.add), r=[T_pt], w=[T_x])
        P.op("dve", lambda e, pt=pt, xt=xt: e.tensor_tensor(out=xt[:], in0=xt[:], in1=pt[0:n, 2, :], op=ALU.add), r=[T_pt, T_x], w=[T_x])
        P.op("dve", lambda e, pt=pt, xt=xt: e.tensor_tensor(out=xt[:], in0=xt[:], in1=pt[0:n, 3, :], op=ALU.add), r=[T_pt, T_x], w=[T_x])
        P.op("dve", lambda e, xt=xt, r=r: e.tensor_scalar(out=xt[:], in0=xt[:], scalar1=K["gbias"][0:n, r:r + 1], scalar2=None, op0=ALU.add),
             r=[T_x, K["T_gbias"]], w=[T_x])
        if d == 1:
            bk, T_bk = P.bank()
            P.op("pe", lambda e, bk=bk, xt=xt: e.matmul(bk[0:n, 0:128], lhsT=K["perm"][:], rhs=xt[:], start=True, stop=True),
                 r=[K["T_perm"], T_x], w=[T_bk])
            P.op("dve", lambda e, bk=bk, xt=xt: e.tensor_copy(out=xt[:], in_=bk[0:n, 0:128]), r=[T_bk], w=[T_x])
        pre.append((xt, T_x))
    (li, T_li), (lf, T_lf) = pre
    P.op("act", lambda e: e.activation(out=lf[:], in_=lf[:], func=AF.Exp, scale=-1.0), r=[T_lf], w=[T_lf])
    P.op("act", lambda e: e.activation(out=lf[:], in_=lf[:], func=AF.Ln, bias=1.0), r=[T_lf], w=[T_lf])
    G, T_G = P.tile(nm + "G", [n, 128])
    P.op("dve", lambda e: e.tensor_tensor_scan(out=G[:], data0=K["ones"][0:n, :], data1=lf[:], initial=0.0, op0=ALU.mult, op1=ALU.add),
         r=[K["T_ones"], T_lf], w=[T_G])
    sm, T_sm = P.tile(nm + "sm", [n, 8])
    P.op("dve", lambda e: e.tensor_copy(out=sm[:, 0:1], in_=G[:, 127:128]), r=[T_G], w=[T_sm])
    if d == 1:
        P.op("dve", lambda e: e.tensor_scalar(out=G[:], in0=G[:], scalar1=-1.0, scalar2=sm[:, 0:1], op0=ALU.mult, op1=ALU.add), r=[T_G, T_sm], w=[T_G])
        P.op("dve", lambda e: e.tensor_tensor(out=G[:], in0=G[:], in1=lf[:], op=ALU.add), r=[T_G, T_lf], w=[T_G])
    bk, T_bk = P.bank()
    P.op("pe", lambda e: e.matmul(bk[0:n, 0:1], lhsT=K["triu"][:], rhs=sm[:, 0:1], start=True, stop=True), r=[K["T_triu"], T_sm], w=[T_bk])
    P.op("dve", lambda e: e.tensor_copy(out=sm[:, 1:2], in_=bk[0:n, 0:1]), r=[T_bk], w=[T_sm])
    P.op("dve", lambda e: e.tensor_scalar(out=G[:], in0=G[:], scalar1=sm[:, 1:2], scalar2=None, op0=ALU.add), r=[T_G, T_sm], w=[T_G])
    a, T_a = P.tile(nm + "a", [n, 128])
    P.op("dve", lambda e: e.tensor_tensor(out=a[:], in0=li[:], in1=G[:], op=ALU.add), r=[T_li, T_G], w=[T_a])
    P.op("dve", lambda e: e.tensor_reduce(out=sm[:, 2:3], in_=a[:], axis=AX.X, op=ALU.max), r=[T_a], w=[T_sm])
    bk2, T_bk2 = P.bank()
    P.op("pe", lambda e: e.matmul(bk2[0:1, 0:n], lhsT=sm[:, 2:3], rhs=K["idf"][0:n, 0:n], start=True, stop=True), r=[T_sm, K["T_idf"]], w=[T_bk2])
    row, T_row = P.tile(nm + "row", [1, 4, n])
    P.op("dve", lambda e: e.tensor_copy(out=row[:, 0, :], in_=bk2[0:1, 0:n]), r=[T_bk2], w=[T_row])
    P.op("dve", lambda e: e.tensor_tensor_scan(out=row[:, 1, :], data0=row[:, 0, :], data1=row[:, 0, :], initial=0.0, op0=ALU.max, op1=ALU.max),
         r=[T_row], w=[T_row])
    P.op("dve", lambda e: e.memset(row[:, 2, 0:1], 0.0), w=[T_row])
    P.op("dve", lambda e: e.tensor_copy(out=row[:, 2, 1:n], in_=row[:, 1, 0:n - 1]), r=[T_row], w=[T_row])
    P.op("dve", lambda e: e.tensor_tensor(out=row[:, 3, :], in0=row[:, 2, :], in1=row[:, 1, :], op=ALU.subtract), r=[T_row], w=[T_row])
    P.op("act", lambda e: e.activation(out=row[:, 3, :], in_=row[:, 3, :], func=AF.Exp), r=[T_row], w=[T_row])
    bk3, T_bk3 = P.bank()
    P.op("pe", lambda e: e.matmul(bk3[0:n, 0:1], lhsT=row[:, 2, :], rhs=K["ones"][0:1, 0:1], start=True, stop=True), r=[T_row, K["T_ones"]], w=[T_bk3])
    P.op("dve", lambda e: e.tensor_scalar(out=sm[:, 3:4], in0=bk3[0:n, 0:1], scalar1=-1.0, scalar2=None, op0=ALU.mult), r=[T_bk3], w=[T_sm])
    bk4, T_bk4 = P.bank()
    P.op("pe", lambda e: e.matmul(bk4[:, 0:n], lhsT=K["ones"][0:1, :], rhs=row[:, 3, :], start=True, stop=True), r=[T_row, K["T_ones"]], w=[T_bk4])
    decb, T_decb = P.tile(nm + "decb", [128, n])
    P.op("dve", lambda e: e.tensor_copy(out=decb[:], in_=bk4[:, 0:n]), r=[T_bk4], w=[T_decb])
    P.op("act", lambda e: e.activation(out=a[:], in_=a[:], func=AF.Exp, bias=sm[:, 3:4]), r=[T_a, T_sm], w=[T_a])
    P.op("act", lambda e: e.activation(out=G[:], in_=G[:], func=AF.Exp, bias=sm[:, 3:4]), r=[T_G, T_sm], w=[T_G])
    outs = {}
    for key, (src, T_src) in (("e1T", (a, T_a)), ("thrT", (G, T_G))):
        bk5, T_bk5 = P.bank()
        P.op("pe", lambda e, bk5=bk5, src=src: e.transpose(out=bk5[:, 0:n], in_=src[:], identity=K["idf"][0:n, 0:n]), r=[T_src, K["T_idf"]], w=[T_bk5])
        dst, T_dst = P.tile(nm + key, [128, n])
        P.op("dve", lambda e, bk5=bk5, dst=dst: e.tensor_copy(out=dst[:], in_=bk5[:, 0:n]), r=[T_bk5], w=[T_dst])
        outs[key] = (dst, T_dst)
    outs["decb"] = (decb, T_decb)
    return outs


def emit_scan(P, K, D, G, TK=None):
    qTv = D["qT"].rearrange("(fc p) t -> p fc t", p=128)
    kTv = D["kT"].rearrange("(fc p) t -> p fc t", p=128)
    NR = 6
    KTr = Ring(P, "KT", NR, [128, 4, 128], BF16)
    QTr = Ring(P, "QT", NR, [128, 4, 128], BF16)
    Ktr = Ring(P, "Kt", NR, [128, 512], BF16)
    Vtr = Ring(P, "Vt", NR, [128, 512], BF16)
    Ver = Ring(P, "Ve", 3, [128, 513], BF16)
    Smr = Ring(P, "Sm", 2, [128, 128], BF16)
    dnr = Ring(P, "dn", 2, [128, 4])
    hor = Ring(P, "ho", 3, [128, 512])
    Cb = [[P.tile("Cb%d_%d" % (d, i), [128, 4, 513], BF16)[0] for i in range(2)] for d in range(2)]
    TC = [[[Tok("TC%d_%d_%d" % (d, i, dc)) for dc in range(5)] for i in range(2)] for d in range(2)]
    for d in range(2):
        c0 = Cb[d][0]
        P.op("pool", lambda e, c0=c0: e.memset(c0[:], 0.0), w=TC[d][0])
    orders = (ORDER_F, ORDER_B)
    rk = (lambda k: [TK[k]]) if TK is not None else (lambda k: [])
    out_toks = [[None] * 64 for _ in range(2)]
    items = [(s, d) for s in range(NCH) for d in range(2)]
    L = {}

    def loads(i):
        s, d = items[i]
        c = orders[d][s]
        t0 = c * 128
        lat = c >= 2
        KT, T_KT = KTr.next(); QT, T_QT = QTr.next(); Kt, T_Kt = Ktr.next(); Vt, T_Vt = Vtr.next()
        P.dma(Kt[:], D["ktok"][t0:t0 + 128, :], r=rk("ktok"), w=[T_Kt], tok=T_Kt)
        P.dma(Vt[:], D["vtok"][t0:t0 + 128, :], r=rk("vtok"), w=[T_Vt], tok=T_Vt)
        if lat:
            P.dma(KT[:], kTv[:, :, t0:t0 + 128], r=rk("kT"), w=[T_KT], tok=T_KT)
            P.dma(QT[:], qTv[:, :, t0:t0 + 128], r=rk("qT"), w=[T_QT], tok=T_QT)
        L[i] = dict(KT=KT, T_KT=T_KT, QT=QT, T_QT=T_QT, Kt=Kt, T_Kt=T_Kt, Vt=Vt, T_Vt=T_Vt)

    def prescale(i):
        s, d = items[i]
        e1T, T_e1T = G[d]["e1T"]
        Ve, T_Ve = Ver.next()
        Vt, T_Vt = L[i]["Vt"], L[i]["T_Vt"]
        P.op("dve", lambda e, Ve=Ve, Vt=Vt, s=s, e1T=e1T: e.tensor_scalar(out=Ve[:, 0:512], in0=Vt[:], scalar1=e1T[:, s:s + 1], scalar2=None,
                                                                          op0=ALU.mult), r=[T_Vt, T_e1T], w=[T_Ve])
        P.op("pool", lambda e, Ve=Ve, s=s, e1T=e1T: e.tensor_copy(out=Ve[:, 512:513], in_=e1T[:, s:s + 1]), r=[T_e1T], w=[T_Ve])
        L[i]["Ve"] = Ve
        L[i]["T_Ve"] = T_Ve

    def compute(i):
        s, d = items[i]
        c = orders[d][s]
        lat = c >= 2
        cl = c - 2
        thrT, T_thr = G[d]["thrT"]
        decb, T_decb = G[d]["decb"]
        Cc, TCc = Cb[d][s % 2], TC[d][s % 2]
        Cn, TCn = Cb[d][(s + 1) % 2], TC[d][(s + 1) % 2]
        X = L.pop(i)
        KT, T_KT, QT, T_QT, Kt, T_Kt, Ve, T_Ve = X["KT"], X["T_KT"], X["QT"], X["T_QT"], X["Kt"], X["T_Kt"], X["Ve"], X["T_Ve"]
        (bs, T_bs), (bn, T_bn), bu0, bu1 = [P.banks[4 * d + j] for j in range(4)]
        bus = (bu0, bu1)

        def state(dc):
            bu, T_bu = bus[dc % 2]
            P.op("pe", lambda e, dc=dc, bu=bu, Cc=Cc: e.matmul(bu[:], lhsT=K["idb"][:], rhs=Cc[:, dc, 0:512], start=True, stop=False), r=[K["T_idb"], TCc[dc]], w=[T_bu])
            P.op("pe", lambda e, dc=dc, bu=bu, Kt=Kt, Ve=Ve: e.matmul(bu[:], lhsT=Kt[:, dc * 128:(dc + 1) * 128], rhs=Ve[:, 0:512], start=False, stop=True),
                 r=[T_Kt, T_Ve], w=[T_bu])
            if dc % 2 == 0:
                P.op("act", lambda e, dc=dc, bu=bu, Cn=Cn, s=s, decb=decb: e.activation(out=Cn[:, dc, 0:512], in_=bu[:], func=AF.Copy, scale=decb[:, s:s + 1]),
                     r=[T_bu, T_decb], w=[TCn[dc]])
            else:
                P.op("dve", lambda e, dc=dc, bu=bu, Cn=Cn, s=s, decb=decb: e.tensor_scalar(out=Cn[:, dc, 0:512], in0=bu[:], scalar1=decb[:, s:s + 1], scalar2=None, op0=ALU.mult),
                     r=[T_bu, T_decb], w=[TCn[dc]])

        if lat:
            for dc in range(4):
                P.op("pe", lambda e, dc=dc, bs=bs, KT=KT, QT=QT: e.matmul(bs[:, 0:128], lhsT=KT[:, dc, :], rhs=QT[:, dc, :], start=(dc == 0), stop=(dc == 3)),
                     r=[T_KT, T_QT], w=[T_bs])
            Sm, T_Sm = Smr.next()
            P.op("dve", lambda e, bs=bs, Sm=Sm, d=d: e.tensor_tensor(out=Sm[:], in0=bs[:, 0:128], in1=K["mask"][:, d, :], op=ALU.mult),
                 r=[T_bs, K["T_mask"]], w=[T_Sm])
        state(0)
        state(1)
        if lat:
            for dc in range(4):
                P.op("pe", lambda e, dc=dc, bn=bn, QT=QT, Cc=Cc: e.matmul(bn[:], lhsT=QT[:, dc, :], rhs=Cc[:, dc, 0:512], start=(dc == 0), stop=False),
                     r=[T_QT, TCc[dc]], w=[T_bn])
            P.op("pe", lambda e, bn=bn, Sm=Sm, Ve=Ve: e.matmul(bn[:], lhsT=Sm[:], rhs=Ve[:, 0:512], start=False, stop=True), r=[T_Sm, T_Ve], w=[T_bn])
            for dc in range(4):
                P.op("pe", lambda e, dc=dc, bs=bs, QT=QT, Cc=Cc: e.matmul(bs[:, 256:257], lhsT=QT[:, dc, :], rhs=Cc[:, dc, 512:513], start=(dc == 0), stop=False),
                     r=[T_QT, TCc[4]], w=[T_bs])
            P.op("pe", lambda e, bs=bs, Sm=Sm, Ve=Ve: e.matmul(bs[:, 256:257], lhsT=Sm[:], rhs=Ve[:, 512:513], start=False, stop=True), r=[T_Sm, T_Ve], w=[T_bs])
        for dc in range(4):
            P.op("pe", lambda e, dc=dc, bs=bs, Cc=Cc: e.matmul(bs[:, 260 + dc:261 + dc], lhsT=K["idb"][:], rhs=Cc[:, dc, 512:513], start=True, stop=False),
                 r=[K["T_idb"], TCc[4]], w=[T_bs])
            P.op("pe", lambda e, dc=dc, bs=bs, Kt=Kt, Ve=Ve: e.matmul(bs[:, 260 + dc:261 + dc], lhsT=Kt[:, dc * 128:(dc + 1) * 128], rhs=Ve[:, 512:513], start=False, stop=True),
                 r=[T_Kt, T_Ve], w=[T_bs])
        state(2)
        state(3)
        P.op("dve", lambda e, bs=bs, Cn=Cn, s=s, decb=decb: e.tensor_scalar(out=Cn[:, :, 512], in0=bs[:, 260:264], scalar1=decb[:, s:s + 1], scalar2=None, op0=ALU.mult),
             r=[T_bs, T_decb], w=[TCn[4]])
        if lat:
            dn, T_dn = dnr.next()
            P.op("dve", lambda e, bs=bs, dn=dn, s=s, thrT=thrT: e.tensor_scalar(out=dn[:, 0:1], in0=bs[:, 256:257], scalar1=-1.0, scalar2=thrT[:, s:s + 1], op0=ALU.mult, op1=ALU.max),
                 r=[T_bs, T_thr], w=[T_dn])
            P.op("dve", lambda e, bs=bs, dn=dn: e.tensor_tensor(out=dn[:, 1:2], in0=bs[:, 256:257], in1=dn[:, 0:1], op=ALU.max), r=[T_bs, T_dn], w=[T_dn])
            P.op("dve", lambda e, dn=dn: e.reciprocal(out=dn[:, 3:4], in_=dn[:, 1:2]), r=[T_dn], w=[T_dn])
            ho, T_ho = hor.next()
            P.op("act", lambda e, bn=bn, ho=ho, dn=dn: e.activation(out=ho[:], in_=bn[:], func=AF.Copy, scale=dn[:, 3:4]), r=[T_bn, T_dn], w=[T_ho])
            T_o = Tok("h_o%d_%d" % (d, cl))
            out_toks[d][cl] = T_o
            P.dma(D["hdir%d" % d][cl * 128:(cl + 1) * 128, :], ho[:], r=[T_ho], w=[T_o], tok=T_ho)

    n = len(items)
    for idx in range(-4, n):
        if 0 <= idx + 4 < n:
            loads(idx + 4)
        if 0 <= idx + 1 < n:
            prescale(idx + 1)
        if idx >= 0:
            compute(idx)
    return out_toks


def emit_outstage(P, K, D, h_toks):
    wo, T_wo = P.tile("wo", [128, 4, 1024], BF16)
    wst = Ring(P, "wost", 2, [128, 1024])
    wov = D["w_out"].rearrange("(fc p) c -> p fc c", p=128)
    for fc in range(4):
        wt, T_wt = wst.next()
        P.dma(wt[:], wov[:, fc, :], w=[T_wt], tok=T_wt)
        P.op("pool", lambda e, fc=fc, wt=wt: e.tensor_copy(out=wo[:, fc, :], in_=wt[:]), r=[T_wt], w=[T_wo])
    hfr = Ring(P, "hf", 2, [128, 512])
    hbr = Ring(P, "hb", 2, [128, 512])
    Ar = Ring(P, "Ain", 2, [128, 512])
    Br = Ring(P, "Bin", 2, [128, 512])
    junk, T_junk = P.tile("ojunk", [128, 512], BF16)
    str_ = Ring(P, "ost", 2, [128, 8])
    ybr = Ring(P, "yb", 2, [128, 512], BF16)
    yTr = Ring(P, "yT", 2, [128, 4, 128], BF16)
    psr = Ring(P, "pst", 2, [128, 1024])
    outs = []
    for cl in range(64):
        hf, T_hf = hfr.next(); hb, T_hb = hbr.next(); At, T_At = Ar.next(); Bt, T_Bt = Br.next()
        rows = slice(cl * 128, (cl + 1) * 128)
        P.dma(hf[:], D["hdir0"][rows, :], r=[h_toks[0][cl]], w=[T_hf], tok=T_hf)
        P.dma(hb[:], D["hdir1"][rows, :], r=[h_toks[1][cl]], w=[T_hb], tok=T_hb)
        P.dma(At[:], D["A"][rows, :], w=[T_At], tok=T_At)
        P.dma(Bt[:], D["Bc"][rows, :], w=[T_Bt], tok=T_Bt)
        st, T_st = str_.next()
        P.op("pool", lambda e, hf=hf, hb=hb: e.tensor_tensor(out=hf[:], in0=hf[:], in1=hb[:], op=ALU.add), r=[T_hf, T_hb], w=[T_hf])
        P.op("act", lambda e, hf=hf, st=st: e.activation(out=junk[:], in_=hf[:], func=AF.Copy, accum_out=st[:, 0:1]), r=[T_hf], w=[T_junk, T_st])
        P.op("act", lambda e, hf=hf, st=st: e.activation(out=junk[:], in_=hf[:], func=AF.Square, accum_out=st[:, 1:2]), r=[T_hf], w=[T_junk, T_st])
        P.op("dve", lambda e, st=st: e.tensor_scalar(out=st[:, 2:3], in0=st[:, 0:1], scalar1=1.0 / 512, scalar2=None, op0=ALU.mult), r=[T_st], w=[T_st])
        P.op("dve", lambda e, st=st: e.tensor_tensor(out=st[:, 3:4], in0=st[:, 2:3], in1=st[:, 2:3], op=ALU.mult), r=[T_st], w=[T_st])
        P.op("dve", lambda e, st=st: e.scalar_tensor_tensor(out=st[:, 4:5], in0=st[:, 1:2], scalar=1.0 / 512, in1=st[:, 3:4], op0=ALU.mult, op1=ALU.subtract),
             r=[T_st], w=[T_st])
        P.op("dve", lambda e, st=st: e.tensor_scalar(out=st[:, 4:5], in0=st[:, 4:5], scalar1=EPS, scalar2=None, op0=ALU.add), r=[T_st], w=[T_st])
        P.op("pool", lambda e, st=st: e.tensor_tensor(out=st[:, 5:6], in0=st[:, 4:5], in1=K["nh"][:], op=ALU.pow), r=[T_st, K["T_nh"]], w=[T_st])
        P.op("dve", lambda e, hf=hf, st=st: e.tensor_scalar(out=hf[:], in0=hf[:], scalar1=st[:, 2:3], scalar2=st[:, 5:6], op0=ALU.subtract, op1=ALU.mult),
             r=[T_hf, T_st], w=[T_hf])
        P.op("pool", lambda e, hf=hf, At=At: e.tensor_tensor(out=hf[:], in0=hf[:], in1=At[:], op=ALU.mult), r=[T_hf, T_At], w=[T_hf])
        yb, T_yb = ybr.next()
        P.op("dve", lambda e, hf=hf, Bt=Bt, yb=yb: e.tensor_tensor(out=yb[:], in0=hf[:], in1=Bt[:], op=ALU.add), r=[T_hf, T_Bt], w=[T_yb])
        bk, T_bk = P.bank()
        bkb = bk[:].bitcast(BF16).rearrange("p (a b) -> p a b", a=8)
        for fc in range(4):
            P.op("pe", lambda e, fc=fc, bkb=bkb, yb=yb: e.transpose(out=bkb[:, fc, :], in_=yb[:, fc * 128:(fc + 1) * 128], identity=K["idb"][:]),
                 r=[T_yb, K["T_idb"]], w=[T_bk])
        yT, T_yT = yTr.next()
        P.op("act", lambda e, bkb=bkb, yT=yT: e.copy(out=yT[:], in_=bkb[:, 0:4, :]), r=[T_bk], w=[T_yT])
        pst, T_pst = psr.next()
        for hfi in range(2):
            bo, T_bo = P.bank()
            for fc in range(4):
                P.op("pe", lambda e, fc=fc, bo=bo, yT=yT, hfi=hfi: e.matmul(bo[:], lhsT=yT[:, fc, :], rhs=wo[:, fc, hfi * 512:(hfi + 1) * 512], start=(fc == 0), stop=(fc == 3)),
                     r=[T_yT, T_wo], w=[T_bo])
            if hfi == 0:
                P.op("act", lambda e, bo=bo, pst=pst: e.copy(out=pst[:, 0:512], in_=bo[:]), r=[T_bo], w=[T_pst])
            else:
                P.op("dve", lambda e, bo=bo, pst=pst: e.tensor_copy(out=pst[:, 512:1024], in_=bo[:]), r=[T_bo], w=[T_pst])
        T_o = Tok("part_o")
        outs.append(T_o)
        P.dma(D["part"][rows, :], pst[:], r=[T_pst], w=[T_o], tok=T_pst)
    return outs


def p2_consts(P, K, D):
    for name, shape in (("gbias", [128, 4]), ("perm", [NCH, NCH]), ("triu", [NCH, NCH]), ("mask", [128, 2, 128])):
        K[name], K["T_" + name] = P.tile("k_" + name, shape)
        P.dma(K[name][:], D[name], w=[K["T_" + name]], tok=K["T_" + name])


def host_p2_consts(inp, h):
    f32 = np.float32
    d = {}
    bf = inp["a_b_gate_f"][0]
    bb = inp["a_b_gate_b"][0]
    d["gbias"] = np.tile(np.array([bf[h], bf[4 + h], bb[h], bb[4 + h]], f32)[None, :], (128, 1))
    perm = np.zeros((NCH, NCH), f32)
    for s, c in enumerate(ORDER_B):
        perm[c, s] = 1.0
    d["perm"] = perm
    d["triu"] = np.triu(np.ones((NCH, NCH), f32), 1)
    m = np.zeros((128, 2, 128), f32)
    m[:, 0, :] = np.triu(np.ones((128, 128), f32))
    m[:, 1, :] = np.tril(np.ones((128, 128), f32))
    d["mask"] = m
    d["ident"] = np.eye(128, dtype=f32)
    return d


def build_p2():
    nc = bass.Bass("TRN2", target_bir_lowering=False)
    P = Prog(nc)
    P.init_banks(8)
    D = {}

    def din(name, shape, dt=F32):
        D[name] = nc.dram_tensor(name, list(shape), dt, kind="ExternalInput").ap()

    def dout(name, shape, dt=F32):
        D[name] = nc.dram_tensor(name, list(shape), dt, kind="ExternalOutput").ap()

    din("qT", [512, TT], BF16); din("kT", [512, TT], BF16); din("ktok", [TT, 512], BF16); din("vtok", [TT, 512], BF16)
    din("A", [TLAT, 512]); din("Bc", [TLAT, 512]); din("gsel", [4, 4, TT]); din("gbias", [128, 4]); din("w_out", [512, 1024])
    din("perm", [NCH, NCH]); din("triu", [NCH, NCH]); din("mask", [128, 2, 128]); din("ident", [128, 128])
    D["hdir0"] = nc.dram_tensor("hdir0", [TLAT, 512], F32, kind="Internal").ap()
    D["hdir1"] = nc.dram_tensor("hdir1", [TLAT, 512], F32, kind="Internal").ap()
    dout("part", [TLAT, 1024])
    K = emit_consts(P, D)
    p2_consts(P, K, D)
    G = [emit_gates(P, K, D, d) for d in range(2)]
    h_toks = emit_scan(P, K, D, G)
    outs = emit_outstage(P, K, D, h_toks)
    P.op("sp", None, r=outs)
    P.finalize()
    return nc, P


POOLW = (2, 4, 8, 16)
DELTAS = ((-1, 0), (-1, 0, 1), (-2, -1, 0, 1, 2), (-4, -3, -2, -1, 0, 1, 2, 3, 4))
BOFF = (0, 2, 5, 10)
NWT = 24


def host_bn(q):
    BN = np.zeros((16, 19, 128, 128), np.float32)
    ar = np.arange(128)
    for j in range(16):
        gt = q * 16 + j
        blk = 0
        for g, w in enumerate(POOLW):
            lo_off, hi_off = -(w // 2), w - 1 - w // 2
            for dl in DELTAS[g]:
                it = gt + dl
                M = BN[j, blk]
                if 0 <= it < 64:
                    for tl in range(128):
                        r = 2 * gt + tl // 64
                        c = tl % 64
                        r_lo = max(r + lo_off, 0); r_hi = min(r + hi_off, 127)
                        c_lo = max(c + lo_off, 0); c_hi = min(c + hi_off, 63)
                        coef = 1.0 / ((r_hi - r_lo + 1) * (c_hi - c_lo + 1))
                        for rr in (2 * it, 2 * it + 1):
                            if r_lo <= rr <= r_hi:
                                tp0 = (rr - 2 * it) * 64
                                M[tp0 + c_lo:tp0 + c_hi + 1, tl] = coef
                    if dl == 0:
                        M[ar, ar] -= 1.0
                blk += 1
    return BN.astype(ml_dtypes.bfloat16)


def emit_p3_ada(P, K, D):
    g0m = [P.tile("g0m", [128, 1024])]
    m1 = [P.tile("m1", [128, 3072])]
    P.push_scope()
    wsl = Ring(P, "adaw", 2, [128, 8, 256])
    wslots = [(t[:], tk) for t, tk in wsl.items]
    scr, T_scr = P.tile("screp", [128, 1024])
    gv, T_gv = P.tile("gvb", [128, 1024])
    emit_ada(P, K, D["cvec"], 1, D["w_ada0"], D["b_ada0"], 2048, 1024, g0m, wslots, (scr[:], T_scr))
    emit_ada(P, K, D["cvec"], 1, D["w_ada1"], D["b_ada1"], 0, 3072, m1, wslots, (scr[:], T_scr))
    gg0, T_gg0 = g0m[0]
    mm1, T_m1 = m1[0]
    for key, dst, T_dst, sl, addone in (("g_post0", gg0, T_gg0, slice(0, 1024), False), ("g_pre1", mm1, T_m1, slice(1024, 2048), True),
                                        ("g_post1", mm1, T_m1, slice(2048, 3072), False)):
        P.dma(gv[:], D[key].partition_broadcast(128), w=[T_gv], tok=T_gv)
        if addone:
            P.op("dve", lambda e, dst=dst, sl=sl: e.scalar_tensor_tensor(out=dst[:, sl], in0=dst[:, sl], scalar=1.0, in1=gv[:], op0=ALU.add, op1=ALU.mult),
                 r=[T_dst, T_gv], w=[T_dst])
        else:
            P.op("dve", lambda e, dst=dst, sl=sl: e.tensor_tensor(out=dst[:, sl], in0=dst[:, sl], in1=gv[:], op=ALU.mult), r=[T_dst, T_gv], w=[T_dst])
    P.pop_scope()
    return g0m, m1


def emit_p3(P, K, D, fused=False, T_Yg=None, ada=None):
    if ada is None:
        ada = emit_p3_ada(P, K, D)
    g0m, m1 = ada
    rings = dict(st=Ring(P, "n_st", 6, [128, 4]), junk=Ring(P, "n_junk", 1, [128, 1024], BF16),
                 tmp=Ring(P, "n_tmp", 2, [128, 1024]), hb=Ring(P, "n_hb", 3, [128, 1024], BF16))
    P.push_scope()
    wu, T_wu = P.tile("wu", [128, 8, 2048], BF16)
    wz, T_wz = P.tile("wz", [128, 8, 2048], BF16)
    if fused:
        wo0, T_wo0 = P.tile("wo0", [128, 16, 1024], BF16)
    P.push_scope()
    wsl = Ring(P, "adaw2", 2, [128, 8, 256])
    wslots = [(t[:], tk) for t, tk in wsl.items]
    gg0, T_gg0 = g0m[0]
    mm1, T_m1 = m1[0]
    sh1 = mm1[:, 0:1024]; gs1 = mm1[:, 1024:2048]; gg1 = mm1[:, 2048:3072]
    wview = D["w_in1"].rearrange("(dc p) c -> p dc c", p=128)
    i = 0
    for dst, T_dst, cbase in ((wu, T_wu, 0), (wz, T_wz, 2048)):
        for cb in range(8):
            wt, T_wt = wslots[i % 2]
            P.dma(wt, wview[:, :, cbase + cb * 256:cbase + (cb + 1) * 256], w=[T_wt], tok=T_wt)
            if i % 2 == 0:
                P.op("act", lambda e, dst=dst, cb=cb, wt=wt: e.copy(out=dst[:, :, cb * 256:(cb + 1) * 256], in_=wt), r=[T_wt], w=[T_dst])
            else:
                P.op("pool", lambda e, dst=dst, cb=cb, wt=wt: e.tensor_copy(out=dst[:, :, cb * 256:(cb + 1) * 256], in_=wt), r=[T_wt], w=[T_dst])
            i += 1
    if fused:
        wov0 = D["w_out0"].rearrange("(fc p) c -> p fc c", p=128)
        for fc2 in range(8):
            wt, T_wt = wslots[fc2 % 2]
            wtv = wt.rearrange("p a b -> p (a b)").rearrange("p (f c) -> p f c", f=2)
            P.dma(wtv, wov0[:, fc2 * 2:(fc2 + 1) * 2, :], w=[T_wt], tok=T_wt)
            if fc2 % 2 == 0:
                P.op("act", lambda e, fc2=fc2, wtv=wtv: e.copy(out=wo0[:, fc2 * 2:(fc2 + 1) * 2, :], in_=wtv), r=[T_wt], w=[T_wo0])
            else:
                P.op("pool", lambda e, fc2=fc2, wtv=wtv: e.tensor_copy(out=wo0[:, fc2 * 2:(fc2 + 1) * 2, :], in_=wtv), r=[T_wt], w=[T_wo0])
    P.pop_scope()
    P.push_scope()
    xr = Ring(P, "xw", 2, [128, 1024])
    if fused:
        ytr = Ring(P, "ytl", 3, [128, 4, 512], BF16)
        yTr = Ring(P, "yTw", 2, [128, 16, 128], BF16)
        ysr = Ring(P, "ysum", 2, [128, 1024])
        Ygv = D["Yg"].rearrange("k r c -> (k r) c")
    else:
        pr = Ring(P, "pw", 12, [128, 1024])
    x1r = Ring(P, "x1", 2, [128, 1024])
    hTr = Ring(P, "h1T", 2, [128, 8, 512], BF16)
    ustr = Ring(P, "ust", 2, [128, 2048], BF16)
    zsr = Ring(P, "zst", 3, [128, 512], BF16)
    sgr = Ring(P, "zsg", 2, [128, 512])
    T_us = [Tok("us%d" % i) for i in range(NWT)]
    T_x1s = [Tok("x1s%d" % i) for i in range(16)]
    T_sz = [[Tok("sz%d_%d" % (tb, f)) for f in range(16)] for tb in range(4)]
    ST = {}
    ST2 = {}
    hTcur = {}

    def GA(wt_i):
        yt, T_yt = ytr.next()
        for r_ in range(4):
            P.op("pool", lambda e, yt=yt, r_=r_, wt_i=wt_i: e.indirect_dma_start(out=yt[:, r_, :], out_offset=None, in_=Ygv,
                                                                               in_offset=bass.IndirectOffsetOnAxis(ap=K["yidx"][:, wt_i * 4 + r_:wt_i * 4 + r_ + 1], axis=0)),
                 r=[T_Yg, K["T_yidx"]], w=[T_yt], dma=T_yt)
        ST[wt_i] = dict(yt=yt, T_yt=T_yt)

    def SA(wt_i):
        rows = slice(wt_i * 128, (wt_i + 1) * 128)
        X = ST.setdefault(wt_i, {})
        xt, T_xt = xr.next()
        P.dma(xt[:], D["xw"][rows, :], w=[T_xt], tok=T_xt)
        X["xt"], X["T_xt"] = xt, T_xt
        if fused:
            yt, T_yt = X["yt"], X["T_yt"]
            yTw, T_yTw = yTr.next()
            for hb_ in range(2):
                bk, T_bk = P.bank()
                bkb = bk[:].bitcast(BF16).rearrange("p (a b) -> p a b", a=8)
                for i8 in range(8):
                    fch = hb_ * 8 + i8
                    P.op("pe", lambda e, bkb=bkb, i8=i8, fch=fch, yt=yt: e.transpose(out=bkb[:, i8, :], in_=yt[:, fch // 4, (fch % 4) * 128:(fch % 4 + 1) * 128],
                                                                                    identity=K["idb"][:]), r=[T_yt, K["T_idb"]], w=[T_bk])
                if hb_ == 0:
                    P.op("act", lambda e, bkb=bkb, yTw=yTw: e.copy(out=yTw[:, 0:8, :], in_=bkb), r=[T_bk], w=[T_yTw])
                else:
                    P.op("dve", lambda e, bkb=bkb, yTw=yTw: e.tensor_copy(out=yTw[:, 8:16, :], in_=bkb), r=[T_bk], w=[T_yTw])
            p0, T0 = ysr.next()
            for hf in range(2):
                bk, T_bk = P.bank()
                for fch in range(16):
                    P.op("pe", lambda e, bk=bk, fch=fch, hf=hf, yTw=yTw: e.matmul(bk[:], lhsT=yTw[:, fch, :], rhs=wo0[:, fch, hf * 512:(hf + 1) * 512],
                                                                                 start=(fch == 0), stop=(fch == 15)), r=[T_yTw, T_wo0], w=[T_bk])
                if hf == 0:
                    P.op("act", lambda e, bk=bk, p0=p0: e.copy(out=p0[:, 0:512], in_=bk[:]), r=[T_bk], w=[T0])
                else:
                    P.op("dve", lambda e, bk=bk, p0=p0: e.tensor_copy(out=p0[:, 512:1024], in_=bk[:]), r=[T_bk], w=[T0])
        else:
            ps_ = []
            for s_ in range(4):
                pt, T_pt = pr.next()
                P.dma(pt[:], D["parts"][s_, rows, :], w=[T_pt], tok=T_pt)
                ps_.append((pt, T_pt))
            (p0, T0), (p1, T1), (p2, T2), (p3, T3) = ps_
            P.op("dve", lambda e, p0=p0, p1=p1: e.tensor_tensor(out=p0[:], in0=p0[:], in1=p1[:], op=ALU.add), r=[T0, T1], w=[T0])
            P.op("pool", lambda e, p2=p2, p3=p3: e.tensor_tensor(out=p2[:], in0=p2[:], in1=p3[:], op=ALU.add), r=[T2, T3], w=[T2])
            P.op("dve", lambda e, p0=p0, p2=p2: e.tensor_tensor(out=p0[:], in0=p0[:], in1=p2[:], op=ALU.add), r=[T0, T2], w=[T0])
        X["p0"], X["T0"] = p0, T0

    def SB(wt_i):
        wb, t = divmod(wt_i, 4)
        own = 1 <= wb <= 4
        rows = slice(wt_i * 128, (wt_i + 1) * 128)
        X = ST.pop(wt_i)
        xt, T_xt, p0, T0 = X["xt"], X["T_xt"], X["p0"], X["T0"]
        if t == 0:
            hTcur[wb] = hTr.next()
        hT, T_hT = hTcur[wb]
        st, T_st = rings["st"].next()
        junk, T_junk = rings["junk"].next()
        P.op("act", lambda e, p0=p0, st=st, junk=junk: e.activation(out=junk[:], in_=p0[:], func=AF.Square, accum_out=st[:, 0:1]), r=[T0], w=[T_junk, T_st])
        P.op("dve", lambda e, st=st: e.tensor_scalar(out=st[:, 1:2], in0=st[:, 0:1], scalar1=1.0 / 1024, scalar2=EPS, op0=ALU.mult, op1=ALU.add), r=[T_st], w=[T_st])
        P.op("pool", lambda e, st=st: e.tensor_tensor(out=st[:, 2:3], in0=st[:, 1:2], in1=K["nh"][:], op=ALU.pow), r=[T_st, K["T_nh"]], w=[T_st])
        x1, T_x1 = x1r.next()
        P.op("dve", lambda e, p0=p0, st=st: e.scalar_tensor_tensor(out=p0[:], in0=p0[:], scalar=st[:, 2:3], in1=gg0[:], op0=ALU.mult, op1=ALU.mult),
             r=[T0, T_st, T_gg0], w=[T0])
        P.op("dve", lambda e, p0=p0, xt=xt, x1=x1: e.tensor_tensor(out=x1[:], in0=p0[:], in1=xt[:], op=ALU.add), r=[T0, T_xt], w=[T_x1])
        if own:
            oi = wt_i - 4
            P.dma(D["x1s"][oi * 128:(oi + 1) * 128, :], x1[:], r=[T_x1], w=[T_x1s[oi]], tok=T_x1)
        hb, T_hb = emit_norm_A(P, K, x1, T_x1, gs1, T_m1, sh1, T_m1, rings)
        ST2[wt_i] = (hb, T_hb, hT, T_hT)

    def SB2(wt_i):
        wb, t = divmod(wt_i, 4)
        rows = slice(wt_i * 128, (wt_i + 1) * 128)
        hb, T_hb, hT, T_hT = ST2.pop(wt_i)
        emit_norm_B(P, K, hb, T_hb, hT[:, :, t * 128:(t + 1) * 128], T_hT)
        ust, T_ust = ustr.next()
        for g in range(4):
            bk, T_bk = P.bank()
            for dc in range(8):
                P.op("pe", lambda e, g=g, dc=dc, bk=bk, t=t, hT=hT: e.matmul(bk[:], lhsT=hT[:, dc, t * 128:(t + 1) * 128], rhs=wu[:, dc, g * 512:(g + 1) * 512],
                                                                            start=(dc == 0), stop=(dc == 7)), r=[T_hT, T_wu], w=[T_bk])
            if g % 2 == 0:
                P.op("act", lambda e, g=g, bk=bk, ust=ust: e.copy(out=ust[:, g * 512:(g + 1) * 512], in_=bk[:]), r=[T_bk], w=[T_ust])
            else:
                P.op("dve", lambda e, g=g, bk=bk, ust=ust: e.tensor_copy(out=ust[:, g * 512:(g + 1) * 512], in_=bk[:]), r=[T_bk], w=[T_ust])
        P.dma(D["u_s"][rows, :], ust[:], r=[T_ust], w=[T_us[wt_i]], tok=T_ust)

    def Z(wb):
        hT, T_hT = hTcur[wb]
        tb = wb - 1
        for fz in range(16):
            bk, T_bk = P.bank()
            for dc in range(8):
                P.op("pe", lambda e, fz=fz, dc=dc, bk=bk, hT=hT: e.matmul(bk[:], lhsT=wz[:, dc, fz * 128:(fz + 1) * 128], rhs=hT[:, dc, :], start=(dc == 0), stop=(dc == 7)),
                     r=[T_wz, T_hT], w=[T_bk])
            sg, T_sg = sgr.next()
            zs, T_zs = zsr.next()
            P.op("act", lambda e, bk=bk, sg=sg: e.activation(out=sg[:], in_=bk[:], func=AF.Sigmoid), r=[T_bk], w=[T_sg])
            P.op("dve", lambda e, bk=bk, sg=sg, zs=zs: e.tensor_tensor(out=zs[:], in0=bk[:], in1=sg[:], op=ALU.mult), r=[T_bk, T_sg], w=[T_zs])
            P.dma(D["szT"][fz * 128:(fz + 1) * 128, tb * 512:(tb + 1) * 512], zs[:], r=[T_zs], w=[T_sz[tb][fz]], tok=T_zs)

    for i in range(-3, NWT):
        if fused and 0 <= i + 3 < NWT:
            GA(i + 3)
        if 0 <= i + 2 < NWT:
            SA(i + 2)
        if 0 <= i + 1 < NWT:
            SB(i + 1)
        if i >= 0:
            SB2(i)
            if i % 4 == 3 and 1 <= i // 4 <= 4:
                Z(i // 4)
    P.pop_scope()
    P.pop_scope()
    vT, T_vT = P.tile("vT", [128, 16, 2048], BF16)
    wo, T_wo = P.tile("wo1", [128, 16, 1024], BF16)
    P.push_scope()
    wov = D["w_out1"].rearrange("(fc p) c -> p fc c", p=128)
    wost = Ring(P, "wo1st", 2, [128, 1024])
    for fc in range(16):
        wt, T_wt = wost.next()
        P.dma(wt[:], wov[:, fc, :], w=[T_wt], tok=T_wt)
        P.op("pool", lambda e, fc=fc, wt=wt: e.tensor_copy(out=wo[:, fc, :], in_=wt[:]), r=[T_wt], w=[T_wo])
    psc, T_psc = P.tile("psc", [128, 16])
    P.dma(psc[:], D["pscale"], w=[T_psc], tok=T_psc)
    ugr = Ring(P, "ug", 1, [128, NWT, 512], BF16)
    dTg, T_dTg = P.tile("dTg", [128, 4, 2048], BF16)
    wpst = Ring(P, "wpst", 1, [128, 4, 512])
    wpr = Ring(P, "wp", 2, [128, 4, 512], BF16)
    bnr = Ring(P, "bn", 3, [128, 9, 128], BF16)
    szr = Ring(P, "szt", 3, [128, 512], BF16)
    usv = D["u_s"].rearrange("(w p) f -> p w f", p=128)
    for g in range(4):
        nd = len(DELTAS[g])
        ug, T_ug = ugr.next()
        P.dma(ug[:], usv[:, :, g * 512:(g + 1) * 512], r=T_us, w=[T_ug], tok=T_ug)
        wps, T_wps = wpst.next()
        wp, T_wp = wpr.next()
        P.dma(wps[:], D["w_pool"][g].rearrange("(fic p) fo -> p fic fo", p=128), w=[T_wps], tok=T_wps)
        P.op("pool", lambda e, wps=wps, wp=wp: e.tensor_copy(out=wp[:], in_=wps[:]), r=[T_wps], w=[T_wp])
        bns = {}

        def ldbn(j):
            bn, T_bn = bnr.next()
            P.dma(bn[:, 0:nd, :], D["BN"][j, BOFF[g]:BOFF[g] + nd].rearrange("k a b -> a k b"), w=[T_bn], tok=T_bn)
            bns[j] = (bn, T_bn)

        ldbn(0)
        for j in range(16):
            if j + 1 < 16:
                ldbn(j + 1)
            bn, T_bn = bns.pop(j)
            bk, T_bk = P.bank()
            for fc in range(4):
                for di, dl in enumerate(DELTAS[g]):
                    P.op("pe", lambda e, fc=fc, di=di, dl=dl, bk=bk, bn=bn, ug=ug, j=j: e.matmul(bk[:, fc * 128:(fc + 1) * 128], lhsT=ug[:, 4 + j + dl, fc * 128:(fc + 1) * 128],
                                                                                              rhs=bn[:, di, :], start=(di == 0), stop=(di == nd - 1)),
                         r=[T_ug, T_bn], w=[T_bk])
            if j % 2 == 0:
                P.op("act", lambda e, bk=bk, j=j: e.copy(out=dTg[:, :, j * 128:(j + 1) * 128], in_=bk[:].rearrange("p (a b) -> p a b", a=4)), r=[T_bk], w=[T_dTg])
            else:
                P.op("dve", lambda e, bk=bk, j=j: e.tensor_copy(out=dTg[:, :, j * 128:(j + 1) * 128], in_=bk[:].rearrange("p (a b) -> p a b", a=4)), r=[T_bk], w=[T_dTg])
        szs = {}

        def ldsz(idx):
            tb, fo = divmod(idx, 4)
            fch = g * 4 + fo
            szt, T_szt = szr.next()
            P.dma(szt[:], D["szT"][fch * 128:(fch + 1) * 128, tb * 512:(tb + 1) * 512], r=[T_sz[tb][fch]], w=[T_szt], tok=T_szt)
            szs[idx] = (szt, T_szt)

        ldsz(0)
        for tb in range(4):
            for fo in range(4):
                fch = g * 4 + fo
                if tb * 4 + fo + 1 < 16:
                    ldsz(tb * 4 + fo + 1)
                szt, T_szt = szs.pop(tb * 4 + fo)
                bk, T_bk = P.bank()
                for fic in range(4):
                    P.op("pe", lambda e, fic=fic, fo=fo, tb=tb, bk=bk, wp=wp: e.matmul(bk[:], lhsT=wp[:, fic, fo * 128:(fo + 1) * 128], rhs=dTg[:, fic, tb * 512:(tb + 1) * 512],
                                                                                      start=(fic == 0), stop=(fic == 3)), r=[T_wp, T_dTg], w=[T_bk])
                P.op("dve", lambda e, bk=bk, fch=fch, tb=tb, szt=szt: e.scalar_tensor_tensor(out=vT[:, fch, tb * 512:(tb + 1) * 512], in0=bk[:], scalar=psc[:, fch:fch + 1], in1=szt[:],
                                                                                            op0=ALU.mult, op1=ALU.mult), r=[T_bk, T_psc, T_szt], w=[T_vT])
    P.pop_scope()
    P.push_scope()
    y2r = Ring(P, "y2", 2, [128, 1024])
    x1lr = Ring(P, "x1l", 3, [128, 1024])
    outs = []
    x1ls = {}

    def ldx1(j):
        x1l, T_x1l = x1lr.next()
        P.dma(x1l[:], D["x1s"][j * 128:(j + 1) * 128, :], r=[T_x1s[j]], w=[T_x1l], tok=T_x1l)
        x1ls[j] = (x1l, T_x1l)

    ldx1(0)
    for j in range(16):
        if j + 1 < 16:
            ldx1(j + 1)
        y2, T_y2 = y2r.next()
        for hf in range(2):
            bk, T_bk = P.bank()
            for fch in range(16):
                P.op("pe", lambda e, fch=fch, hf=hf, bk=bk, j=j: e.matmul(bk[:], lhsT=vT[:, fch, j * 128:(j + 1) * 128], rhs=wo[:, fch, hf * 512:(hf + 1) * 512],
                                                                        start=(fch == 0), stop=(fch == 15)), r=[T_vT, T_wo], w=[T_bk])
            if hf == 0:
                P.op("act", lambda e, bk=bk, y2=y2: e.copy(out=y2[:, 0:512], in_=bk[:]), r=[T_bk], w=[T_y2])
            else:
                P.op("dve", lambda e, bk=bk, y2=y2: e.tensor_copy(out=y2[:, 512:1024], in_=bk[:]), r=[T_bk], w=[T_y2])
        x1l, T_x1l = x1ls.pop(j)
        st, T_st = rings["st"].next()
        junk, T_junk = rings["junk"].next()
        P.op("act", lambda e, y2=y2, st=st, junk=junk: e.activation(out=junk[:], in_=y2[:], func=AF.Square, accum_out=st[:, 0:1]), r=[T_y2], w=[T_junk, T_st])
        P.op("dve", lambda e, st=st: e.tensor_scalar(out=st[:, 1:2], in0=st[:, 0:1], scalar1=1.0 / 1024, scalar2=EPS, op0=ALU.mult, op1=ALU.add), r=[T_st], w=[T_st])
        P.op("pool", lambda e, st=st: e.tensor_tensor(out=st[:, 2:3], in0=st[:, 1:2], in1=K["nh"][:], op=ALU.pow), r=[T_st, K["T_nh"]], w=[T_st])
        P.op("dve", lambda e, y2=y2, st=st: e.scalar_tensor_tensor(out=y2[:], in0=y2[:], scalar=st[:, 2:3], in1=gg1, op0=ALU.mult, op1=ALU.mult), r=[T_y2, T_st, T_m1], w=[T_y2])
        P.op("pool", lambda e, y2=y2, x1l=x1l: e.tensor_tensor(out=y2[:], in0=y2[:], in1=x1l[:], op=ALU.add), r=[T_y2, T_x1l], w=[T_y2])
        T_o = Tok("out_o")
        outs.append(T_o)
        P.dma(D["out"][j * 128:(j + 1) * 128, :], y2[:], r=[T_y2], w=[T_o], tok=T_y2)
    P.pop_scope()
    return outs


def host_p3_inputs(inp, b, q, parts_b):
    f32 = np.float32
    d = {}
    lo = q * 2048 - 512
    xw = np.zeros((NWT * 128, 1024), f32)
    pw = np.zeros((4, NWT * 128, 1024), f32)
    a = max(lo, 0); e_ = min(lo + NWT * 128, TLAT)
    xw[a - lo:e_ - lo] = inp["x"][b][a:e_]
    pw[:, a - lo:e_ - lo] = parts_b[:, a:e_]
    d["xw"] = xw
    d["parts"] = pw
    d["cvec"] = np.ascontiguousarray(inp["c"][b].reshape(8, 128, 1).transpose(1, 0, 2))
    d["w_ada0"] = np.ascontiguousarray(inp["w_ada"][0]); d["b_ada0"] = np.ascontiguousarray(inp["b_ada"][0])
    d["w_ada1"] = np.ascontiguousarray(inp["w_ada"][1]); d["b_ada1"] = np.ascontiguousarray(inp["b_ada"][1])
    d["g_post0"] = np.ascontiguousarray(inp["a_norm_post"][0]); d["g_pre1"] = np.ascontiguousarray(inp["b_norm_pre"][0])
    d["g_post1"] = np.ascontiguousarray(inp["b_norm_post"][0])
    d["w_in1"] = np.ascontiguousarray(inp["b_w_in"][0]); d["w_pool"] = np.ascontiguousarray(inp["b_w_pool"][0])
    d["pscale"] = np.ascontiguousarray(inp["b_pool_scale"][0].reshape(16, 128).T)
    d["w_out1"] = np.ascontiguousarray(inp["b_w_out"][0])
    d["BN"] = host_bn(q)
    d["ident"] = np.eye(128, dtype=f32)
    return d


def build_p3(debug=False):
    nc = bass.Bass("TRN2", target_bir_lowering=False)
    P = Prog(nc)
    P.init_banks(8)
    D = {}

    def din(name, shape, dt=F32):
        D[name] = nc.dram_tensor(name, list(shape), dt, kind="ExternalInput").ap()

    din("xw", [NWT * 128, 1024]); din("parts", [4, NWT * 128, 1024]); din("cvec", [128, 8, 1])
    din("w_ada0", [1024, 3072]); din("b_ada0", [3072]); din("w_ada1", [1024, 3072]); din("b_ada1", [3072])
    din("g_post0", [1024]); din("g_pre1", [1024]); din("g_post1", [1024])
    din("w_in1", [1024, 4096]); din("w_pool", [4, 512, 512]); din("pscale", [128, 16]); din("w_out1", [2048, 1024])
    din("BN", [16, 19, 128, 128], BF16); din("ident", [128, 128])
    kd = "ExternalOutput" if debug else "Internal"
    D["x1s"] = nc.dram_tensor("x1s", [2048, 1024], F32, kind=kd).ap()
    D["u_s"] = nc.dram_tensor("u_s", [NWT * 128, 2048], BF16, kind=kd).ap()
    D["szT"] = nc.dram_tensor("szT", [2048, 2048], BF16, kind=kd).ap()
    D["out"] = nc.dram_tensor("out", [2048, 1024], F32, kind="ExternalOutput").ap()
    K = emit_consts(P, D)
    outs = emit_p3(P, K, D)
    P.op("sp", None, r=outs)
    P.finalize()
    return nc, P


I32 = mybir.dt.int32
RG4 = [[0, 1, 2, 3], [4, 5, 6, 7]]


def emit_outstage_f(P, K, D, h_toks, TK):
    NR = 6
    hfr = Ring(P, "hf", NR, [128, 512])
    hbr = Ring(P, "hb", NR, [128, 512])
    Ar = Ring(P, "Ain", NR, [128, 512])
    Br = Ring(P, "Bin", NR, [128, 512])
    junk, T_junk = P.tile("ojunk", [128, 512], BF16)
    str_ = Ring(P, "ost", 5, [128, 8])
    ybr = Ring(P, "yb", 3, [128, 512], BF16)
    T_yc = [Tok("yc%d" % k) for k in range(8)]
    T_Yg = Tok("Yg")
    T_cc = Tok("ccY")
    L = {}

    def loads(cl):
        hf, T_hf = hfr.next(); hb, T_hb = hbr.next(); At, T_At = Ar.next(); Bt, T_Bt = Br.next()
        rows = slice(cl * 128, (cl + 1) * 128)
        P.dma(hf[:], D["hdir0"][rows, :], r=[h_toks[0][cl]], w=[T_hf], tok=T_hf)
        P.dma(hb[:], D["hdir1"][rows, :], r=[h_toks[1][cl]], w=[T_hb], tok=T_hb)
        P.dma(At[:], D["A"][rows, :], r=[TK["A"]], w=[T_At], tok=T_At)
        P.dma(Bt[:], D["Bc"][rows, :], r=[TK["Bc"]], w=[T_Bt], tok=T_Bt)
        L[cl] = (hf, T_hf, hb, T_hb, At, T_At, Bt, T_Bt)

    def st1(cl):
        hf, T_hf, hb, T_hb, At, T_At, Bt, T_Bt = L[cl]
        st, T_st = str_.next()
        L[cl] = L[cl] + (st, T_st)
        P.op("dve", lambda e, hf=hf, hb=hb: e.tensor_tensor(out=hf[:], in0=hf[:], in1=hb[:], op=ALU.add), r=[T_hf, T_hb], w=[T_hf])
        P.op("act", lambda e, hf=hf, st=st: e.activation(out=junk[:], in_=hf[:], func=AF.Copy, accum_out=st[:, 0:1]), r=[T_hf], w=[T_junk, T_st])
        P.op("act", lambda e, hf=hf, st=st: e.activation(out=junk[:], in_=hf[:], func=AF.Square, accum_out=st[:, 1:2]), r=[T_hf], w=[T_junk, T_st])

    def st2(cl):
        st, T_st = L[cl][8], L[cl][9]
        P.op("dve", lambda e, st=st: e.tensor_scalar(out=st[:, 2:3], in0=st[:, 0:1], scalar1=1.0 / 512, scalar2=None, op0=ALU.mult), r=[T_st], w=[T_st])
        P.op("dve", lambda e, st=st: e.tensor_tensor(out=st[:, 3:4], in0=st[:, 2:3], in1=st[:, 2:3], op=ALU.mult), r=[T_st], w=[T_st])
        P.op("dve", lambda e, st=st: e.scalar_tensor_tensor(out=st[:, 4:5], in0=st[:, 1:2], scalar=1.0 / 512, in1=st[:, 3:4], op0=ALU.mult, op1=ALU.subtract),
             r=[T_st], w=[T_st])
        P.op("dve", lambda e, st=st: e.tensor_scalar(out=st[:, 4:5], in0=st[:, 4:5], scalar1=EPS, scalar2=None, op0=ALU.add), r=[T_st], w=[T_st])
        P.op("pool", lambda e, st=st: e.tensor_tensor(out=st[:, 5:6], in0=st[:, 4:5], in1=K["nh"][:], op=ALU.pow), r=[T_st, K["T_nh"]], w=[T_st])

    def st3(cl):
        hf, T_hf, hb, T_hb, At, T_At, Bt, T_Bt, st, T_st = L.pop(cl)
        P.op("dve", lambda e, hf=hf, st=st: e.tensor_scalar(out=hf[:], in0=hf[:], scalar1=st[:, 2:3], scalar2=st[:, 5:6], op0=ALU.subtract, op1=ALU.mult),
             r=[T_hf, T_st], w=[T_hf])
        P.op("dve", lambda e, hf=hf, At=At: e.tensor_tensor(out=hf[:], in0=hf[:], in1=At[:], op=ALU.mult), r=[T_hf, T_At], w=[T_hf])
        yb, T_yb = ybr.next()
        P.op("dve", lambda e, hf=hf, Bt=Bt, yb=yb: e.tensor_tensor(out=yb[:], in0=hf[:], in1=Bt[:], op=ALU.add), r=[T_hf, T_Bt], w=[T_yb])
        k = cl // 8
        P.dma(D["ycin%d" % k][(cl % 8) * 128:(cl % 8 + 1) * 128, :], yb[:], r=[T_yb], w=[T_yc[k]], tok=T_yb)
        if cl % 8 == 7:
            P.op("pool", lambda e, k=k: e.collective_compute("AllGather", ALU.bypass, replica_groups=RG4, ins=[D["ycin%d" % k]], outs=[D["Yg"][k]]),
                 r=[T_yc[k]], w=[T_Yg], dma=T_cc, inc=1)

    for cl in range(-4, 64):
        if 0 <= cl + 4 < 64:
            loads(cl + 4)
        if 0 <= cl + 2 < 64:
            st1(cl + 2)
        if 0 <= cl + 1 < 64:
            st2(cl + 1)
        if cl >= 0:
            st3(cl)
    return T_Yg


def host_fused_inputs(inp, b, h):
    d = host_p1_inputs(inp, b, h)
    d["cvec2"] = d.pop("cvec")
    d["w_ada0"] = d.pop("w_ada")
    d["b_ada0"] = d.pop("b_ada")
    d.update(host_p2_consts(inp, h))
    q = h
    lo = q * 2048 - 512
    xw = np.zeros((NWT * 128, 1024), np.float32)
    a = max(lo, 0); e_ = min(lo + NWT * 128, TLAT)
    xw[a - lo:e_ - lo] = inp["x"][b][a:e_]
    d["xw"] = xw
    d["cvec1"] = np.ascontiguousarray(inp["c"][b].reshape(8, 128, 1).transpose(1, 0, 2))
    d["w_ada1"] = np.ascontiguousarray(inp["w_ada"][1]); d["b_ada1"] = np.ascontiguousarray(inp["b_ada"][1])
    d["g_post0"] = np.ascontiguousarray(inp["a_norm_post"][0]); d["g_pre1"] = np.ascontiguousarray(inp["b_norm_pre"][0])
    d["g_post1"] = np.ascontiguousarray(inp["b_norm_post"][0])
    d["w_in1"] = np.ascontiguousarray(inp["b_w_in"][0]); d["w_pool"] = np.ascontiguousarray(inp["b_w_pool"][0])
    d["pscale"] = np.ascontiguousarray(inp["b_pool_scale"][0].reshape(16, 128).T)
    d["w_out1"] = np.ascontiguousarray(inp["b_w_out"][0])
    d["w_out0"] = np.ascontiguousarray(inp["a_w_out"][0])
    d["BN"] = host_bn(q)
    gidx = np.zeros((128, 16), np.int32)
    cols = (h, 4 + h, 8 + h, 12 + h)
    p = np.arange(NCH)
    for r in range(4):
        for s in range(4):
            gidx[:NCH, r * 4 + s] = (s * 16 + cols[r]) * NCH + p
    d["gidx"] = gidx
    yidx = np.zeros((128, NWT * 4), np.int32)
    pp = np.arange(128)
    for w in range(NWT):
        t = np.clip(lo + 128 * w + pp, 0, TLAT - 1)
        for r in range(4):
            yidx[:, w * 4 + r] = (t // 1024) * 4096 + r * 1024 + (t % 1024)
    d["yidx"] = yidx
    return d


def build_fused():
    nc = bass.Bass("TRN2", target_bir_lowering=False)
    P = Prog(nc)
    P.init_banks(8)
    D = {}

    def din(name, shape, dt=F32):
        D[name] = nc.dram_tensor(name, list(shape), dt, kind="ExternalInput").ap()

    def dint(name, shape, dt=F32):
        D[name] = nc.dram_tensor(name, list(shape), dt, kind="Internal").ap()

    din("xin", [TT, 1024]); din("cvec2", [128, 8, 2]); din("w_ada0", [1024, 3072]); din("b_ada0", [3072]); din("g_pre", [1024])
    din("w_in", [1024, 1536]); din("convw", [128, 4, 3]); din("convb", [128, 4]); din("skip", [128, 4]); din("hn", [512])
    din("BD", [128, 3, 4, 128]); din("BDT", [128, 3, 4, 128]); din("wg", [128, 3, 4, 16]); din("ident", [128, 128])
    din("gbias", [128, 4]); din("perm", [NCH, NCH]); din("triu", [NCH, NCH]); din("mask", [128, 2, 128])
    din("gidx", [128, 16], I32); din("yidx", [128, NWT * 4], I32)
    din("xw", [NWT * 128, 1024]); din("cvec1", [128, 8, 1]); din("w_ada1", [1024, 3072]); din("b_ada1", [3072])
    din("g_post0", [1024]); din("g_pre1", [1024]); din("g_post1", [1024])
    din("w_in1", [1024, 4096]); din("w_pool", [4, 512, 512]); din("pscale", [128, 16]); din("w_out1", [2048, 1024]); din("w_out0", [2048, 1024])
    din("BN", [16, 19, 128, 128], BF16)
    dint("qT", [512, TT], BF16); dint("kT", [512, TT], BF16); dint("ktok", [TT, 512], BF16); dint("vtok", [TT, 512], BF16)
    dint("A", [TLAT, 512]); dint("Bc", [TLAT, 512]); dint("gpT", [16, TT]); dint("GP", [64, TT])
    dint("hdir0", [TLAT, 512]); dint("hdir1", [TLAT, 512])
    for k in range(8):
        dint("ycin%d" % k, [1024, 512], BF16)
    dint("Yg", [8, 4096, 512], BF16)
    dint("x1s", [2048, 1024]); dint("u_s", [NWT * 128, 2048], BF16); dint("szT", [2048, 2048], BF16)
    D["out"] = nc.dram_tensor("out", [2048, 1024], F32, kind="ExternalOutput").ap()

    K = emit_consts(P, D)
    p2_consts(P, K, D)
    for name, shape in (("gidx", [128, 16]), ("yidx", [128, NWT * 4])):
        K[name], K["T_" + name] = P.tile("k_" + name, shape, I32)
        P.dma(K[name][:], D[name], w=[K["T_" + name]], tok=K["T_" + name])
    TK = {k: Tok("o_" + k) for k in ("qT", "kT", "ktok", "vtok", "A", "Bc", "gpT")}
    P.push_scope()
    D1 = dict(D, cvec=D["cvec2"], w_ada=D["w_ada0"], b_ada=D["b_ada0"])
    emit_p1(P, K, D1, TK)
    P.pop_scope()
    T_GP = Tok("GP")
    T_ccg = Tok("ccG")
    P.op("pool", lambda e: e.collective_compute("AllGather", ALU.bypass, replica_groups=RG4, ins=[D["gpT"]], outs=[D["GP"]]),
         r=[TK["gpT"]], w=[T_GP], dma=T_ccg, inc=1)
    D3 = dict(D, cvec=D["cvec1"])
    ada = emit_p3_ada(P, K, D3)
    P.push_scope()
    G = [emit_gates(P, K, D, d, T_GP) for d in range(2)]
    h_toks = emit_scan(P, K, D, G, TK)
    T_Yg = emit_outstage_f(P, K, D, h_toks, TK)
    P.pop_scope()
    outs = emit_p3(P, K, D3, fused=True, T_Yg=T_Yg, ada=ada)
    P.op("sp", None, r=outs)
    P.finalize()
    return nc, P


def kernel(**inputs):
    inp = {k: np.ascontiguousarray(np.asarray(v)) for k, v in inputs.items()}
    cores = [(b, h) for b in range(2) for h in range(NHEAD)]
    nc, _ = build_fused()
    maps = [host_fused_inputs(inp, b, h) for b, h in cores]
    res = run_bass_kernel_spmd(nc, maps, core_ids=list(range(8))).results
    out = np.zeros((2, TLAT, 1024), np.float32)
    for ci, (b, q) in enumerate(cores):
        out[b, q * 2048:(q + 1) * 2048] = res[ci]["out"]
    return out
```
